# Optimizing a Trainium2 kernel written in Bass

```python
import math
import jax, jax.numpy as jnp
from jax import lax
import numpy as np

D_MODEL = 1024
BATCH = 16
SEQ = 256
DEPTH = 4
DEC_BATCH = 2
DEC_SEQ = 1024
PAST_LEN = 512

GRID_W = 64
FOURIER_WIDTH = 512
FOURIER_GROUPS = 4
FOURIER_GROUP_DIM = FOURIER_WIDTH // FOURIER_GROUPS
CONV_CHANNELS = 512
CONV_TAPS = 31
CONV_PAD = CONV_TAPS // 2
SSM_WIDTH = 512
SSM_GROUP_DIM = 16
SSM_GROUPS = SSM_WIDTH // SSM_GROUP_DIM
SSM_STATE = 64
N_BRANCHES = 3
IN_COLS = FOURIER_WIDTH + 2 * CONV_CHANNELS + SSM_WIDTH + N_BRANCHES * D_MODEL
N_EXPERTS = 32
TOP_K = 4
D_FF = 1024
SWIGLU_LIMIT = 7.0
SWIGLU_ALPHA = 1.702
EPS = 1e-6
N_MOD = 6

kernel_name = "hybrid_fourier_conv_s5_moe_flow_step"

F32 = jnp.float32


def _rmsnorm(x, g):
    xf = x.astype(F32)
    y = xf * lax.rsqrt(jnp.mean(xf * xf, axis=-1, keepdims=True) + EPS)
    return (y * g.astype(F32)).astype(x.dtype)


def _layernorm(x, g, b):
    xf = x.astype(F32)
    mu = jnp.mean(xf, axis=-1, keepdims=True)
    var = jnp.mean(jnp.square(xf - mu), axis=-1, keepdims=True)
    y = (xf - mu) * lax.rsqrt(var + EPS)
    return (y * g.astype(F32) + b.astype(F32)).astype(x.dtype)


def _grid_pos_embed(rows):
    quarter = D_MODEL // 4
    freqs = jnp.exp(-math.log(10000.0) * jnp.arange(quarter, dtype=F32) / quarter)
    r = jnp.repeat(jnp.arange(rows, dtype=F32), GRID_W)
    col = jnp.tile(jnp.arange(GRID_W, dtype=F32), rows)
    ar = r[:, None] * freqs
    ac = col[:, None] * freqs
    return jnp.concatenate([jnp.sin(ar), jnp.cos(ar), jnp.sin(ac), jnp.cos(ac)], axis=-1)


def _adaln(cond, w_mod, b_mod):
    return jax.nn.silu(cond) @ w_mod + b_mod


def _fourier_mix(z):
    bsz, L, _ = z.shape
    zf = z.astype(F32).reshape(bsz, L, FOURIER_GROUPS, FOURIER_GROUP_DIM)
    mixed = jnp.fft.fft2(zf, axes=(1, 3), norm="ortho").real
    return mixed.reshape(bsz, L, FOURIER_WIDTH).astype(z.dtype)


def _conformer_conv(za, zb, w_dw, b_dw, ln_g, ln_b):
    v = za * jax.nn.sigmoid(zb)
    y = lax.conv_general_dilated(
        v, w_dw[:, None, :].astype(v.dtype), window_strides=(1,), padding=[(CONV_PAD, CONV_PAD)],
        dimension_numbers=("NWC", "WIO", "NWC"), feature_group_count=CONV_CHANNELS) + b_dw
    return jax.nn.silu(_layernorm(y, ln_g, ln_b))


def _scan_combine(left, right):
    a_l, b_l = left
    a_r, b_r = right
    return a_l * a_r, a_r * b_l + b_r


def _s5_bidirectional(u, lam_re, lam_im, log_dt, b_re, b_im, c_re, c_im, d, h0):
    bsz, L, _ = u.shape
    ug = u.astype(F32).reshape(bsz, L, SSM_GROUPS, SSM_GROUP_DIM)
    uc = ug.astype(jnp.complex64)
    lam = lax.complex(lam_re.astype(F32), lam_im.astype(F32))
    dt = jnp.exp(log_dt.astype(F32))[..., None]
    lam_bar = jnp.exp(lam * dt)
    b_bar = ((lam_bar - 1.0) / lam)[..., None] * lax.complex(b_re.astype(F32), b_im.astype(F32))
    c_mat = lax.complex(c_re.astype(F32), c_im.astype(F32))
    y = d.astype(F32).reshape(SSM_GROUPS, SSM_GROUP_DIM) * ug
    finals = []
    for direction, rev in ((0, False), (1, True)):
        bu = jnp.einsum("blgh,gph->blgp", uc, b_bar[direction])
        if h0 is not None:
            edge = -1 if rev else 0
            bu = bu.at[:, edge].add(lam_bar[direction] * h0[:, direction])
        a = jnp.broadcast_to(lam_bar[direction], bu.shape)
        _, s = lax.associative_scan(_scan_combine, (a, bu), reverse=rev, axis=1)
        y = y + jnp.einsum("blgp,ghp->blgh", s, c_mat[direction]).real
        if h0 is None:
            finals.append(s[:, 0] if rev else s[:, -1])
    y = y.reshape(bsz, L, SSM_WIDTH).astype(u.dtype)
    if h0 is None:
        return y, jnp.stack(finals, axis=1)
    return y, None


def _moe(h, router_w, router_b, w_gu, b_gu, w_dn, b_dn):
    bsz, L, D = h.shape
    t = h.reshape(-1, D)
    logits = (t @ router_w + router_b).astype(F32)
    top_v, top_i = lax.top_k(logits, TOP_K)
    probs = jax.nn.softmax(top_v, axis=-1)
    combine = jnp.sum(jax.nn.one_hot(top_i, N_EXPERTS, dtype=F32) * probs[..., None], axis=1)

    def expert_step(acc, xs):
        wgu, bgu, wdn, bdn, wt = xs
        gu = t @ wgu + bgu
        gate, up = jnp.split(gu, 2, axis=-1)
        gate = jnp.minimum(gate, SWIGLU_LIMIT)
        up = jnp.clip(up, -SWIGLU_LIMIT, SWIGLU_LIMIT)
        act = (up + 1.0) * (gate * jax.nn.sigmoid(SWIGLU_ALPHA * gate))
        out = act @ wdn + bdn
        return acc + wt[:, None].astype(t.dtype) * out, None

    acc, _ = lax.scan(expert_step, jnp.zeros_like(t), (w_gu, b_gu, w_dn, b_dn, combine.T))
    return acc.reshape(bsz, L, D)


def _layer(x, mod, p, h0):
    sh1, sc1, g1, sh2, sc2, g2 = jnp.split(mod[:, None, :].astype(x.dtype), N_MOD, axis=-1)
    h = _rmsnorm(x, p["norm1_g"]) * (1.0 + sc1) + sh1
    z = h @ p["w_in"] + p["b_in"]
    cuts = [FOURIER_WIDTH, FOURIER_WIDTH + CONV_CHANNELS, FOURIER_WIDTH + 2 * CONV_CHANNELS,
            FOURIER_WIDTH + 2 * CONV_CHANNELS + SSM_WIDTH]
    z_f, z_a, z_b, z_s, z_g = jnp.split(z, cuts, axis=-1)
    br_f = _fourier_mix(z_f) @ p["w_four"]
    br_c = _conformer_conv(z_a, z_b, p["conv_dw"], p["conv_dw_b"], p["conv_ln_g"], p["conv_ln_b"]) @ p["w_conv_out"]
    y_s, finals = _s5_bidirectional(z_s, p["ssm_lam_re"], p["ssm_lam_im"], p["ssm_log_dt"], p["ssm_b_re"],
                                    p["ssm_b_im"], p["ssm_c_re"], p["ssm_c_im"], p["ssm_d"], h0)
    gs = jax.nn.gelu(y_s)
    y_s = gs * jax.nn.sigmoid(gs @ p["w_ssm_glu"] + p["b_ssm_glu"])
    br_s = y_s @ p["w_ssm_out"]
    gate_f, gate_c, gate_s = jnp.split(jax.nn.sigmoid(z_g), N_BRANCHES, axis=-1)
    mixed = (gate_f * br_f + gate_c * br_c + gate_s * br_s) @ p["w_out"]
    x = x + g1 * mixed
    h = _rmsnorm(x, p["norm2_g"]) * (1.0 + sc2) + sh2
    x = x + g2 * _moe(h, p["router_w"], p["router_b"], p["w_gate_up"], p["b_gate_up"], p["w_down"], p["b_down"])
    return x, finals


def setup_inputs(seed: int = 0) -> dict:
    key = jax.random.key(seed)
    ks = iter(jax.random.split(key, 48))

    def nrm(shape, scale):
        return jax.random.normal(next(ks), shape, F32) * scale

    G, P, H = SSM_GROUPS, SSM_STATE, SSM_GROUP_DIM
    lam_im_init = jnp.broadcast_to(math.pi * jnp.arange(P, dtype=F32), (DEPTH, 2, G, P))
    inp = {
        "x_prompt": nrm((BATCH, SEQ, D_MODEL), 1.0),
        "x_sample": nrm((DEC_BATCH, DEC_SEQ, D_MODEL), 1.0),
        "state_ssm_re": nrm((DEC_BATCH, DEPTH, 2, G, P), 0.5),
        "state_ssm_im": nrm((DEC_BATCH, DEPTH, 2, G, P), 0.5),
        "c": nrm((DEC_BATCH, D_MODEL), 1.0),
        "c_ctx": nrm((D_MODEL,), 1.0),
        "w_mod": nrm((DEPTH, D_MODEL, N_MOD * D_MODEL), 0.3 * D_MODEL ** -0.5),
        "b_mod": nrm((DEPTH, N_MOD * D_MODEL), 0.02),
        "norm1_g": 1.0 + nrm((DEPTH, D_MODEL), 0.02),
        "norm2_g": 1.0 + nrm((DEPTH, D_MODEL), 0.02),
        "w_in": nrm((DEPTH, D_MODEL, IN_COLS), D_MODEL ** -0.5),
        "b_in": nrm((DEPTH, IN_COLS), 0.02),
        "w_four": nrm((DEPTH, FOURIER_WIDTH, D_MODEL), FOURIER_WIDTH ** -0.5),
        "conv_dw": nrm((DEPTH, CONV_TAPS, CONV_CHANNELS), CONV_TAPS ** -0.5),
        "conv_dw_b": nrm((DEPTH, CONV_CHANNELS), 0.02),
        "conv_ln_g": 1.0 + nrm((DEPTH, CONV_CHANNELS), 0.02),
        "conv_ln_b": nrm((DEPTH, CONV_CHANNELS), 0.02),
        "w_conv_out": nrm((DEPTH, CONV_CHANNELS, D_MODEL), CONV_CHANNELS ** -0.5),
        "ssm_lam_re": -0.5 + nrm((DEPTH, 2, G, P), 0.01),
        "ssm_lam_im": lam_im_init + nrm((DEPTH, 2, G, P), 0.01),
        "ssm_log_dt": jax.random.uniform(next(ks), (DEPTH, 2, G), F32, math.log(1e-3), math.log(1e-1)),
        "ssm_b_re": nrm((DEPTH, 2, G, P, H), (2.0 * H) ** -0.5),
        "ssm_b_im": nrm((DEPTH, 2, G, P, H), (2.0 * H) ** -0.5),
        "ssm_c_re": nrm((DEPTH, 2, G, H, P), (2.0 * P) ** -0.5),
        "ssm_c_im": nrm((DEPTH, 2, G, H, P), (2.0 * P) ** -0.5),
        "ssm_d": nrm((DEPTH, SSM_WIDTH), 0.5),
        "w_ssm_glu": nrm((DEPTH, SSM_WIDTH, SSM_WIDTH), SSM_WIDTH ** -0.5),
        "b_ssm_glu": nrm((DEPTH, SSM_WIDTH), 0.02),
        "w_ssm_out": nrm((DEPTH, SSM_WIDTH, D_MODEL), SSM_WIDTH ** -0.5),
        "w_out": nrm((DEPTH, D_MODEL, D_MODEL), D_MODEL ** -0.5),
        "router_w": nrm((DEPTH, D_MODEL, N_EXPERTS), D_MODEL ** -0.5),
        "router_b": nrm((DEPTH, N_EXPERTS), 0.01),
        "w_gate_up": nrm((DEPTH, N_EXPERTS, D_MODEL, 2 * D_FF), D_MODEL ** -0.5),
        "b_gate_up": nrm((DEPTH, N_EXPERTS, 2 * D_FF), 0.02),
        "w_down": nrm((DEPTH, N_EXPERTS, D_FF, D_MODEL), D_FF ** -0.5),
        "b_down": nrm((DEPTH, N_EXPERTS, D_MODEL), 0.02),
        "final_norm_g": 1.0 + nrm((D_MODEL,), 0.02),
    }
    return inp


def reference(x_prompt, x_sample, state_ssm_re, state_ssm_im, c, c_ctx, w_mod, b_mod, norm1_g, norm2_g, w_in, b_in,
              w_four, conv_dw, conv_dw_b, conv_ln_g, conv_ln_b, w_conv_out, ssm_lam_re, ssm_lam_im, ssm_log_dt,
              ssm_b_re, ssm_b_im, ssm_c_re, ssm_c_im, ssm_d, w_ssm_glu, b_ssm_glu, w_ssm_out, w_out, router_w,
              router_b, w_gate_up, b_gate_up, w_down, b_down, final_norm_g):
    rows = x_sample.shape[1] // GRID_W
    xs = x_sample + _grid_pos_embed(rows).astype(x_sample.dtype)[None]
    xp = x_prompt
    ctx_finals = []
    for i in range(DEPTH):
        p = dict(norm1_g=norm1_g[i], norm2_g=norm2_g[i], w_in=w_in[i], b_in=b_in[i], w_four=w_four[i],
                 conv_dw=conv_dw[i], conv_dw_b=conv_dw_b[i], conv_ln_g=conv_ln_g[i], conv_ln_b=conv_ln_b[i],
                 w_conv_out=w_conv_out[i], ssm_lam_re=ssm_lam_re[i], ssm_lam_im=ssm_lam_im[i],
                 ssm_log_dt=ssm_log_dt[i], ssm_b_re=ssm_b_re[i], ssm_b_im=ssm_b_im[i], ssm_c_re=ssm_c_re[i],
                 ssm_c_im=ssm_c_im[i], ssm_d=ssm_d[i], w_ssm_glu=w_ssm_glu[i], b_ssm_glu=b_ssm_glu[i],
                 w_ssm_out=w_ssm_out[i], w_out=w_out[i], router_w=router_w[i], router_b=router_b[i],
                 w_gate_up=w_gate_up[i], b_gate_up=b_gate_up[i], w_down=w_down[i], b_down=b_down[i])
        mod_ctx = _adaln(c_ctx[None, :], w_mod[i], b_mod[i])
        xp, fin = _layer(xp, mod_ctx, p, None)
        ctx_finals.append(fin)
        mod_lat = _adaln(c, w_mod[i], b_mod[i])
        h0 = lax.complex(state_ssm_re[:, i].astype(F32), state_ssm_im[:, i].astype(F32))
        xs, _ = _layer(xs, mod_lat, p, h0)
    states = jnp.stack(ctx_finals, axis=1)
    new_state_ssm_re = jnp.real(states).astype(x_prompt.dtype)
    new_state_ssm_im = jnp.imag(states).astype(x_prompt.dtype)
    y_prompt = _rmsnorm(xp, final_norm_g)
    y_sample = _rmsnorm(xs, final_norm_g)
    return (y_prompt, y_sample, new_state_ssm_re, new_state_ssm_im)
```

```python
import math
from contextlib import ExitStack
import numpy as np
import concourse.bass as bass
import concourse.mybir as mybir
from concourse.bass_utils import run_bass_kernel_spmd

F32 = mybir.dt.float32
BF16 = mybir.dt.bfloat16
AF = mybir.ActivationFunctionType
ALU = mybir.AluOpType
AX = mybir.AxisListType

PE, DVE, ACT, POOL, SP = 0, 1, 2, 3, 4
NDMA_SEMS = 24
DEPTH = 4
NEXP = 32
NEXP_RUN = [32]
EPS = 1e-6
TWO_PI = 2.0 * math.pi
ANG_OFF = 64.0 * math.pi


class Buf:
    __slots__ = ("name", "t", "w", "r")

    def __init__(self, name, t):
        self.name = name
        self.t = t
        self.w = None
        self.r = []

    def __getitem__(self, k):
        return self.t[k]


class FW:
    def __init__(self, nc, es):
        self.nc = nc
        self.es = es
        self.engs = [nc.tensor, nc.vector, nc.scalar, nc.gpsimd, nc.sync]
        self.esem = [es.enter_context(nc.semaphore(f"es{i}")) for i in range(5)]
        self.ecnt = [0] * 5
        self.dsem = [es.enter_context(nc.semaphore(f"ds{i}")) for i in range(NDMA_SEMS)]
        self.dcnt = [0] * NDMA_SEMS
        self.dnext = 0
        self.dnext_sw = 0
        self.waited = [dict() for _ in range(5)]
        self.uid = 0
        self.mute = False

    def sb(self, name, shape, dt=F32, es=None):
        self.uid += 1
        t = (es or self.es).enter_context(self.nc.sbuf_tensor(f"{name}_{self.uid}", list(shape), dt))
        esz = 2 if dt == BF16 else 4
        nbytes = esz * int(np.prod(shape[1:]))
        pad = (-nbytes) % 64
        if pad:
            self.uid += 1
            (es or self.es).enter_context(self.nc.sbuf_tensor(f"pad_{self.uid}", [128, pad // 2], BF16))
        return Buf(name, t)

    def ps(self, name, shape, dt=F32, es=None):
        self.uid += 1
        t = (es or self.es).enter_context(self.nc.psum_tensor(f"{name}_{self.uid}", list(shape), dt))
        return Buf(name, t)

    def view(self, name, t):
        return Buf(name, t)

    def _wait(self, e, dep):
        if dep is None:
            return
        kind, idx, cnt = dep
        key = (kind, idx)
        if kind == "e" and idx == PE and e == PE:
            return
        if self.waited[e].get(key, 0) >= cnt:
            return
        sem = self.esem[idx] if kind == "e" else self.dsem[idx]
        self.engs[e].wait_ge(sem, cnt)
        self.waited[e][key] = cnt

    def _deps(self, e, reads, writes):
        for b in reads:
            self._wait(e, b.w)
        for b in writes:
            self._wait(e, b.w)
            for d in b.r:
                self._wait(e, d)

    def _mark(self, dep, reads, writes):
        for b in reads:
            b.r.append(dep)
            if len(b.r) > 16:
                m = {}
                for k, i, c in b.r:
                    m[(k, i)] = max(m.get((k, i), 0), c)
                b.r = [(k, i, c) for (k, i), c in m.items()]
        for b in writes:
            b.w = dep
            b.r = []

    def op(self, e, fn, reads=(), writes=(), sig=True):
        if self.mute:
            return None
        self._deps(e, reads, writes)
        ins = fn()
        if sig:
            self.ecnt[e] += 1
            ins.then_inc(self.esem[e], 1)
            dep = ("e", e, self.ecnt[e])
        else:
            dep = ("e", e, self.ecnt[e] + 1)
        self._mark(dep, reads, writes)
        return ins

    def dma(self, e, out, in_, reads=(), writes=(), **kw):
        if self.mute:
            return None
        self._deps(e, reads, writes)
        half = NDMA_SEMS // 2
        if e == POOL:
            k = half + self.dnext_sw
            self.dnext_sw = (self.dnext_sw + 1) % half
        else:
            k = self.dnext
            self.dnext = (self.dnext + 1) % half
        if self.dcnt[k] > 0:
            self._wait(e, ("d", k, self.dcnt[k]))
        ins = self.engs[e].dma_start(out=out, in_=in_, **kw)
        self.dcnt[k] += 16
        ins.then_inc(self.dsem[k], 16)
        dep = ("d", k, self.dcnt[k])
        self._mark(dep, reads, writes)
        return dep

    def barrier(self):
        for e in range(5):
            for i in range(5):
                if self.ecnt[i] > 0:
                    self._wait(e, ("e", i, self.ecnt[i]))
            for k in range(NDMA_SEMS):
                if self.dcnt[k] > 0:
                    self._wait(e, ("d", k, self.dcnt[k]))


def build_nc(depth=DEPTH, dbg=False, skip=()):
    nc = bass.Bass("TRN2", target_bir_lowering=False)
    V, S, G_, T_ = nc.vector, nc.scalar, nc.gpsimd, nc.tensor

    def din(name, shape, dt=F32):
        return nc.dram_tensor(name, list(shape), dt, kind="ExternalInput").ap()

    def dout(name, shape, dt=F32):
        return nc.dram_tensor(name, list(shape), dt, kind="ExternalOutput").ap()

    xin = din("xin", [1024, 1024])
    pos = din("pos", [1024, 1024])
    condT = din("condT", [128, 8])
    h0row = din("h0row", [depth, 2, 1, 4096])
    CLt = din("CLt", [1024, 1024])
    SLt = din("SLt", [1024, 1024])
    ccsc = din("ccsc", [128, 256])
    tri = din("tri", [2, 128, 128])
    kcol = din("kcol", [128, 4])
    msel = din("msel", [128, 4])
    flag = din("flag", [128, 1])
    jvals = din("jvals", [2, 128, 4, 8])
    dmask = din("dmask", [2, 128, 128])
    w_mod = din("w_mod", [depth, 1024, 6144])
    b_mod = din("b_mod", [depth, 1, 6144])
    n1gT = din("n1gT", [depth, 128, 8])
    n2gT = din("n2gT", [depth, 128, 8])
    w_in = din("w_in", [depth, 1024, 5120])
    b_inT = din("b_inT", [depth, 128, 40])
    b_zs = din("b_zs", [depth, 1, 512])
    w_four = din("w_four", [depth, 512, 1024])
    cdwT = din("cdwT", [depth, 128, 4, 31])
    cvec = din("cvec", [depth, 128, 3, 4])
    w_conv_out = din("w_conv_out", [depth, 512, 1024])
    lam_row = din("lam_row", [depth, 2, 3, 1, 2048])
    lamT = din("lamT", [depth, 2, 128, 3, 32])
    BT2 = din("BT2", [depth, 2, 128, 2, 512])
    CT2 = din("CT2", [depth, 2, 128, 2, 512])
    dcolrep = din("dcolrep", [depth, 128, 32])
    w_glu = din("w_glu", [depth, 512, 512])
    b_gluT = din("b_gluT", [depth, 128, 4])
    w_ssm_out = din("w_ssm_out", [depth, 512, 1024])
    w_out = din("w_out", [depth, 1024, 1024])
    router_w = din("router_w", [depth, 1024, 32])
    router_b = din("router_b", [depth, 1, 32])
    w_gu = din("w_gu", [depth, NEXP_RUN[0], 1024, 2048])
    b_guT = din("b_guT", [depth, 128, NEXP * 16])
    w_dn = din("w_dn", [depth, NEXP_RUN[0], 1024, 1024])
    b_dn = din("b_dn", [depth, NEXP, 1024])
    fin_g = din("fin_g", [1, 1024])
    y_out = dout("y_out", [1024, 1024])
    st_out = dout("st_out", [depth, 2, 4, 4096])
    if dbg:
        dbg_x = dout("dbg_x", [1024, 1024])
        dbg_a = dout("dbg_a", [128, 8192])

    with ExitStack() as es:
        fw = FW(nc, es)
        fw.skipset = set(skip)
        op, dma = fw.op, fw.dma

        x = fw.sb("x", [128, 8, 1024])
        hT = fw.sb("hT", [128, 8, 1024], BF16)
        identf = fw.sb("identf", [128, 128])
        identb = fw.sb("identb", [128, 128], BF16)
        onesf = fw.sb("onesf", [128, 128])
        onesb = fw.sb("onesb", [128, 128], BF16)
        g1b = fw.sb("g1b", [128, 1024])
        g2b = fw.sb("g2b", [128, 1024])
        modcol = fw.sb("modcol", [128, 48])
        gscol = fw.sb("gscol", [128, 16])
        scol = fw.sb("scol", [128, 8], BF16)
        small = fw.sb("small", [128, 64])
        flag_sb = fw.sb("flag_sb", [128, 1])
        kcol_sb = fw.sb("kcol_sb", [128, 4])
        PS = [fw.ps(f"ps{i}", [128, 512]) for i in range(8)]
        OUTB = fw.view("y_out", y_out)
        STB = fw.view("st_out", st_out)

        op(POOL, lambda: G_.memset(identf[:], 1.0), writes=[identf])
        op(POOL, lambda: G_.affine_select(identf[:], identf[:], pattern=[[-1, 128]], compare_op=ALU.is_equal,
                                           fill=0.0, base=0, channel_multiplier=1), reads=[identf], writes=[identf])
        op(DVE, lambda: V.tensor_copy(identb[:], identf[:]), reads=[identf], writes=[identb])
        op(POOL, lambda: G_.memset(onesf[:], 1.0), writes=[onesf])
        op(POOL, lambda: G_.memset(onesb[:], 1.0), writes=[onesb])
        dma(SP, flag_sb[:], flag, writes=[flag_sb])
        dma(SP, kcol_sb[:], kcol, writes=[kcol_sb])

        with ExitStack() as sub:
            ptmp = fw.sb("ptmp", [128, 8, 1024], es=sub)
            for tt in range(8):
                dma(SP, x[:, tt, :], xin[tt * 128:(tt + 1) * 128, :], writes=[x])
                dma(ACT, ptmp[:, tt, :], pos[tt * 128:(tt + 1) * 128, :], writes=[ptmp])
            op(DVE, lambda: V.tensor_tensor(out=x[:], in0=x[:], in1=ptmp[:], op=ALU.add), reads=[x, ptmp], writes=[x])
            ctmp = fw.sb("ctmp", [128, 8], es=sub)
            csig = fw.sb("csig", [128, 8], es=sub)
            dma(SP, ctmp[:], condT, writes=[ctmp])
            op(ACT, lambda: S.activation(out=csig[:], in_=ctmp[:], func=AF.Sigmoid), reads=[ctmp], writes=[csig])
            op(DVE, lambda: V.tensor_tensor(out=scol[:], in0=ctmp[:], in1=csig[:], op=ALU.mult), reads=[ctmp, csig], writes=[scol])
            fw.barrier()

        def norm_transpose(sub, gs_off, sh_off, router=None):
            xn = fw.sb("xn", [128, 1024], es=sub)
            junk = fw.sb("junk", [128, 1024], es=sub)
            ss = fw.sb("ss", [128, 8], es=sub)
            rstd = fw.sb("rstd", [128, 8], es=sub)
            hf = fw.sb("hf", [128, 8, 128], es=sub) if router is not None else None
            op(DVE, lambda: V.memset(ss[:], 0.0), writes=[ss])
            for tt in range(8):
                op(ACT, lambda: S.activation(out=junk[:], in_=x[:, tt, :], func=AF.Square, accum_out=ss[:, tt:tt + 1]),
                   reads=[x], writes=[junk, ss])
                op(DVE, lambda: V.tensor_scalar(out=rstd[:, tt:tt + 1], in0=ss[:, tt:tt + 1], scalar1=1.0 / 1024.0, scalar2=EPS,
                                                op0=ALU.mult, op1=ALU.add), reads=[ss], writes=[rstd])
                op(ACT, lambda: S.sqrt(out=rstd[:, tt:tt + 1], in_=rstd[:, tt:tt + 1]), reads=[rstd], writes=[rstd])
                op(DVE, lambda: V.reciprocal(out=rstd[:, tt:tt + 1], in_=rstd[:, tt:tt + 1]), reads=[rstd], writes=[rstd])
                op(ACT, lambda: S.activation(out=xn[:], in_=x[:, tt, :], func=AF.Copy, scale=rstd[:, tt:tt + 1]),
                   reads=[x, rstd], writes=[xn])
                for half in range(2):
                    pa = PS[half]
                    for kk in range(4):
                        k = half * 4 + kk
                        op(PE, lambda: T_.transpose(pa[:, kk * 128:(kk + 1) * 128], xn[:, k * 128:(k + 1) * 128], identf[:]),
                           reads=[xn, identf], writes=[pa], sig=(kk == 3))
                    for kk in range(4):
                        k = half * 4 + kk
                        op(ACT, lambda: S.activation(out=hT[:, k, tt * 128:(tt + 1) * 128], in_=pa[:, kk * 128:(kk + 1) * 128],
                                                     func=AF.Identity, scale=gscol[:, gs_off + k:gs_off + k + 1],
                                                     bias=modcol[:, sh_off + k:sh_off + k + 1]),
                           reads=[pa, gscol, modcol], writes=[hT])
                        if router is not None:
                            op(ACT, lambda: S.activation(out=hf[:, k, :], in_=pa[:, kk * 128:(kk + 1) * 128],
                                                         func=AF.Identity, scale=gscol[:, gs_off + k:gs_off + k + 1],
                                                         bias=modcol[:, sh_off + k:sh_off + k + 1]),
                               reads=[pa, gscol, modcol], writes=[hf])
                if router is not None:
                    router(tt, hf)

        for L in range(depth):
            with ExitStack() as sub:
                modrow = fw.sb("modrow", [1, 6144], es=sub)
                bmrow = fw.sb("bmrow", [1, 6144], es=sub)
                ngT = fw.sb("ngT", [128, 16], es=sub)
                wm = [fw.sb(f"wm{i}", [128, 8, 512], BF16, es=sub) for i in range(2)]
                dma(SP, bmrow[:], b_mod[L], writes=[bmrow])
                dma(SP, ngT[:, 0:8], n1gT[L], writes=[ngT])
                dma(SP, ngT[:, 8:16], n2gT[L], writes=[ngT])
                for n in range(12):
                    wb = wm[n % 2]
                    dma(POOL, wb[:], w_mod[L][:, n * 512:(n + 1) * 512].rearrange("(k p) n -> p k n", p=128), writes=[wb])
                    pa = PS[n % 2]
                    for k in range(8):
                        op(PE, lambda: T_.matmul(pa[0:1, :], lhsT=scol[:, k:k + 1], rhs=wb[:, k, :], start=(k == 0), stop=(k == 7)),
                           reads=[scol, wb], writes=[pa], sig=(k == 7))
                    op(DVE, lambda: V.tensor_tensor(out=modrow[0:1, n * 512:(n + 1) * 512], in0=pa[0:1, :],
                                                    in1=bmrow[0:1, n * 512:(n + 1) * 512], op=ALU.add),
                       reads=[pa, bmrow], writes=[modrow])
                pa = PS[2]
                for j in range(48):
                    op(PE, lambda: T_.matmul(pa[:, j:j + 1], lhsT=modrow[0:1, j * 128:(j + 1) * 128], rhs=onesf[0:1, 0:1],
                                             start=True, stop=True), reads=[modrow, onesf], writes=[pa], sig=(j == 47))
                op(DVE, lambda: V.tensor_copy(modcol[:], pa[:, 0:48]), reads=[pa], writes=[modcol])
                for i, m in enumerate((1, 4)):
                    op(DVE, lambda: V.scalar_tensor_tensor(out=gscol[:, i * 8:(i + 1) * 8], in0=modcol[:, m * 8:(m + 1) * 8], scalar=1.0,
                                                           in1=ngT[:, i * 8:(i + 1) * 8], op0=ALU.add, op1=ALU.mult),
                       reads=[modcol, ngT], writes=[gscol])
                for gb, m in ((g1b, 2), (g2b, 5)):
                    for hh in range(2):
                        pb = PS[3 + hh]
                        op(PE, lambda: T_.matmul(pb[:], lhsT=onesf[0:1, :], rhs=modrow[0:1, m * 1024 + hh * 512: m * 1024 + (hh + 1) * 512],
                                                 start=True, stop=True), reads=[onesf, modrow], writes=[pb])
                        op(DVE, lambda: V.tensor_copy(gb[:, hh * 512:(hh + 1) * 512], pb[:]), reads=[pb], writes=[gb])
                fw.barrier()

            with ExitStack() as sub:
                norm_transpose(sub, 0, 0)
                fw.barrier()

            with ExitStack() as mix:
                ys2T = fw.sb("ys2T", [128, 4, 1024], BF16, es=mix)
                binT = fw.sb("binT", [128, 40], es=mix)
                dma(SP, binT[:], b_inT[L], writes=[binT])

                fw.mute = "s5" in skip
                with ExitStack() as sub:
                    Zc = fw.sb("Zc", [128, 32, 128], BF16, es=sub)
                    with ExitStack() as sz:
                        ws = fw.sb("ws", [128, 8, 512], BF16, es=sz)
                        bzs = fw.sb("bzs", [128, 512], es=sz)
                        dma(SP, bzs[:], b_zs[L].partition_broadcast(128), writes=[bzs])
                        dma(POOL, ws[:], w_in[L][:, 1536:2048].rearrange("(k p) n -> p k n", p=128), writes=[ws])
                        for j in range(8):
                            pa = PS[4 + j % 2]
                            for k in range(8):
                                op(PE, lambda: T_.matmul(pa[:], lhsT=hT[:, k, j::8], rhs=ws[:, k, :], start=(k == 0), stop=(k == 7)),
                                   reads=[ws, hT], writes=[pa], sig=(k == 7))
                            op(DVE, lambda: V.tensor_tensor(out=Zc[:].rearrange("c g (j h) -> c g j h", h=16)[:, :, j, :],
                                                            in0=pa[:].rearrange("c (g h) -> c g h", h=16),
                                                            in1=bzs[:].rearrange("c (g h) -> c g h", h=16), op=ALU.add),
                               reads=[pa, bzs], writes=[Zc])
                        fw.barrier()
                    s5_block(fw, nc, sub, L, PS, Zc, ys2T, identb, identf, onesb, kcol_sb,
                             dict(lam_row=lam_row, lamT=lamT, BT2=BT2, CT2=CT2, dcolrep=dcolrep, jvals=jvals, dmask=dmask,
                                  tri=tri, msel=msel, h0row=h0row, w_glu=w_glu, b_gluT=b_gluT), STB, st_out)
                    fw.barrier()

                fw.mute = "stageA" in skip
                mixT = fw.sb("mixT", [128, 4, 1024], BF16, es=mix)
                convT = fw.sb("convT", [128, 4, 1024], BF16, es=mix)

                with ExitStack() as sub:
                    wi = [fw.sb(f"wi{i}", [128, 8, 512], BF16, es=sub) for i in range(2)]
                    zfT = fw.sb("zfT", [128, 4, 1024], BF16, es=sub)
                    vpad = fw.sb("vpad", [128, 4, 4, 286], es=sub)
                    sig = fw.sb("sig", [128, 512], es=sub)
                    op(POOL, lambda: G_.memset(vpad[:], 0.0), writes=[vpad])

                    def load_wi(piece):
                        wb = wi[piece % 2]
                        dma(POOL, wb[:], w_in[L][:, piece * 512:(piece + 1) * 512].rearrange("(k p) n -> p k n", p=128), writes=[wb])
                        return wb

                    wb = load_wi(0)
                    for cc in range(4):
                        for th in range(2):
                            pa = PS[(cc * 2 + th) % 2]
                            for k in range(8):
                                op(PE, lambda: T_.matmul(pa[:], lhsT=wb[:, k, cc * 128:(cc + 1) * 128], rhs=hT[:, k, th * 512:(th + 1) * 512],
                                                         start=(k == 0), stop=(k == 7)), reads=[wb, hT], writes=[pa], sig=(k == 7))
                            op(ACT, lambda: S.activation(out=zfT[:, cc, th * 512:(th + 1) * 512], in_=pa[:], func=AF.Identity,
                                                         bias=binT[:, cc:cc + 1]), reads=[pa, binT], writes=[zfT])
                    wa = load_wi(1)
                    wbb = load_wi(2)
                    for q in range(4):
                        for th in range(2):
                            pa, pb = PS[2], PS[3]
                            for k in range(8):
                                op(PE, lambda: T_.matmul(pa[:], lhsT=wa[:, k, q * 128:(q + 1) * 128], rhs=hT[:, k, th * 512:(th + 1) * 512],
                                                         start=(k == 0), stop=(k == 7)), reads=[wa, hT], writes=[pa], sig=(k == 7))
                            for k in range(8):
                                op(PE, lambda: T_.matmul(pb[:], lhsT=wbb[:, k, q * 128:(q + 1) * 128], rhs=hT[:, k, th * 512:(th + 1) * 512],
                                                         start=(k == 0), stop=(k == 7)), reads=[wbb, hT], writes=[pb], sig=(k == 7))
                            op(ACT, lambda: S.activation(out=sig[:], in_=pb[:], func=AF.Sigmoid, bias=binT[:, 8 + q:9 + q]),
                               reads=[pb, binT], writes=[sig])
                            op(DVE, lambda: V.scalar_tensor_tensor(
                                out=vpad[:, q, th * 2:(th + 1) * 2, 15:271], in0=pa[:].rearrange("p (s t) -> p s t", s=2),
                                scalar=binT[:, 4 + q:5 + q], in1=sig[:].rearrange("p (s t) -> p s t", s=2), op0=ALU.add, op1=ALU.mult),
                               reads=[pa, sig, binT], writes=[vpad])

                    with ExitStack() as sf:
                        CL = fw.sb("CL", [128, 8, 1024], BF16, es=sf)
                        SL = fw.sb("SL", [128, 8, 1024], BF16, es=sf)
                        ccs = fw.sb("ccs", [128, 256], BF16, es=sf)
                        Ucs = fw.sb("Ucs", [128, 8, 1024], BF16, es=sf)
                        dma(POOL, ccs[:], ccsc, writes=[ccs])
                        for h2 in range(2):
                            dma(POOL, CL[:, h2 * 4:(h2 + 1) * 4, :], CLt[h2 * 512:(h2 + 1) * 512, :].rearrange("(k p) n -> p k n", p=128), writes=[CL])
                            dma(POOL, SL[:, h2 * 4:(h2 + 1) * 4, :], SLt[h2 * 512:(h2 + 1) * 512, :].rearrange("(k p) n -> p k n", p=128), writes=[SL])
                        for tt in range(8):
                            for gh in range(2):
                                pa = PS[gh]
                                for gg in range(2):
                                    g = gh * 2 + gg
                                    op(PE, lambda: T_.matmul(pa[:, gg * 256:(gg + 1) * 256], lhsT=zfT[:, g, tt * 128:(tt + 1) * 128], rhs=ccs[:],
                                                             start=True, stop=True), reads=[zfT, ccs], writes=[pa], sig=(gg == 1))
                                op(ACT if gh else DVE,
                                   (lambda: S.copy(out=Ucs[:, tt, gh * 512:(gh + 1) * 512], in_=pa[:])) if gh else
                                   (lambda: V.tensor_copy(Ucs[:, tt, gh * 512:(gh + 1) * 512], pa[:])),
                                   reads=[pa], writes=[Ucs])
                        for g in range(4):
                            for th in range(2):
                                pa = PS[2 + (g * 2 + th) % 2]
                                for lc in range(8):
                                    op(PE, lambda: T_.matmul(pa[:], lhsT=Ucs[:, lc, g * 256:g * 256 + 128], rhs=CL[:, lc, th * 512:(th + 1) * 512],
                                                             start=(lc == 0), stop=False), reads=[Ucs, CL], writes=[pa], sig=False)
                                    op(PE, lambda: T_.matmul(pa[:], lhsT=Ucs[:, lc, g * 256 + 128:g * 256 + 256], rhs=SL[:, lc, th * 512:(th + 1) * 512],
                                                             start=False, stop=(lc == 7)), reads=[Ucs, SL], writes=[pa], sig=(lc == 7))
                                op(ACT, lambda: S.copy(out=mixT[:, g, th * 512:(th + 1) * 512], in_=pa[:]), reads=[pa], writes=[mixT])
                        fw.barrier()

                    with ExitStack() as sc:
                        cw = fw.sb("cw", [128, 4, 31], es=sc)
                        cv = fw.sb("cv", [128, 3, 4], es=sc)
                        yc = fw.sb("yc", [128, 4, 1024], es=sc)
                        ysq = fw.sb("ysq", [128, 1024], es=sc)
                        mean = fw.sb("mean", [128, 1024], es=sc)
                        rs = fw.sb("rs", [128, 1024], es=sc)
                        t1 = fw.sb("t1", [128, 1024], es=sc)
                        t2 = fw.sb("t2", [128, 1024], es=sc)
                        dma(SP, cw[:], cdwT[L], writes=[cw])
                        dma(SP, cv[:], cvec[L], writes=[cv])
                        for q in range(4):
                            op(DVE, lambda: V.tensor_scalar(out=vpad[:, q, 1:4, 0:15], in0=vpad[:, q, 0:3, 256:271], scalar1=flag_sb[:, 0:1],
                                                            scalar2=None, op0=ALU.mult), reads=[vpad, flag_sb], writes=[vpad])
                            op(DVE, lambda: V.tensor_scalar(out=vpad[:, q, 0:3, 271:286], in0=vpad[:, q, 1:4, 15:30], scalar1=flag_sb[:, 0:1],
                                                            scalar2=None, op0=ALU.mult), reads=[vpad, flag_sb], writes=[vpad])
                            yv = yc[:, q, :].rearrange("p (s t) -> p s t", s=4)
                            op(DVE, lambda: V.tensor_scalar(out=yv, in0=vpad[:, q, :, 0:256], scalar1=cw[:, q, 0:1], scalar2=cv[:, 0, q:q + 1],
                                                            op0=ALU.mult, op1=ALU.add), reads=[vpad, cw, cv], writes=[yc])
                            for k in range(1, 31):
                                op(DVE, lambda: V.scalar_tensor_tensor(out=yv, in0=vpad[:, q, :, k:k + 256], scalar=cw[:, q, k:k + 1], in1=yv,
                                                                       op0=ALU.mult, op1=ALU.add), reads=[vpad, cw, yc], writes=[yc])
                        for th in range(2):
                            pa, pb = PS[th], PS[2 + th]
                            for q in range(4):
                                op(ACT, lambda: S.activation(out=ysq[:, 0:512], in_=yc[:, q, th * 512:(th + 1) * 512], func=AF.Square),
                                   reads=[yc], writes=[ysq])
                                op(PE, lambda: T_.matmul(pa[:], lhsT=onesf[:], rhs=yc[:, q, th * 512:(th + 1) * 512], start=(q == 0), stop=(q == 3)),
                                   reads=[onesf, yc], writes=[pa], sig=True)
                                op(PE, lambda: T_.matmul(pb[:], lhsT=onesf[:], rhs=ysq[:, 0:512], start=(q == 0), stop=(q == 3)),
                                   reads=[onesf, ysq], writes=[pb], sig=True)
                            sl = slice(th * 512, (th + 1) * 512)
                            op(DVE, lambda: V.tensor_scalar(out=mean[:, sl], in0=pa[:], scalar1=1.0 / 512.0, scalar2=None, op0=ALU.mult),
                               reads=[pa], writes=[mean])
                            op(DVE, lambda: V.tensor_tensor(out=t1[:, sl], in0=mean[:, sl], in1=mean[:, sl], op=ALU.mult), reads=[mean], writes=[t1])
                            op(DVE, lambda: V.scalar_tensor_tensor(out=rs[:, sl], in0=pb[:], scalar=1.0 / 512.0, in1=t1[:, sl],
                                                                   op0=ALU.mult, op1=ALU.subtract), reads=[pb, t1], writes=[rs])
                            op(DVE, lambda: V.tensor_scalar(out=rs[:, sl], in0=rs[:, sl], scalar1=EPS, scalar2=None, op0=ALU.add),
                               reads=[rs], writes=[rs])
                            op(ACT, lambda: S.sqrt(out=rs[:, sl], in_=rs[:, sl]), reads=[rs], writes=[rs])
                            op(DVE, lambda: V.reciprocal(out=rs[:, sl], in_=rs[:, sl]), reads=[rs], writes=[rs])
                        for q in range(4):
                            op(DVE, lambda: V.tensor_tensor(out=t1[:], in0=yc[:, q, :], in1=mean[:], op=ALU.subtract), reads=[yc, mean], writes=[t1])
                            op(DVE, lambda: V.tensor_tensor(out=t1[:], in0=t1[:], in1=rs[:], op=ALU.mult), reads=[t1, rs], writes=[t1])
                            op(DVE, lambda: V.tensor_scalar(out=t1[:], in0=t1[:], scalar1=cv[:, 1, q:q + 1], scalar2=cv[:, 2, q:q + 1],
                                                            op0=ALU.mult, op1=ALU.add), reads=[t1, cv], writes=[t1])
                            op(ACT, lambda: S.activation(out=t2[:], in_=t1[:], func=AF.Sigmoid), reads=[t1], writes=[t2])
                            op(DVE, lambda: V.tensor_tensor(out=convT[:, q, :], in0=t1[:], in1=t2[:], op=ALU.mult), reads=[t1, t2], writes=[convT])
                        fw.barrier()
                    fw.barrier()

                fw.mute = "stageC" in skip
                with ExitStack() as sub:
                    wbr = [fw.sb(f"wbr{i}", [128, 4, 1024], BF16, es=sub) for i in range(3)]
                    wg = [fw.sb(f"wg{i}", [128, 8, 3, 128], BF16, es=sub) for i in range(2)]
                    wo = fw.sb("wo", [128, 8, 1024], BF16, es=sub)
                    mixedT = fw.sb("mixedT", [128, 8, 1024], BF16, es=sub)
                    sg = [fw.sb(f"sg{i}", [128, 512], es=sub) for i in range(3)]
                    acc = fw.sb("accm", [128, 512], es=sub)
                    tmp = fw.sb("tmpm", [128, 512], es=sub)
                    for i, wsrc in enumerate((w_four, w_conv_out, w_ssm_out)):
                        dma(POOL, wbr[i][:], wsrc[L].rearrange("(k p) n -> p k n", p=128), writes=[wbr[i]])
                    for h2 in range(2):
                        dma(POOL, wo[:, h2 * 4:(h2 + 1) * 4, :], w_out[L][h2 * 512:(h2 + 1) * 512, :].rearrange("(k p) n -> p k n", p=128), writes=[wo])
                    brT = (mixT, convT, ys2T)
                    for dc in range(8):
                        wgb = wg[dc % 2]
                        for b in range(3):
                            c0 = 2048 + b * 1024 + dc * 128
                            dma(POOL, wgb[:, :, b, :], w_in[L][:, c0:c0 + 128].rearrange("(k p) n -> p k n", p=128), writes=[wgb])
                        for th in range(2):
                            ts = slice(th * 512, (th + 1) * 512)
                            for b in range(3):
                                pg = PS[b]
                                for k in range(8):
                                    op(PE, lambda: T_.matmul(pg[:], lhsT=wgb[:, k, b, :], rhs=hT[:, k, ts], start=(k == 0), stop=(k == 7)),
                                       reads=[wgb, hT], writes=[pg], sig=(k == 7))
                                op(ACT, lambda: S.activation(out=sg[b][:], in_=pg[:], func=AF.Sigmoid, bias=binT[:, 16 + b * 8 + dc:17 + b * 8 + dc]),
                                   reads=[pg, binT], writes=[sg[b]])
                                pb = PS[3 + b]
                                for k in range(4):
                                    op(PE, lambda: T_.matmul(pb[:], lhsT=wbr[b][:, k, dc * 128:(dc + 1) * 128], rhs=brT[b][:, k, ts],
                                                             start=(k == 0), stop=(k == 3)), reads=[wbr[b], brT[b]], writes=[pb], sig=(k == 3))
                                if b == 0:
                                    op(DVE, lambda: V.tensor_tensor(out=acc[:], in0=pb[:], in1=sg[b][:], op=ALU.mult), reads=[pb, sg[b]], writes=[acc])
                                else:
                                    op(DVE, lambda: V.tensor_tensor(out=tmp[:], in0=pb[:], in1=sg[b][:], op=ALU.mult), reads=[pb, sg[b]], writes=[tmp])
                                    if b == 1:
                                        op(DVE, lambda: V.tensor_tensor(out=acc[:], in0=acc[:], in1=tmp[:], op=ALU.add), reads=[acc, tmp], writes=[acc])
                                    else:
                                        op(DVE, lambda: V.tensor_tensor(out=mixedT[:, dc, ts], in0=acc[:], in1=tmp[:], op=ALU.add),
                                           reads=[acc, tmp], writes=[mixedT])
                    for tt in range(8):
                        for dh in range(2):
                            pa = PS[6 + dh]
                            ds_ = slice(dh * 512, (dh + 1) * 512)
                            for k in range(8):
                                op(PE, lambda: T_.matmul(pa[:], lhsT=mixedT[:, k, tt * 128:(tt + 1) * 128], rhs=wo[:, k, ds_], start=(k == 0), stop=(k == 7)),
                                   reads=[mixedT, wo], writes=[pa], sig=(k == 7))
                            op(DVE, lambda: V.tensor_tensor(out=tmp[:], in0=pa[:], in1=g1b[:, ds_], op=ALU.mult), reads=[pa, g1b], writes=[tmp])
                            op(DVE, lambda: V.tensor_tensor(out=x[:, tt, ds_], in0=x[:, tt, ds_], in1=tmp[:], op=ALU.add), reads=[x, tmp], writes=[x])
                    fw.barrier()
                fw.barrier()

            fw.mute = False
            if dbg and L == 0:
                DX = fw.view("dbg_x", dbg_x)
                for tt in range(8):
                    dma(SP, dbg_x[tt * 128:(tt + 1) * 128, :], x[:, tt, :], reads=[x], writes=[DX])

            fw.mute = "moe" in skip
            with ExitStack() as moe:
                moe_block(fw, nc, moe, L, PS, x, hT, g2b, identf, norm_transpose,
                          dict(router_w=router_w, router_b=router_b, w_gu=w_gu, b_guT=b_guT, w_dn=w_dn, b_dn=b_dn))
                fw.barrier()

        fw.mute = False
        with ExitStack() as sub:
            fg = fw.sb("fg", [128, 1024], es=sub)
            junk = fw.sb("junkf", [128, 1024], es=sub)
            ss = fw.sb("ssf", [128, 8], es=sub)
            yo = [fw.sb(f"yo{i}", [128, 1024], es=sub) for i in range(2)]
            dma(SP, fg[:], fin_g.partition_broadcast(128), writes=[fg])
            op(DVE, lambda: V.memset(ss[:], 0.0), writes=[ss])
            for tt in range(8):
                op(ACT, lambda: S.activation(out=junk[:], in_=x[:, tt, :], func=AF.Square, accum_out=ss[:, tt:tt + 1]), reads=[x], writes=[junk, ss])
                op(DVE, lambda: V.tensor_scalar(out=ss[:, tt:tt + 1], in0=ss[:, tt:tt + 1], scalar1=1.0 / 1024.0, scalar2=EPS, op0=ALU.mult, op1=ALU.add),
                   reads=[ss], writes=[ss])
                op(ACT, lambda: S.sqrt(out=ss[:, tt:tt + 1], in_=ss[:, tt:tt + 1]), reads=[ss], writes=[ss])
                op(DVE, lambda: V.reciprocal(out=ss[:, tt:tt + 1], in_=ss[:, tt:tt + 1]), reads=[ss], writes=[ss])
                yb = yo[tt % 2]
                op(DVE, lambda: V.scalar_tensor_tensor(out=yb[:], in0=x[:, tt, :], scalar=ss[:, tt:tt + 1], in1=fg[:], op0=ALU.mult, op1=ALU.mult),
                   reads=[x, ss, fg], writes=[yb])
                dma(SP, y_out[tt * 128:(tt + 1) * 128, :], yb[:], reads=[yb], writes=[OUTB])
            fw.barrier()
    return nc


def moe_block(fw, nc, es, L, PS, x, hT, g2b, identf, norm_transpose, W):
    V, S, G_, T_ = nc.vector, nc.scalar, nc.gpsimd, nc.tensor
    op, dma = fw.op, fw.dma
    if "moe_early" in fw.skipset:
        with ExitStack() as sub0:
            norm_transpose(sub0, 8, 24, router=None)
            fw.barrier()
    comb = fw.sb("comb", [128, 8, 32], es=es)
    rw = fw.sb("rw", [128, 8, 32], es=es)
    rb = fw.sb("rb", [128, 32], es=es)
    lg = fw.sb("lg", [128, 32], es=es)
    ex = fw.sb("ex", [128, 32], es=es)
    mk = fw.sb("mk", [128, 32], es=es)
    m8 = fw.sb("m8", [128, 8], es=es)
    sm = fw.sb("sm", [128, 4], es=es)
    fw.mute = ("moe" in fw.skipset) or ("moe_loads" in fw.skipset)
    dma(SP, rw[:], W["router_w"][L].rearrange("(k p) e -> p k e", p=128), writes=[rw])
    dma(SP, rb[:], W["router_b"][L].partition_broadcast(128), writes=[rb])
    fw.barrier()
    fw.mute = "moe" in fw.skipset

    def router(tt, hf):
        pl = PS[6]
        for k in range(8):
            op(PE, lambda: T_.matmul(pl[:, 0:32], lhsT=hf[:, k, :], rhs=rw[:, k, :], start=(k == 0), stop=(k == 7)),
               reads=[hf, rw], writes=[pl], sig=(k == 7))
        op(DVE, lambda: V.tensor_tensor(out=lg[:], in0=pl[:, 0:32], in1=rb[:], op=ALU.add), reads=[pl, rb], writes=[lg])
        if "moe_rt_mm_only" in fw.skipset:
            return
        op(DVE, lambda: V.max(m8[:], lg[:]), reads=[lg], writes=[m8])
        op(DVE, lambda: V.tensor_scalar(out=sm[:, 0:1], in0=m8[:, 0:1], scalar1=-1.0, scalar2=None, op0=ALU.mult), reads=[m8], writes=[sm])
        op(ACT, lambda: S.activation(out=ex[:], in_=lg[:], func=AF.Exp, bias=sm[:, 0:1]), reads=[lg, sm], writes=[ex])
        op(DVE, lambda: V.tensor_scalar(out=mk[:], in0=lg[:], scalar1=m8[:, 3:4], scalar2=None, op0=ALU.is_ge), reads=[lg, m8], writes=[mk])
        op(DVE, lambda: V.tensor_tensor(out=ex[:], in0=ex[:], in1=mk[:], op=ALU.mult), reads=[ex, mk], writes=[ex])
        op(DVE, lambda: V.reduce_sum(out=sm[:, 1:2], in_=ex[:], axis=AX.X), reads=[ex], writes=[sm])
        op(DVE, lambda: V.reciprocal(out=sm[:, 2:3], in_=sm[:, 1:2]), reads=[sm], writes=[sm])
        op(DVE, lambda: V.tensor_scalar(out=comb[:, tt, :], in0=ex[:], scalar1=sm[:, 2:3], scalar2=None, op0=ALU.mult), reads=[ex, sm], writes=[comb])

    with ExitStack() as sub:
        fw.mute = "moe_router" in fw.skipset
        norm_transpose(sub, 8, 24, router=(None if "moe_norouter" in fw.skipset else router))
        fw.barrier()
        fw.mute = "moe" in fw.skipset

    acc = fw.sb("acc", [128, 8, 1024], es=es)
    bgu = fw.sb("bgu", [128, 512], es=es)
    bgu1 = fw.sb("bgu1", [128, 512], es=es)
    bdn = fw.sb("bdn", [32, 1024], es=es)
    combT = fw.sb("combT", [32, 1024], es=es)
    fw.mute = ("moe" in fw.skipset) or ("moe_loads" in fw.skipset)
    dma(SP, bgu[:], W["b_guT"][L], writes=[bgu])
    dma(SP, bdn[:], W["b_dn"][L], writes=[bdn])
    op(DVE, lambda: V.tensor_scalar(out=bgu1[:], in0=bgu[:], scalar1=1.0, scalar2=None, op0=ALU.add), reads=[bgu], writes=[bgu1])
    fw.mute = "moe_init" in fw.skipset
    for half in range(2):
        pa = PS[half]
        for t4 in range(4):
            tt = half * 4 + t4
            op(PE, lambda: T_.transpose(pa[0:32, t4 * 128:(t4 + 1) * 128], comb[:, tt, :], identf[:]), reads=[comb, identf], writes=[pa], sig=(t4 == 3))
        op(DVE, lambda: V.tensor_copy(combT[:, half * 512:(half + 1) * 512], pa[0:32, :]), reads=[pa], writes=[combT])
    for tt in range(8):
        for dh in range(2):
            pa = PS[2 + dh]
            op(PE, lambda: T_.matmul(pa[:], lhsT=combT[:, tt * 128:(tt + 1) * 128], rhs=bdn[:, dh * 512:(dh + 1) * 512], start=True, stop=True),
               reads=[combT, bdn], writes=[pa])
            op(ACT, lambda: S.copy(out=acc[:, tt, dh * 512:(dh + 1) * 512], in_=pa[:]), reads=[pa], writes=[acc])

    fw.mute = "moe" in fw.skipset
    wgu = [fw.sb(f"wgu{i}", [128, 8, 2, 256], BF16, es=es) for i in range(3)]
    wdn = [fw.sb(f"wdn{i}", [128, 8, 1024], BF16, es=es) for i in range(2)]
    actT = [fw.sb(f"actT{i}", [128, 8, 1024], BF16, es=es) for i in range(2)]
    gm = [fw.sb(f"gm{i}", [128, 512], es=es) for i in range(2)]
    sg = [fw.sb(f"sgm{i}", [128, 512], es=es) for i in range(2)]
    um = [fw.sb(f"um{i}", [128, 512], es=es) for i in range(2)]
    cnt = [0]

    def GU(e):
        at = actT[e % 2]
        wd = wdn[e % 2]
        for h2 in range(2):
            dma(POOL, wd[:, h2 * 4:(h2 + 1) * 4, :], W["w_dn"][L, e][h2 * 512:(h2 + 1) * 512, :].rearrange("(k p) n -> p k n", p=128), writes=[wd])
        for pc in range(4):
            wb = wgu[cnt[0] % 3]
            cnt[0] += 1
            for two in range(2):
                c0 = two * 1024 + pc * 256
                dma(POOL, wb[:, :, two, :], W["w_gu"][L, e][:, c0:c0 + 256].rearrange("(k p) n -> p k n", p=128), writes=[wb])
            for fl in range(2):
                fc = pc * 2 + fl
                for th in range(2):
                    u = (fc * 2 + th) % 2
                    ts = slice(th * 512, (th + 1) * 512)
                    pg, pu = PS[u], PS[2 + u]
                    for k in range(8):
                        op(PE, lambda: T_.matmul(pg[:], lhsT=wb[:, k, 0, fl * 128:(fl + 1) * 128], rhs=hT[:, k, ts], start=(k == 0), stop=(k == 7)),
                           reads=[wb, hT], writes=[pg], sig=(k == 7))
                    for k in range(8):
                        op(PE, lambda: T_.matmul(pu[:], lhsT=wb[:, k, 1, fl * 128:(fl + 1) * 128], rhs=hT[:, k, ts], start=(k == 0), stop=(k == 7)),
                           reads=[wb, hT], writes=[pu], sig=(k == 7))
                    cg = e * 16 + fc
                    op(DVE, lambda: V.tensor_scalar(out=gm[u][:], in0=pg[:], scalar1=bgu[:, cg:cg + 1], scalar2=7.0, op0=ALU.add, op1=ALU.min),
                       reads=[pg, bgu], writes=[gm[u]])
                    op(ACT, lambda: S.activation(out=sg[u][:], in_=gm[u][:], func=AF.Sigmoid, scale=1.702), reads=[gm[u]], writes=[sg[u]])
                    op(DVE, lambda: V.tensor_scalar(out=um[u][:], in0=pu[:], scalar1=bgu1[:, cg + 8:cg + 9], scalar2=8.0, op0=ALU.add, op1=ALU.min),
                       reads=[pu, bgu1], writes=[um[u]])
                    op(DVE, lambda: V.tensor_tensor(out=gm[u][:], in0=gm[u][:], in1=sg[u][:], op=ALU.mult), reads=[gm[u], sg[u]], writes=[gm[u]])
                    op(DVE, lambda: V.scalar_tensor_tensor(out=at[:, fc, ts], in0=um[u][:], scalar=-6.0, in1=gm[u][:], op0=ALU.max, op1=ALU.mult),
                       reads=[um[u], gm[u]], writes=[at])

    def DN(e):
        at = actT[e % 2]
        wd = wdn[e % 2]
        for tt in range(8):
            for dh in range(2):
                pa = PS[4 + (tt * 2 + dh) % 2]
                ds_ = slice(dh * 512, (dh + 1) * 512)
                for f in range(8):
                    op(PE, lambda: T_.matmul(pa[:], lhsT=at[:, f, tt * 128:(tt + 1) * 128], rhs=wd[:, f, ds_], start=(f == 0), stop=(f == 7)),
                       reads=[at, wd], writes=[pa], sig=(f == 7))
                op(DVE, lambda: V.scalar_tensor_tensor(out=acc[:, tt, ds_], in0=pa[:], scalar=comb[:, tt, e:e + 1], in1=acc[:, tt, ds_],
                                                       op0=ALU.mult, op1=ALU.add), reads=[pa, comb, acc], writes=[acc])

    fw.mute = "moe_exp" in fw.skipset
    GU(0)
    for e in range(NEXP_RUN[0]):
        if e + 1 < NEXP_RUN[0]:
            GU(e + 1)
        DN(e)
    fw.mute = ("moe" in fw.skipset) or ("moe_fin" in fw.skipset)
    for tt in range(8):
        op(DVE, lambda: V.tensor_tensor(out=acc[:, tt, :], in0=acc[:, tt, :], in1=g2b[:], op=ALU.mult), reads=[acc, g2b], writes=[acc])
        op(DVE, lambda: V.tensor_tensor(out=x[:, tt, :], in0=x[:, tt, :], in1=acc[:, tt, :], op=ALU.add), reads=[x, acc], writes=[x])


def s5_block(fw, nc, es, L, PS, Zc, ys2T, identb, identf, onesb, kcol_sb, W, STB, st_out):
    V, S, G_, T_ = nc.vector, nc.scalar, nc.gpsimd, nc.tensor
    op, dma = fw.op, fw.dma
    PI = math.pi
    U = fw.sb("U", [128, 32, 128], BF16, es=es)
    ST = [fw.sb(f"ST{r}", [128, 32, 128], BF16, es=es) for r in range(2)]
    ChT = [fw.sb(f"ChT{r}", [128, 32, 128], BF16, es=es) for r in range(2)]
    Dh = [fw.sb(f"Dh{r}", [128, 32, 128], BF16, es=es) for r in range(2)]
    fin = fw.sb("fin", [4, 1024], es=es)
    cst = fw.sb("cst", [128, 8], es=es)
    op(POOL, lambda: G_.memset(cst[:, 0:1], 0.5 * PI), writes=[cst])
    op(POOL, lambda: G_.memset(cst[:, 1:2], 256.0), writes=[cst])
    op(DVE, lambda: V.tensor_scalar(out=cst[:, 2:4], in0=kcol_sb[:, 0:2], scalar1=-1.0, scalar2=None, op0=ALU.mult), reads=[kcol_sb], writes=[cst])
    negpi = cst[:, 0:1]

    def bfview(pb):
        return pb[:].bitcast(BF16)

    def sincos(th, t1, s_out, c_out, ti):
        I2P = 1.0 / TWO_PI
        op(DVE, lambda: V.tensor_scalar(out=ti[1], in0=th[1], scalar1=I2P, scalar2=None, op0=ALU.mult), reads=[th[0]], writes=[ti[0]])
        op(DVE, lambda: V.tensor_copy(t1[1], ti[1]), reads=[ti[0]], writes=[t1[0]])
        op(DVE, lambda: V.scalar_tensor_tensor(out=t1[1], in0=t1[1], scalar=-TWO_PI, in1=th[1], op0=ALU.mult, op1=ALU.add),
           reads=[t1[0], th[0]], writes=[t1[0]])
        op(ACT, lambda: S.activation(out=s_out[1], in_=t1[1], func=AF.Sin), reads=[t1[0]], writes=[s_out[0]])
        op(DVE, lambda: V.tensor_scalar(out=ti[1], in0=th[1], scalar1=I2P, scalar2=0.25, op0=ALU.mult, op1=ALU.add), reads=[th[0]], writes=[ti[0]])
        op(DVE, lambda: V.tensor_copy(t1[1], ti[1]), reads=[ti[0]], writes=[t1[0]])
        op(DVE, lambda: V.scalar_tensor_tensor(out=t1[1], in0=t1[1], scalar=-TWO_PI, in1=th[1], op0=ALU.mult, op1=ALU.add),
           reads=[t1[0], th[0]], writes=[t1[0]])
        op(ACT, lambda: S.activation(out=c_out[1], in_=t1[1], func=AF.Sin, bias=negpi), reads=[t1[0], cst], writes=[c_out[0]])

    for rd in range(4):
        pb = PS[rd % 2]
        pv = bfview(pb)
        for gg in range(8):
            g = rd * 8 + gg
            op(PE, lambda: T_.transpose(pv[:, gg * 128:(gg + 1) * 128], Zc[:, g, :], identb[:]), reads=[Zc, identb], writes=[pb], sig=(gg == 7))
        op(DVE, lambda: V.tensor_copy(U[:, rd * 8:(rd + 1) * 8, :], pv.rearrange("p (g c) -> p g c", c=128)), reads=[pb], writes=[U])

    for r in range(2):
        with ExitStack() as sd:
            Bh = fw.sb("Bh", [128, 32, 128], BF16, es=sd)
            with ExitStack() as st:
                lt = fw.sb("lt", [128, 3, 32], es=st)
                B2 = fw.sb("B2", [128, 2, 512], es=st)
                C2 = fw.sb("C2", [128, 2, 512], es=st)
                jv = fw.sb("jv", [128, 4, 8], es=st)
                mask = fw.sb("mask", [128, 128], es=st)
                dcr = fw.sb("dcr", [128, 32], es=st)
                dma(SP, lt[:], W["lamT"][L, r], writes=[lt])
                dma(SP, B2[:], W["BT2"][L, r], writes=[B2])
                dma(SP, C2[:], W["CT2"][L, r], writes=[C2])
                dma(SP, jv[:], W["jvals"][r], writes=[jv])
                dma(SP, mask[:], W["dmask"][r], writes=[mask])
                dma(SP, dcr[:], W["dcolrep"][L], writes=[dcr])
                aT = fw.sb("aT", [128, 2, 32], es=st)
                dtT = fw.sb("dtT", [128, 32], es=st)
                op(ACT, lambda: S.activation(out=dtT[:], in_=lt[:, 2, :], func=AF.Exp), reads=[lt], writes=[dtT])
                for i in range(2):
                    op(DVE, lambda: V.tensor_tensor(out=aT[:, i, :], in0=lt[:, i, :], in1=dtT[:], op=ALU.mult), reads=[lt, dtT], writes=[aT])
                mg = fw.sb("mg", [128, 32, 8], es=st)
                th = fw.sb("th", [128, 32, 8], es=st)
                tq = fw.sb("tq", [128, 32, 8], es=st)
                sn = fw.sb("sn", [128, 32, 8], es=st)
                cs = fw.sb("cs", [128, 32, 8], es=st)
                tiT = fw.sb("tiT", [128, 32, 8], mybir.dt.int32, es=st)
                Pt = [fw.sb(f"Pt{i}", [128, 2, 32, 8], es=st) for i in range(4)]

                def genT(ei):
                    jb = jv[:, ei, :].unsqueeze(1).to_broadcast([128, 32, 8])
                    ar = aT[:, 0, :].unsqueeze(2).to_broadcast([128, 32, 8])
                    ai = aT[:, 1, :].unsqueeze(2).to_broadcast([128, 32, 8])
                    op(DVE, lambda: V.tensor_tensor(out=mg[:], in0=ar, in1=jb, op=ALU.mult), reads=[aT, jv], writes=[mg])
                    op(ACT, lambda: S.activation(out=mg[:], in_=mg[:], func=AF.Exp), reads=[mg], writes=[mg])
                    op(DVE, lambda: V.tensor_tensor(out=th[:], in0=ai, in1=jb, op=ALU.mult), reads=[aT, jv], writes=[th])
                    sincos((th, th[:]), (tq, tq[:]), (sn, sn[:]), (cs, cs[:]), (tiT, tiT[:]))
                    op(DVE, lambda: V.tensor_tensor(out=Pt[ei][:, 0], in0=mg[:], in1=cs[:], op=ALU.mult), reads=[mg, cs], writes=[Pt[ei]])
                    op(DVE, lambda: V.tensor_tensor(out=Pt[ei][:, 1], in0=mg[:], in1=sn[:], op=ALU.mult), reads=[mg, sn], writes=[Pt[ei]])

                for ei in range(4):
                    genT(ei)
                op(DVE, lambda: V.tensor_tensor(out=Pt[1][:], in0=Pt[1][:], in1=Pt[0][:], op=ALU.subtract), reads=[Pt[1], Pt[0]], writes=[Pt[1]])
                inv = fw.sb("inv", [128, 4, 32], es=st)
                op(DVE, lambda: V.tensor_tensor(out=inv[:, 0, :], in0=lt[:, 0, :], in1=lt[:, 0, :], op=ALU.mult), reads=[lt], writes=[inv])
                op(DVE, lambda: V.tensor_tensor(out=inv[:, 1, :], in0=lt[:, 1, :], in1=lt[:, 1, :], op=ALU.mult), reads=[lt], writes=[inv])
                op(DVE, lambda: V.tensor_tensor(out=inv[:, 0, :], in0=inv[:, 0, :], in1=inv[:, 1, :], op=ALU.add), reads=[inv], writes=[inv])
                op(DVE, lambda: V.reciprocal(out=inv[:, 1, :], in_=inv[:, 0, :]), reads=[inv], writes=[inv])
                op(DVE, lambda: V.tensor_tensor(out=inv[:, 2, :], in0=lt[:, 0, :], in1=inv[:, 1, :], op=ALU.mult), reads=[lt, inv], writes=[inv])
                op(DVE, lambda: V.scalar_tensor_tensor(out=inv[:, 3, :], in0=lt[:, 1, :], scalar=-1.0, in1=inv[:, 1, :], op0=ALU.mult, op1=ALU.mult),
                   reads=[lt, inv], writes=[inv])
                ir = inv[:, 2, :].unsqueeze(2).to_broadcast([128, 32, 8])
                ii = inv[:, 3, :].unsqueeze(2).to_broadcast([128, 32, 8])
                op(DVE, lambda: V.tensor_tensor(out=mg[:], in0=Pt[1][:, 0], in1=ir, op=ALU.mult), reads=[Pt[1], inv], writes=[mg])
                op(DVE, lambda: V.tensor_tensor(out=th[:], in0=Pt[1][:, 1], in1=ii, op=ALU.mult), reads=[Pt[1], inv], writes=[th])
                op(DVE, lambda: V.tensor_tensor(out=Pt[0][:, 0], in0=mg[:], in1=th[:], op=ALU.subtract), reads=[mg, th], writes=[Pt[0]])
                op(DVE, lambda: V.tensor_tensor(out=mg[:], in0=Pt[1][:, 0], in1=ii, op=ALU.mult), reads=[Pt[1], inv], writes=[mg])
                op(DVE, lambda: V.tensor_tensor(out=th[:], in0=Pt[1][:, 1], in1=ir, op=ALU.mult), reads=[Pt[1], inv], writes=[th])
                op(DVE, lambda: V.tensor_tensor(out=Pt[0][:, 1], in0=mg[:], in1=th[:], op=ALU.add), reads=[mg, th], writes=[Pt[0]])

                ta = fw.sb("ta", [128, 8, 8, 16], es=st)
                tb = fw.sb("tb", [128, 8, 8, 16], es=st)
                taL, taH = fw.view("taL", ta.t), fw.view("taH", ta.t)
                tbL, tbH = fw.view("tbL", tb.t), fw.view("tbH", tb.t)
                BhT = fw.sb("BhT", [128, 32, 128], BF16, es=st)
                CDT = fw.sb("CDT", [128, 32, 128], BF16, es=st)

                def cplx_table(out, X, Y, neg):
                    oL, oH = fw.view("oL", out.t), fw.view("oH", out.t)
                    for gh in range(4):
                        gs = slice(gh * 8, (gh + 1) * 8)

                        def xb(ps_, c):
                            return X[ps_, c, gs, :].unsqueeze(3).to_broadcast([64, 8, 8, 16])

                        def yb(ps_, c):
                            return Y[ps_, c, gh * 128:(gh + 1) * 128].rearrange("p (g h) -> p g h", h=16).unsqueeze(2).to_broadcast([64, 8, 8, 16])

                        lo, hi = slice(0, 64), slice(64, 128)
                        ov = out[:].rearrange("p g (j h) -> p g j h", h=16)
                        op(DVE, lambda: V.tensor_tensor(out=ta[lo], in0=xb(lo, 0), in1=yb(lo, 0), op=ALU.mult), reads=[X, Y], writes=[taL])
                        op(DVE, lambda: V.tensor_tensor(out=tb[lo], in0=xb(lo, 1), in1=yb(lo, 1), op=ALU.mult), reads=[X, Y], writes=[tbL])
                        op(DVE, lambda: V.tensor_tensor(out=ov[lo, gs], in0=ta[lo], in1=tb[lo], op=ALU.subtract), reads=[taL, tbL], writes=[oL])
                        op(POOL, lambda: G_.tensor_tensor(out=ta[hi], in0=xb(hi, 0), in1=yb(hi, 1), op=ALU.mult), reads=[X, Y], writes=[taH])
                        op(POOL, lambda: G_.tensor_tensor(out=tb[hi], in0=xb(hi, 1), in1=yb(hi, 0), op=ALU.mult), reads=[X, Y], writes=[tbH])
                        if neg:
                            op(POOL, lambda: G_.tensor_tensor(out=ta[hi], in0=ta[hi], in1=tb[hi], op=ALU.add), reads=[taH, tbH], writes=[taH])
                            op(POOL, lambda: G_.tensor_scalar(out=ov[hi, gs], in0=ta[hi], scalar1=-1.0, scalar2=None, op0=ALU.mult),
                               reads=[taH], writes=[oH])
                        else:
                            op(POOL, lambda: G_.tensor_tensor(out=ov[hi, gs], in0=ta[hi], in1=tb[hi], op=ALU.add), reads=[taH, tbH], writes=[oH])
                    return [oL, oH]

                bh_d = cplx_table(BhT, Pt[0], B2, False)
                ch_d = cplx_table(ChT[r], Pt[2], C2, True)
                cd_d = cplx_table(CDT, Pt[3], C2, True)
                op(DVE, lambda: V.tensor_copy(ChT[r][0:1, 0, 0:1], ChT[r][0:1, 0, 0:1]), reads=ch_d, writes=[ChT[r]])
                for rd in range(4):
                    pb = PS[2 + rd % 2]
                    pv = bfview(pb)
                    for gg in range(8):
                        g = rd * 8 + gg
                        op(PE, lambda: T_.transpose(pv[:, gg * 128:(gg + 1) * 128], BhT[:, g, :], identb[:]), reads=bh_d + [identb], writes=[pb], sig=(gg == 7))
                    op(ACT, lambda: S.copy(out=Bh[:, rd * 8:(rd + 1) * 8, :], in_=pv.rearrange("p (g c) -> p g c", c=128)), reads=[pb], writes=[Bh])
                dtmp = fw.sb("dtmp", [128, 4, 128], es=st)
                dtmp2 = fw.sb("dtmp2", [128, 4, 128], es=st)
                for rd in range(8):
                    pd_ = PS[4 + rd % 2]
                    for gg in range(4):
                        g = rd * 4 + gg
                        op(PE, lambda: T_.matmul(pd_[:, gg * 128:(gg + 1) * 128], lhsT=BhT[:, g, :], rhs=CDT[:, g, :], start=True, stop=True),
                           reads=bh_d + cd_d, writes=[pd_], sig=(gg == 3))
                    mb = mask[:].unsqueeze(1).to_broadcast([128, 4, 128])
                    pdv = pd_[:].rearrange("p (g c) -> p g c", c=128)
                    if r == 0:
                        op(DVE, lambda: V.tensor_tensor(out=dtmp[:], in0=pdv, in1=mb, op=ALU.mult), reads=[pd_, mask], writes=[dtmp])
                        op(DVE, lambda: V.tensor_tensor(out=dtmp2[:], in0=identf[:].unsqueeze(1).to_broadcast([128, 4, 128]),
                                                        in1=dcr[:, rd * 4:(rd + 1) * 4].unsqueeze(2).to_broadcast([128, 4, 128]), op=ALU.mult),
                           reads=[identf, dcr], writes=[dtmp2])
                        op(DVE, lambda: V.tensor_tensor(out=Dh[r][:, rd * 4:(rd + 1) * 4, :], in0=dtmp[:], in1=dtmp2[:], op=ALU.add),
                           reads=[dtmp, dtmp2], writes=[Dh[r]])
                    else:
                        op(DVE, lambda: V.tensor_tensor(out=Dh[r][:, rd * 4:(rd + 1) * 4, :], in0=pdv, in1=mb, op=ALU.mult), reads=[pd_, mask], writes=[Dh[r]])
                fw.barrier()

            with ExitStack() as sr:
                Wt = fw.sb("Wt", [128, 32, 128], BF16, es=sr)
                Sx = fw.sb("Sx", [128, 32, 128], BF16, es=sr)
                trib = fw.sb("trib", [128, 128], BF16, es=sr)
                mselb = fw.sb("mselb", [128, 4], BF16, es=sr)
                h0b = fw.sb("h0b", [1, 4096], BF16, es=sr)
                dma(POOL, trib[:], W["tri"][r], writes=[trib])
                dma(POOL, mselb[:], W["msel"], writes=[mselb])
                dma(POOL, h0b[:], W["h0row"][L, r], writes=[h0b])
                lrow = fw.sb("lrow", [128, 3, 512], es=sr)
                Aa = fw.sb("Aa", [128, 2, 512], es=sr)
                dtb = fw.sb("dtb", [128, 512], es=sr)
                mgr = fw.sb("mgr", [128, 512], es=sr)
                thr = fw.sb("thr", [128, 512], es=sr)
                tqr = fw.sb("tqr", [128, 512], es=sr)
                snr = fw.sb("snr", [128, 512], es=sr)
                csr = fw.sb("csr", [128, 512], es=sr)
                tiR = fw.sb("tiR", [128, 512], mybir.dt.int32, es=sr)
                E = [fw.sb(f"E{i}", [128, 2, 512], es=sr) for i in range(3)]
                tar = fw.sb("tar", [128, 256], es=sr)
                tbr = fw.sb("tbr", [128, 256], es=sr)

                def genR(Eo, col, magcol, conj):
                    op(ACT, lambda: S.activation(out=mgr[:], in_=Aa[:, 0, :], func=AF.Exp, scale=magcol), reads=[Aa, cst, kcol_sb], writes=[mgr])
                    op(DVE, lambda: V.tensor_scalar(out=thr[:], in0=Aa[:, 1, :], scalar1=col, scalar2=None, op0=ALU.mult), reads=[Aa, cst, kcol_sb], writes=[thr])
                    sincos((thr, thr[:]), (tqr, tqr[:]), (snr, snr[:]), (csr, csr[:]), (tiR, tiR[:]))
                    op(DVE, lambda: V.tensor_tensor(out=Eo[:, 0, :], in0=mgr[:], in1=csr[:], op=ALU.mult), reads=[mgr, csr], writes=[Eo])
                    if conj:
                        op(DVE, lambda: V.scalar_tensor_tensor(out=Eo[:, 1, :], in0=mgr[:], scalar=-1.0, in1=snr[:], op0=ALU.mult, op1=ALU.mult),
                           reads=[mgr, snr], writes=[Eo])
                    else:
                        op(DVE, lambda: V.tensor_tensor(out=Eo[:, 1, :], in0=mgr[:], in1=snr[:], op=ALU.mult), reads=[mgr, snr], writes=[Eo])

                def cmul(outv, outbuf, srcv, srcbuf, Et, hb, parts=slice(0, 128)):
                    er = Et[parts, 0, hb * 256:(hb + 1) * 256].rearrange("c (g p) -> c g p", p=64)
                    ei = Et[parts, 1, hb * 256:(hb + 1) * 256].rearrange("c (g p) -> c g p", p=64)
                    tav = tar[parts].rearrange("c (g p) -> c g p", p=64)
                    tbv = tbr[parts].rearrange("c (g p) -> c g p", p=64)
                    op(DVE, lambda: V.tensor_tensor(out=tav, in0=srcv[:, :, 0, :], in1=er, op=ALU.mult), reads=[srcbuf, Et], writes=[tar])
                    op(DVE, lambda: V.tensor_tensor(out=tbv, in0=srcv[:, :, 1, :], in1=ei, op=ALU.mult), reads=[srcbuf, Et], writes=[tbr])
                    op(DVE, lambda: V.tensor_tensor(out=outv[:, :, 0, :], in0=tav, in1=tbv, op=ALU.subtract), reads=[tar, tbr], writes=[outbuf])
                    op(DVE, lambda: V.tensor_tensor(out=tav, in0=srcv[:, :, 0, :], in1=ei, op=ALU.mult), reads=[srcbuf, Et], writes=[tar])
                    op(DVE, lambda: V.tensor_tensor(out=tbv, in0=srcv[:, :, 1, :], in1=er, op=ALU.mult), reads=[srcbuf, Et], writes=[tbr])
                    op(DVE, lambda: V.tensor_tensor(out=outv[:, :, 1, :], in0=tav, in1=tbv, op=ALU.add), reads=[tar, tbr], writes=[outbuf])

                for rd in range(4):
                    for i in range(3):
                        dma(SP, lrow[:, i, :], W["lam_row"][L, r, i][:, rd * 512:(rd + 1) * 512].partition_broadcast(128), writes=[lrow])
                    op(ACT, lambda: S.activation(out=dtb[:], in_=lrow[:, 2, :], func=AF.Exp), reads=[lrow], writes=[dtb])
                    for i in range(2):
                        op(DVE, lambda: V.tensor_tensor(out=Aa[:, i, :], in0=lrow[:, i, :], in1=dtb[:], op=ALU.mult), reads=[lrow, dtb], writes=[Aa])
                    genR(E[0], kcol_sb[:, r:r + 1], cst[:, 2 + r:3 + r], True)
                    genR(E[1], kcol_sb[:, 2 + r:3 + r], kcol_sb[:, 2 + r:3 + r], False)
                    genR(E[2], cst[:, 1:2], cst[:, 1:2], False)
                    for hb in range(2):
                        g0 = rd * 8 + hb * 4
                        pz = PS[hb]
                        for gg in range(4):
                            op(PE, lambda: T_.matmul(pz[:, gg * 128:(gg + 1) * 128], lhsT=U[:, g0 + gg, :], rhs=Bh[:, g0 + gg, :], start=True, stop=True),
                               reads=[U, Bh], writes=[pz], sig=(gg == 3))
                        zv = pz[:].rearrange("c (g two p) -> c g two p", two=2, p=64)
                        wv = Wt[:, g0:g0 + 4, :].rearrange("c g (two p) -> c g two p", two=2)
                        cmul(wv, Wt, zv, pz, E[0], hb)
                        px = PS[2 + hb]
                        op(PE, lambda: T_.matmul(px[:], lhsT=trib[:], rhs=Wt[:, g0:g0 + 4, :].rearrange("c g p -> c (g p)"), start=True, stop=False),
                           reads=[trib, Wt], writes=[px], sig=False)
                        op(PE, lambda: T_.matmul(px[:], lhsT=onesb[0:1, :], rhs=h0b[0:1, g0 * 128:(g0 + 4) * 128], start=False, stop=True),
                           reads=[onesb, h0b], writes=[px])
                        xv = px[:].rearrange("c (g two p) -> c g two p", two=2, p=64)
                        sv = Sx[:, g0:g0 + 4, :].rearrange("c g (two p) -> c g two p", two=2)
                        cmul(sv, Sx, xv, px, E[1], hb)
                        pf = PS[4 + hb]
                        op(PE, lambda: T_.matmul(pf[0:4, :], lhsT=mselb[:], rhs=Wt[:, g0:g0 + 4, :].rearrange("c g p -> c (g p)"), start=True, stop=True),
                           reads=[mselb, Wt], writes=[pf])
                        fv = pf[0:4, :].rearrange("c (g two p) -> c g two p", two=2, p=64)
                        ov = fin[0:4, hb * 512:(hb + 1) * 512].rearrange("c (g two p) -> c g two p", two=2, p=64)
                        cmul(ov, fin, fv, pf, E[2], hb, parts=slice(0, 4))
                    dma(SP, st_out[L, r][:, rd * 1024:(rd + 1) * 1024], fin[:], reads=[fin], writes=[STB])
                    pb = PS[6 + rd % 2]
                    pv = bfview(pb)
                    for gg in range(8):
                        g = rd * 8 + gg
                        op(PE, lambda: T_.transpose(pv[:, gg * 128:(gg + 1) * 128], Sx[:, g, :], identb[:]), reads=[Sx, identb], writes=[pb], sig=(gg == 7))
                    op(ACT, lambda: S.copy(out=ST[r][:, rd * 8:(rd + 1) * 8, :], in_=pv.rearrange("p (g c) -> p g c", c=128)), reads=[pb], writes=[ST[r]])
                fw.barrier()
            fw.barrier()


    with ExitStack() as sy:
        Ycm = fw.sb("Ycm", [128, 8, 512], es=sy)
        ysT = fw.sb("ysT", [128, 4, 1024], es=sy)
        gsT = fw.sb("gsT", [128, 4, 1024], BF16, es=sy)
        q1 = fw.sb("q1", [128, 1024], es=sy)
        q2_ = fw.sb("q2", [128, 1024], es=sy)
        wgl = fw.sb("wgl", [128, 4, 512], BF16, es=sy)
        bgl = fw.sb("bgl", [128, 4], es=sy)
        dma(POOL, wgl[:], W["w_glu"][L].rearrange("(k p) n -> p k n", p=128), writes=[wgl])
        dma(SP, bgl[:], W["b_gluT"][L], writes=[bgl])
        for rd in range(8):
            py = PS[rd % 2]
            for gg in range(4):
                g = rd * 4 + gg
                sl = py[:, gg * 128:(gg + 1) * 128]
                op(PE, lambda: T_.matmul(sl, lhsT=ST[0][:, g, :], rhs=ChT[0][:, g, :], start=True, stop=False), reads=[ST[0], ChT[0]], writes=[py], sig=False)
                op(PE, lambda: T_.matmul(sl, lhsT=ST[1][:, g, :], rhs=ChT[1][:, g, :], start=False, stop=False), reads=[ST[1], ChT[1]], writes=[py], sig=False)
                op(PE, lambda: T_.matmul(sl, lhsT=U[:, g, :], rhs=Dh[0][:, g, :], start=False, stop=False), reads=[U, Dh[0]], writes=[py], sig=False)
                op(PE, lambda: T_.matmul(sl, lhsT=U[:, g, :], rhs=Dh[1][:, g, :], start=False, stop=True), reads=[U, Dh[1]], writes=[py], sig=(gg == 3))
            op(DVE, lambda: V.tensor_copy(Ycm[:].rearrange("c j (g h) -> c g j h", h=16)[:, rd * 4:(rd + 1) * 4, :, :],
                                          py[:].rearrange("c (g j h) -> c g j h", g=4, j=8)), reads=[py], writes=[Ycm])
        for j in range(8):
            pt = PS[2 + j % 2]
            for q in range(4):
                op(PE, lambda: T_.transpose(pt[:, q * 128:(q + 1) * 128], Ycm[:, j, q * 128:(q + 1) * 128], identf[:]), reads=[Ycm, identf], writes=[pt], sig=(q == 3))
            op(ACT, lambda: S.copy(out=ysT[:, :, j::8], in_=pt[:].rearrange("p (q c) -> p q c", c=128)), reads=[pt], writes=[ysT])
        for q in range(4):
            op(ACT, lambda: S.activation(out=q1[:], in_=ysT[:, q, :], func=AF.Square), reads=[ysT], writes=[q1])
            op(DVE, lambda: V.tensor_scalar(out=q1[:], in0=q1[:], scalar1=0.044715, scalar2=1.0, op0=ALU.mult, op1=ALU.add), reads=[q1], writes=[q1])
            op(DVE, lambda: V.tensor_tensor(out=q1[:], in0=q1[:], in1=ysT[:, q, :], op=ALU.mult), reads=[q1, ysT], writes=[q1])
            op(ACT, lambda: S.activation(out=q2_[:], in_=q1[:], func=AF.Sigmoid, scale=1.5957691216057308), reads=[q1], writes=[q2_])
            op(DVE, lambda: V.tensor_tensor(out=gsT[:, q, :], in0=ysT[:, q, :], in1=q2_[:], op=ALU.mult), reads=[ysT, q2_], writes=[gsT])
        for qo in range(4):
            for th in range(2):
                ts = slice(th * 512, (th + 1) * 512)
                pa = PS[4 + (qo * 2 + th) % 2]
                for k in range(4):
                    op(PE, lambda: T_.matmul(pa[:], lhsT=wgl[:, k, qo * 128:(qo + 1) * 128], rhs=gsT[:, k, ts], start=(k == 0), stop=(k == 3)),
                       reads=[wgl, gsT], writes=[pa], sig=(k == 3))
                op(ACT, lambda: S.activation(out=q1[:, 0:512], in_=pa[:], func=AF.Sigmoid, bias=bgl[:, qo:qo + 1]), reads=[pa, bgl], writes=[q1])
                op(DVE, lambda: V.tensor_tensor(out=ys2T[:, qo, ts], in0=gsT[:, qo, ts], in1=q1[:, 0:512], op=ALU.mult), reads=[gsT, q1], writes=[ys2T])
        fw.barrier()


PROMPT_SPLIT = [(0, 3), (3, 6), (6, 9), (9, 12), (12, 14), (14, 16)]


def _pos_embed():
    quarter = 256
    freqs = np.exp(np.float32(-math.log(10000.0)) * np.arange(quarter, dtype=np.float32) / np.float32(quarter)).astype(np.float32)
    r = np.repeat(np.arange(16, dtype=np.float32), 64)
    col = np.tile(np.arange(64, dtype=np.float32), 16)
    ar = (r[:, None] * freqs).astype(np.float32)
    ac = (col[:, None] * freqs).astype(np.float32)
    return np.concatenate([np.sin(ar), np.cos(ar), np.sin(ac), np.cos(ac)], axis=-1).astype(np.float32)


def _core_consts(is_sample):
    Lq = 1024 if is_sample else 256
    nseq = 1024 // Lq
    idx = np.arange(1024)
    seq = idx // Lq
    loc = idx % Lq
    ang = 2.0 * np.pi * ((loc[:, None] * loc[None, :]) % Lq) / Lq
    same = (seq[:, None] == seq[None, :])
    sc = 1.0 / math.sqrt(128.0 * Lq)
    CL = (np.cos(ang) * sc * same).astype(np.float32)
    SL = (-np.sin(ang) * sc * same).astype(np.float32)
    nloc = Lq // 8
    c = np.arange(128)
    cs, cl = c // nloc, c % nloc
    kf, kb = cl, nloc - 1 - cl
    tri = np.zeros((2, 128, 128), np.float32)
    samec = cs[:, None] == cs[None, :]
    tri[0] = (samec & (kf[:, None] < kf[None, :])).astype(np.float32)
    tri[1] = (samec & (kb[:, None] < kb[None, :])).astype(np.float32)
    kcol = np.stack([8.0 * (kf + 1), 8.0 * (kb + 1), 8.0 * kf, 8.0 * kb], axis=1).astype(np.float32)
    msel = np.zeros((128, 4), np.float32)
    msel[c, np.minimum(c // 32, 3)] = 1.0
    flag = np.full((128, 1), 1.0 if is_sample else 0.0, np.float32)
    return dict(CLt=CL, SLt=SL, tri=tri, kcol=kcol, msel=msel, flag=flag)


_NC_CACHE = {}
_DBG = {}


def kernel(**inp):
    f32 = lambda a: np.ascontiguousarray(np.asarray(a), dtype=np.float32)
    I = {k: np.asarray(v) for k, v in inp.items()}
    D_ = I["w_mod"].shape[0]
    shared = {}
    shared["w_mod"] = f32(I["w_mod"])
    shared["b_mod"] = f32(I["b_mod"][:, None, :])
    shared["n1gT"] = f32(I["norm1_g"].reshape(D_, 8, 128).transpose(0, 2, 1))
    shared["n2gT"] = f32(I["norm2_g"].reshape(D_, 8, 128).transpose(0, 2, 1))
    shared["w_in"] = f32(I["w_in"])
    shared["b_inT"] = f32(I["b_in"].reshape(D_, 40, 128).transpose(0, 2, 1))
    shared["b_zs"] = f32(I["b_in"][:, None, 1536:2048])
    shared["w_four"] = f32(I["w_four"])
    shared["cdwT"] = f32(I["conv_dw"].reshape(D_, 31, 4, 128).transpose(0, 3, 2, 1))
    shared["cvec"] = f32(np.stack([I["conv_dw_b"], I["conv_ln_g"], I["conv_ln_b"]], axis=1).reshape(D_, 3, 4, 128).transpose(0, 3, 1, 2))
    shared["w_conv_out"] = f32(I["w_conv_out"])
    ldt = np.repeat(I["ssm_log_dt"][..., None], 64, axis=-1)
    shared["lam_row"] = f32(np.stack([I["ssm_lam_re"], I["ssm_lam_im"], ldt], axis=2).reshape(D_, 2, 3, 1, 2048))
    lt = np.stack([I["ssm_lam_re"], I["ssm_lam_im"], ldt], axis=2)
    lt = lt.transpose(0, 1, 4, 2, 3)
    shared["lamT"] = f32(np.concatenate([lt, lt], axis=2))
    b2 = np.stack([I["ssm_b_re"], I["ssm_b_im"]], axis=2)
    b2 = b2.transpose(0, 1, 4, 2, 3, 5).reshape(D_, 2, 64, 2, 512)
    shared["BT2"] = f32(np.concatenate([b2, b2], axis=2))
    c2 = np.stack([I["ssm_c_re"], I["ssm_c_im"]], axis=2)
    c2 = c2.transpose(0, 1, 5, 2, 3, 4).reshape(D_, 2, 64, 2, 512)
    shared["CT2"] = f32(np.concatenate([c2, c2], axis=2))
    dd = I["ssm_d"].reshape(D_, 32, 16)
    shared["dcolrep"] = f32(np.broadcast_to(dd.transpose(0, 2, 1)[:, None, :, :], (D_, 8, 16, 32)).reshape(D_, 128, 32))
    shared["w_glu"] = f32(I["w_ssm_glu"])
    shared["b_gluT"] = f32(I["b_ssm_glu"].reshape(D_, 4, 128).transpose(0, 2, 1))
    shared["w_ssm_out"] = f32(I["w_ssm_out"])
    shared["w_out"] = f32(I["w_out"])
    shared["router_w"] = f32(I["router_w"])
    shared["router_b"] = f32(I["router_b"][:, None, :])
    shared["w_gu"] = f32(I["w_gate_up"])
    shared["b_guT"] = f32(I["b_gate_up"].reshape(D_, NEXP, 16, 128).transpose(0, 3, 1, 2).reshape(D_, 128, NEXP * 16))
    shared["w_dn"] = f32(I["w_down"])
    shared["b_dn"] = f32(I["b_down"])
    shared["fin_g"] = f32(I["final_norm_g"][None, :])
    cc = np.arange(128)
    angc = 2.0 * np.pi * ((cc[:, None] * cc[None, :]) % 128) / 128.0
    shared["ccsc"] = f32(np.concatenate([np.cos(angc), np.sin(angc)], axis=1))
    j = np.arange(8, dtype=np.float32)
    jv = np.zeros((2, 128, 4, 8), np.float32)
    jv[0, :, 0], jv[0, :, 1], jv[0, :, 2], jv[0, :, 3] = 7 - j, 8 - j, j + 1, j - 7
    jv[1, :, 0], jv[1, :, 1], jv[1, :, 2], jv[1, :, 3] = j, j + 1, 8 - j, -j
    shared["jvals"] = jv
    jj = np.arange(128) // 16
    dm = np.zeros((2, 128, 128), np.float32)
    dm[0] = (jj[:, None] <= jj[None, :])
    dm[1] = (jj[:, None] >= jj[None, :])
    shared["dmask"] = dm

    cs_s, cs_p = _core_consts(True), _core_consts(False)
    pos = _pos_embed()
    zeros_pos = np.zeros((1024, 1024), np.float32)
    zeros_h0 = np.zeros((D_, 2, 1, 4096), np.float32)
    in_maps = []
    for core in range(8):
        m = dict(shared)
        if core < 2:
            b = core
            m.update(cs_s)
            m["xin"] = f32(I["x_sample"][b])
            m["pos"] = pos
            cond = I["c"][b]
            h0 = np.stack([I["state_ssm_re"][b], I["state_ssm_im"][b]], axis=3)
            m["h0row"] = f32(h0.reshape(D_, 2, 1, 4096))
        else:
            lo, hi = PROMPT_SPLIT[core - 2]
            m.update(cs_p)
            xi = np.zeros((4, 256, 1024), np.float32)
            xi[:hi - lo] = I["x_prompt"][lo:hi]
            m["xin"] = xi.reshape(1024, 1024)
            m["pos"] = zeros_pos
            cond = I["c_ctx"]
            m["h0row"] = zeros_h0
        m["condT"] = f32(np.asarray(cond).reshape(8, 128).T)
        in_maps.append(m)

    if "nc" not in _NC_CACHE:
        _NC_CACHE["nc"] = build_nc(depth=_DBG.get("depth", DEPTH), dbg=_DBG.get("dbg", False), skip=_DBG.get("skip", ()))
    nc = _NC_CACHE["nc"]
    res = run_bass_kernel_spmd(nc, in_maps, core_ids=list(range(8)))
    R = res.results
    _DBG["R"] = R if _DBG.get("dbg", False) else None
    y_prompt = np.zeros((16, 256, 1024), np.float32)
    y_sample = np.zeros((2, 1024, 1024), np.float32)
    ns_re = np.zeros((16, D_, 2, 32, 64), np.float32)
    ns_im = np.zeros((16, D_, 2, 32, 64), np.float32)
    for core in range(8):
        yo = np.asarray(R[core]["y_out"], dtype=np.float32)
        if core < 2:
            y_sample[core] = yo
        else:
            lo, hi = PROMPT_SPLIT[core - 2]
            so = np.asarray(R[core]["st_out"], dtype=np.float32).reshape(D_, 2, 4, 32, 2, 64)
            for s in range(hi - lo):
                y_prompt[lo + s] = yo[s * 256:(s + 1) * 256]
                ns_re[lo + s] = so[:, :, s, :, 0, :]
                ns_im[lo + s] = so[:, :, s, :, 1, :]
    return (y_prompt, y_sample, ns_re, ns_im)
```

```python
import math
from contextlib import ExitStack
import numpy as np
import concourse.bass as bass
import concourse.mybir as mybir
from concourse.bass_utils import run_bass_kernel_spmd

F32 = mybir.dt.float32
BF16 = mybir.dt.bfloat16
AF = mybir.ActivationFunctionType
ALU = mybir.AluOpType
AX = mybir.AxisListType

PE, DVE, ACT, POOL, SP = 0, 1, 2, 3, 4
NDMA_SEMS = 24
DEPTH = 4
NEXP = 32
NEXP_RUN = [32]
EPS = 1e-6
TWO_PI = 2.0 * math.pi
ANG_OFF = 64.0 * math.pi


class Buf:
    __slots__ = ("name", "t", "w", "r")

    def __init__(self, name, t):
        self.name = name
        self.t = t
        self.w = None
        self.r = []

    def __getitem__(self, k):
        return self.t[k]


class FW:
    def __init__(self, nc, es):
        self.nc = nc
        self.es = es
        self.engs = [nc.tensor, nc.vector, nc.scalar, nc.gpsimd, nc.sync]
        self.esem = [es.enter_context(nc.semaphore(f"es{i}")) for i in range(5)]
        self.ecnt = [0] * 5
        self.dsem = [es.enter_context(nc.semaphore(f"ds{i}")) for i in range(NDMA_SEMS)]
        self.dcnt = [0] * NDMA_SEMS
        self.dnext = 0
        self.dnext_sw = 0
        self.waited = [dict() for _ in range(5)]
        self.uid = 0
        self.mute = False

    def sb(self, name, shape, dt=F32, es=None):
        self.uid += 1
        t = (es or self.es).enter_context(self.nc.sbuf_tensor(f"{name}_{self.uid}", list(shape), dt))
        esz = 2 if dt == BF16 else 4
        nbytes = esz * int(np.prod(shape[1:]))
        pad = (-nbytes) % 64
        if pad:
            self.uid += 1
            (es or self.es).enter_context(self.nc.sbuf_tensor(f"pad_{self.uid}", [128, pad // 2], BF16))
        return Buf(name, t)

    def ps(self, name, shape, dt=F32, es=None):
        self.uid += 1
        t = (es or self.es).enter_context(self.nc.psum_tensor(f"{name}_{self.uid}", list(shape), dt))
        return Buf(name, t)

    def view(self, name, t):
        return Buf(name, t)

    def _wait(self, e, dep):
        if dep is None:
            return
        kind, idx, cnt = dep
        key = (kind, idx)
        if kind == "e" and idx == PE and e == PE:
            return
        if self.waited[e].get(key, 0) >= cnt:
            return
        sem = self.esem[idx] if kind == "e" else self.dsem[idx]
        self.engs[e].wait_ge(sem, cnt)
        self.waited[e][key] = cnt

    def _deps(self, e, reads, writes):
        for b in reads:
            self._wait(e, b.w)
        for b in writes:
            self._wait(e, b.w)
            for d in b.r:
                self._wait(e, d)

    def _mark(self, dep, reads, writes):
        for b in reads:
            b.r.append(dep)
            if len(b.r) > 16:
                m = {}
                for k, i, c in b.r:
                    m[(k, i)] = max(m.get((k, i), 0), c)
                b.r = [(k, i, c) for (k, i), c in m.items()]
        for b in writes:
            b.w = dep
            b.r = []

    def op(self, e, fn, reads=(), writes=(), sig=True):
        if self.mute:
            return None
        self._deps(e, reads, writes)
        ins = fn()
        if sig:
            self.ecnt[e] += 1
            ins.then_inc(self.esem[e], 1)
            dep = ("e", e, self.ecnt[e])
        else:
            dep = ("e", e, self.ecnt[e] + 1)
        self._mark(dep, reads, writes)
        return ins

    def dma(self, e, out, in_, reads=(), writes=(), **kw):
        if self.mute:
            return None
        self._deps(e, reads, writes)
        half = NDMA_SEMS // 2
        if e == POOL:
            k = half + self.dnext_sw
            self.dnext_sw = (self.dnext_sw + 1) % half
        else:
            k = self.dnext
            self.dnext = (self.dnext + 1) % half
        if self.dcnt[k] > 0:
            self._wait(e, ("d", k, self.dcnt[k]))
        ins = self.engs[e].dma_start(out=out, in_=in_, **kw)
        self.dcnt[k] += 16
        ins.then_inc(self.dsem[k], 16)
        dep = ("d", k, self.dcnt[k])
        self._mark(dep, reads, writes)
        return dep

    def barrier(self):
        for e in range(5):
            for i in range(5):
                if self.ecnt[i] > 0:
                    self._wait(e, ("e", i, self.ecnt[i]))
            for k in range(NDMA_SEMS):
                if self.dcnt[k] > 0:
                    self._wait(e, ("d", k, self.dcnt[k]))


def build_nc(depth=DEPTH, dbg=False, skip=()):
    nc = bass.Bass("TRN2", target_bir_lowering=False)
    V, S, G_, T_ = nc.vector, nc.scalar, nc.gpsimd, nc.tensor

    def din(name, shape, dt=F32):
        return nc.dram_tensor(name, list(shape), dt, kind="ExternalInput").ap()

    def dout(name, shape, dt=F32):
        return nc.dram_tensor(name, list(shape), dt, kind="ExternalOutput").ap()

    xin = din("xin", [1024, 1024])
    pos = din("pos", [1024, 1024])
    condT = din("condT", [128, 8])
    h0row = din("h0row", [depth, 2, 1, 4096])
    CLt = din("CLt", [1024, 1024])
    SLt = din("SLt", [1024, 1024])
    ccsc = din("ccsc", [128, 256])
    tri = din("tri", [2, 128, 128])
    kcol = din("kcol", [128, 4])
    msel = din("msel", [128, 4])
    flag = din("flag", [128, 1])
    jvals = din("jvals", [2, 128, 4, 8])
    dmask = din("dmask", [2, 128, 128])
    w_mod = din("w_mod", [depth, 1024, 6144])
    b_mod = din("b_mod", [depth, 1, 6144])
    n1gT = din("n1gT", [depth, 128, 8])
    n2gT = din("n2gT", [depth, 128, 8])
    w_in = din("w_in", [depth, 1024, 5120])
    b_inT = din("b_inT", [depth, 128, 40])
    b_zs = din("b_zs", [depth, 1, 512])
    w_four = din("w_four", [depth, 512, 1024])
    cdwT = din("cdwT", [depth, 128, 4, 31])
    cvec = din("cvec", [depth, 128, 3, 4])
    w_conv_out = din("w_conv_out", [depth, 512, 1024])
    lam_row = din("lam_row", [depth, 2, 3, 1, 2048])
    lamT = din("lamT", [depth, 2, 128, 3, 32])
    BT2 = din("BT2", [depth, 2, 128, 2, 512])
    CT2 = din("CT2", [depth, 2, 128, 2, 512])
    dcolrep = din("dcolrep", [depth, 128, 32])
    w_glu = din("w_glu", [depth, 512, 512])
    b_gluT = din("b_gluT", [depth, 128, 4])
    w_ssm_out = din("w_ssm_out", [depth, 512, 1024])
    w_out = din("w_out", [depth, 1024, 1024])
    router_w = din("router_w", [depth, 1024, 32])
    router_b = din("router_b", [depth, 1, 32])
    w_gu = din("w_gu", [depth, NEXP_RUN[0], 1024, 2048])
    b_guT = din("b_guT", [depth, 128, NEXP * 16])
    w_dn = din("w_dn", [depth, NEXP_RUN[0], 1024, 1024])
    b_dn = din("b_dn", [depth, NEXP, 1024])
    fin_g = din("fin_g", [1, 1024])
    y_out = dout("y_out", [1024, 1024])
    st_out = dout("st_out", [depth, 2, 4, 4096])
    if dbg:
        dbg_x = dout("dbg_x", [1024, 1024])
        dbg_a = dout("dbg_a", [128, 8192])

    with ExitStack() as es:
        fw = FW(nc, es)
        fw.skipset = set(skip)
        op, dma = fw.op, fw.dma

        x = fw.sb("x", [128, 8, 1024])
        hT = fw.sb("hT", [128, 8, 1024], BF16)
        identf = fw.sb("identf", [128, 128])
        identb = fw.sb("identb", [128, 128], BF16)
        onesf = fw.sb("onesf", [128, 128])
        onesb = fw.sb("onesb", [128, 128], BF16)
        g1b = fw.sb("g1b", [128, 1024])
        g2b = fw.sb("g2b", [128, 1024])
        modcol = fw.sb("modcol", [128, 48])
        gscol = fw.sb("gscol", [128, 16])
        scol = fw.sb("scol", [128, 8], BF16)
        small = fw.sb("small", [128, 64])
        flag_sb = fw.sb("flag_sb", [128, 1])
        kcol_sb = fw.sb("kcol_sb", [128, 4])
        PS = [fw.ps(f"ps{i}", [128, 512]) for i in range(8)]
        OUTB = fw.view("y_out", y_out)
        STB = fw.view("st_out", st_out)

        op(POOL, lambda: G_.memset(identf[:], 1.0), writes=[identf])
        op(POOL, lambda: G_.affine_select(identf[:], identf[:], pattern=[[-1, 128]], compare_op=ALU.is_equal,
                                           fill=0.0, base=0, channel_multiplier=1), reads=[identf], writes=[identf])
        op(DVE, lambda: V.tensor_copy(identb[:], identf[:]), reads=[identf], writes=[identb])
        op(POOL, lambda: G_.memset(onesf[:], 1.0), writes=[onesf])
        op(POOL, lambda: G_.memset(onesb[:], 1.0), writes=[onesb])
        dma(SP, flag_sb[:], flag, writes=[flag_sb])
        dma(SP, kcol_sb[:], kcol, writes=[kcol_sb])

        with ExitStack() as sub:
            ptmp = fw.sb("ptmp", [128, 8, 1024], es=sub)
            for tt in range(8):
                dma(SP, x[:, tt, :], xin[tt * 128:(tt + 1) * 128, :], writes=[x])
                dma(ACT, ptmp[:, tt, :], pos[tt * 128:(tt + 1) * 128, :], writes=[ptmp])
            op(DVE, lambda: V.tensor_tensor(out=x[:], in0=x[:], in1=ptmp[:], op=ALU.add), reads=[x, ptmp], writes=[x])
            ctmp = fw.sb("ctmp", [128, 8], es=sub)
            csig = fw.sb("csig", [128, 8], es=sub)
            dma(SP, ctmp[:], condT, writes=[ctmp])
            op(ACT, lambda: S.activation(out=csig[:], in_=ctmp[:], func=AF.Sigmoid), reads=[ctmp], writes=[csig])
            op(DVE, lambda: V.tensor_tensor(out=scol[:], in0=ctmp[:], in1=csig[:], op=ALU.mult), reads=[ctmp, csig], writes=[scol])
            fw.barrier()

        def norm_transpose(sub, gs_off, sh_off, router=None):
            xn = fw.sb("xn", [128, 1024], es=sub)
            junk = fw.sb("junk", [128, 1024], es=sub)
            ss = fw.sb("ss", [128, 8], es=sub)
            rstd = fw.sb("rstd", [128, 8], es=sub)
            hf = fw.sb("hf", [128, 8, 128], es=sub) if router is not None else None
            op(DVE, lambda: V.memset(ss[:], 0.0), writes=[ss])
            for tt in range(8):
                op(ACT, lambda: S.activation(out=junk[:], in_=x[:, tt, :], func=AF.Square, accum_out=ss[:, tt:tt + 1]),
                   reads=[x], writes=[junk, ss])
                op(DVE, lambda: V.tensor_scalar(out=rstd[:, tt:tt + 1], in0=ss[:, tt:tt + 1], scalar1=1.0 / 1024.0, scalar2=EPS,
                                                op0=ALU.mult, op1=ALU.add), reads=[ss], writes=[rstd])
                op(ACT, lambda: S.sqrt(out=rstd[:, tt:tt + 1], in_=rstd[:, tt:tt + 1]), reads=[rstd], writes=[rstd])
                op(DVE, lambda: V.reciprocal(out=rstd[:, tt:tt + 1], in_=rstd[:, tt:tt + 1]), reads=[rstd], writes=[rstd])
                op(ACT, lambda: S.activation(out=xn[:], in_=x[:, tt, :], func=AF.Copy, scale=rstd[:, tt:tt + 1]),
                   reads=[x, rstd], writes=[xn])
                for half in range(2):
                    pa = PS[half]
                    for kk in range(4):
                        k = half * 4 + kk
                        op(PE, lambda: T_.transpose(pa[:, kk * 128:(kk + 1) * 128], xn[:, k * 128:(k + 1) * 128], identf[:]),
                           reads=[xn, identf], writes=[pa], sig=(kk == 3))
                    for kk in range(4):
                        k = half * 4 + kk
                        op(ACT, lambda: S.activation(out=hT[:, k, tt * 128:(tt + 1) * 128], in_=pa[:, kk * 128:(kk + 1) * 128],
                                                     func=AF.Identity, scale=gscol[:, gs_off + k:gs_off + k + 1],
                                                     bias=modcol[:, sh_off + k:sh_off + k + 1]),
                           reads=[pa, gscol, modcol], writes=[hT])
                        if router is not None:
                            op(ACT, lambda: S.activation(out=hf[:, k, :], in_=pa[:, kk * 128:(kk + 1) * 128],
                                                         func=AF.Identity, scale=gscol[:, gs_off + k:gs_off + k + 1],
                                                         bias=modcol[:, sh_off + k:sh_off + k + 1]),
                               reads=[pa, gscol, modcol], writes=[hf])
                if router is not None:
                    router(tt, hf)

        for L in range(depth):
            with ExitStack() as sub:
                modrow = fw.sb("modrow", [1, 6144], es=sub)
                bmrow = fw.sb("bmrow", [1, 6144], es=sub)
                ngT = fw.sb("ngT", [128, 16], es=sub)
                wm = [fw.sb(f"wm{i}", [128, 8, 512], BF16, es=sub) for i in range(2)]
                dma(SP, bmrow[:], b_mod[L], writes=[bmrow])
                dma(SP, ngT[:, 0:8], n1gT[L], writes=[ngT])
                dma(SP, ngT[:, 8:16], n2gT[L], writes=[ngT])
                for n in range(12):
                    wb = wm[n % 2]
                    dma(POOL, wb[:], w_mod[L][:, n * 512:(n + 1) * 512].rearrange("(k p) n -> p k n", p=128), writes=[wb])
                    pa = PS[n % 2]
                    for k in range(8):
                        op(PE, lambda: T_.matmul(pa[0:1, :], lhsT=scol[:, k:k + 1], rhs=wb[:, k, :], start=(k == 0), stop=(k == 7)),
                           reads=[scol, wb], writes=[pa], sig=(k == 7))
                    op(DVE, lambda: V.tensor_tensor(out=modrow[0:1, n * 512:(n + 1) * 512], in0=pa[0:1, :],
                                                    in1=bmrow[0:1, n * 512:(n + 1) * 512], op=ALU.add),
                       reads=[pa, bmrow], writes=[modrow])
                pa = PS[2]
                for j in range(48):
                    op(PE, lambda: T_.matmul(pa[:, j:j + 1], lhsT=modrow[0:1, j * 128:(j + 1) * 128], rhs=onesf[0:1, 0:1],
                                             start=True, stop=True), reads=[modrow, onesf], writes=[pa], sig=(j == 47))
                op(DVE, lambda: V.tensor_copy(modcol[:], pa[:, 0:48]), reads=[pa], writes=[modcol])
                for i, m in enumerate((1, 4)):
                    op(DVE, lambda: V.scalar_tensor_tensor(out=gscol[:, i * 8:(i + 1) * 8], in0=modcol[:, m * 8:(m + 1) * 8], scalar=1.0,
                                                           in1=ngT[:, i * 8:(i + 1) * 8], op0=ALU.add, op1=ALU.mult),
                       reads=[modcol, ngT], writes=[gscol])
                for gb, m in ((g1b, 2), (g2b, 5)):
                    for hh in range(2):
                        pb = PS[3 + hh]
                        op(PE, lambda: T_.matmul(pb[:], lhsT=onesf[0:1, :], rhs=modrow[0:1, m * 1024 + hh * 512: m * 1024 + (hh + 1) * 512],
                                                 start=True, stop=True), reads=[onesf, modrow], writes=[pb])
                        op(DVE, lambda: V.tensor_copy(gb[:, hh * 512:(hh + 1) * 512], pb[:]), reads=[pb], writes=[gb])
                fw.barrier()

            with ExitStack() as sub:
                norm_transpose(sub, 0, 0)
                fw.barrier()

            with ExitStack() as mix:
                ys2T = fw.sb("ys2T", [128, 4, 1024], BF16, es=mix)
                binT = fw.sb("binT", [128, 40], es=mix)
                dma(SP, binT[:], b_inT[L], writes=[binT])

                fw.mute = "s5" in skip
                with ExitStack() as sub:
                    Zc = fw.sb("Zc", [128, 32, 128], BF16, es=sub)
                    with ExitStack() as sz:
                        ws = fw.sb("ws", [128, 8, 512], BF16, es=sz)
                        bzs = fw.sb("bzs", [128, 512], es=sz)
                        dma(SP, bzs[:], b_zs[L].partition_broadcast(128), writes=[bzs])
                        dma(POOL, ws[:], w_in[L][:, 1536:2048].rearrange("(k p) n -> p k n", p=128), writes=[ws])
                        for j in range(8):
                            pa = PS[4 + j % 2]
                            for k in range(8):
                                op(PE, lambda: T_.matmul(pa[:], lhsT=hT[:, k, j::8], rhs=ws[:, k, :], start=(k == 0), stop=(k == 7)),
                                   reads=[ws, hT], writes=[pa], sig=(k == 7))
                            op(DVE, lambda: V.tensor_tensor(out=Zc[:].rearrange("c g (j h) -> c g j h", h=16)[:, :, j, :],
                                                            in0=pa[:].rearrange("c (g h) -> c g h", h=16),
                                                            in1=bzs[:].rearrange("c (g h) -> c g h", h=16), op=ALU.add),
                               reads=[pa, bzs], writes=[Zc])
                        fw.barrier()
                    s5_block(fw, nc, sub, L, PS, Zc, ys2T, identb, identf, onesb, kcol_sb,
                             dict(lam_row=lam_row, lamT=lamT, BT2=BT2, CT2=CT2, dcolrep=dcolrep, jvals=jvals, dmask=dmask,
                                  tri=tri, msel=msel, h0row=h0row, w_glu=w_glu, b_gluT=b_gluT), STB, st_out)
                    fw.barrier()

                fw.mute = "stageA" in skip
                mixT = fw.sb("mixT", [128, 4, 1024], BF16, es=mix)
                convT = fw.sb("convT", [128, 4, 1024], BF16, es=mix)

                with ExitStack() as sub:
                    wi = [fw.sb(f"wi{i}", [128, 8, 512], BF16, es=sub) for i in range(2)]
                    zfT = fw.sb("zfT", [128, 4, 1024], BF16, es=sub)
                    vpad = fw.sb("vpad", [128, 4, 4, 286], es=sub)
                    sig = fw.sb("sig", [128, 512], es=sub)
                    op(POOL, lambda: G_.memset(vpad[:], 0.0), writes=[vpad])

                    def load_wi(piece):
                        wb = wi[piece % 2]
                        dma(POOL, wb[:], w_in[L][:, piece * 512:(piece + 1) * 512].rearrange("(k p) n -> p k n", p=128), writes=[wb])
                        return wb

                    wb = load_wi(0)
                    for cc in range(4):
                        for th in range(2):
                            pa = PS[(cc * 2 + th) % 2]
                            for k in range(8):
                                op(PE, lambda: T_.matmul(pa[:], lhsT=wb[:, k, cc * 128:(cc + 1) * 128], rhs=hT[:, k, th * 512:(th + 1) * 512],
                                                         start=(k == 0), stop=(k == 7)), reads=[wb, hT], writes=[pa], sig=(k == 7))
                            op(ACT, lambda: S.activation(out=zfT[:, cc, th * 512:(th + 1) * 512], in_=pa[:], func=AF.Identity,
                                                         bias=binT[:, cc:cc + 1]), reads=[pa, binT], writes=[zfT])
                    wa = load_wi(1)
                    wbb = load_wi(2)
                    for q in range(4):
                        for th in range(2):
                            pa, pb = PS[2], PS[3]
                            for k in range(8):
                                op(PE, lambda: T_.matmul(pa[:], lhsT=wa[:, k, q * 128:(q + 1) * 128], rhs=hT[:, k, th * 512:(th + 1) * 512],
                                                         start=(k == 0), stop=(k == 7)), reads=[wa, hT], writes=[pa], sig=(k == 7))
                            for k in range(8):
                                op(PE, lambda: T_.matmul(pb[:], lhsT=wbb[:, k, q * 128:(q + 1) * 128], rhs=hT[:, k, th * 512:(th + 1) * 512],
                                                         start=(k == 0), stop=(k == 7)), reads=[wbb, hT], writes=[pb], sig=(k == 7))
                            op(ACT, lambda: S.activation(out=sig[:], in_=pb[:], func=AF.Sigmoid, bias=binT[:, 8 + q:9 + q]),
                               reads=[pb, binT], writes=[sig])
                            op(DVE, lambda: V.scalar_tensor_tensor(
                                out=vpad[:, q, th * 2:(th + 1) * 2, 15:271], in0=pa[:].rearrange("p (s t) -> p s t", s=2),
                                scalar=binT[:, 4 + q:5 + q], in1=sig[:].rearrange("p (s t) -> p s t", s=2), op0=ALU.add, op1=ALU.mult),
                               reads=[pa, sig, binT], writes=[vpad])

                    with ExitStack() as sf:
                        CL = fw.sb("CL", [128, 8, 1024], BF16, es=sf)
                        SL = fw.sb("SL", [128, 8, 1024], BF16, es=sf)
                        ccs = fw.sb("ccs", [128, 256], BF16, es=sf)
                        Ucs = fw.sb("Ucs", [128, 8, 1024], BF16, es=sf)
                        dma(POOL, ccs[:], ccsc, writes=[ccs])
                        for h2 in range(2):
                            dma(POOL, CL[:, h2 * 4:(h2 + 1) * 4, :], CLt[h2 * 512:(h2 + 1) * 512, :].rearrange("(k p) n -> p k n", p=128), writes=[CL])
                            dma(POOL, SL[:, h2 * 4:(h2 + 1) * 4, :], SLt[h2 * 512:(h2 + 1) * 512, :].rearrange("(k p) n -> p k n", p=128), writes=[SL])
                        for tt in range(8):
                            for gh in range(2):
                                pa = PS[gh]
                                for gg in range(2):
                                    g = gh * 2 + gg
                                    op(PE, lambda: T_.matmul(pa[:, gg * 256:(gg + 1) * 256], lhsT=zfT[:, g, tt * 128:(tt + 1) * 128], rhs=ccs[:],
                                                             start=True, stop=True), reads=[zfT, ccs], writes=[pa], sig=(gg == 1))
                                op(ACT if gh else DVE,
                                   (lambda: S.copy(out=Ucs[:, tt, gh * 512:(gh + 1) * 512], in_=pa[:])) if gh else
                                   (lambda: V.tensor_copy(Ucs[:, tt, gh * 512:(gh + 1) * 512], pa[:])),
                                   reads=[pa], writes=[Ucs])
                        for g in range(4):
                            for th in range(2):
                                pa = PS[2 + (g * 2 + th) % 2]
                                for lc in range(8):
                                    op(PE, lambda: T_.matmul(pa[:], lhsT=Ucs[:, lc, g * 256:g * 256 + 128], rhs=CL[:, lc, th * 512:(th + 1) * 512],
                                                             start=(lc == 0), stop=False), reads=[Ucs, CL], writes=[pa], sig=False)
                                    op(PE, lambda: T_.matmul(pa[:], lhsT=Ucs[:, lc, g * 256 + 128:g * 256 + 256], rhs=SL[:, lc, th * 512:(th + 1) * 512],
                                                             start=False, stop=(lc == 7)), reads=[Ucs, SL], writes=[pa], sig=(lc == 7))
                                op(ACT, lambda: S.copy(out=mixT[:, g, th * 512:(th + 1) * 512], in_=pa[:]), reads=[pa], writes=[mixT])
                        fw.barrier()

                    with ExitStack() as sc:
                        cw = fw.sb("cw", [128, 4, 31], es=sc)
                        cv = fw.sb("cv", [128, 3, 4], es=sc)
                        yc = fw.sb("yc", [128, 4, 1024], es=sc)
                        ysq = fw.sb("ysq", [128, 1024], es=sc)
                        mean = fw.sb("mean", [128, 1024], es=sc)
                        rs = fw.sb("rs", [128, 1024], es=sc)
                        t1 = fw.sb("t1", [128, 1024], es=sc)
                        t2 = fw.sb("t2", [128, 1024], es=sc)
                        dma(SP, cw[:], cdwT[L], writes=[cw])
                        dma(SP, cv[:], cvec[L], writes=[cv])
                        for q in range(4):
                            op(DVE, lambda: V.tensor_scalar(out=vpad[:, q, 1:4, 0:15], in0=vpad[:, q, 0:3, 256:271], scalar1=flag_sb[:, 0:1],
                                                            scalar2=None, op0=ALU.mult), reads=[vpad, flag_sb], writes=[vpad])
                            op(DVE, lambda: V.tensor_scalar(out=vpad[:, q, 0:3, 271:286], in0=vpad[:, q, 1:4, 15:30], scalar1=flag_sb[:, 0:1],
                                                            scalar2=None, op0=ALU.mult), reads=[vpad, flag_sb], writes=[vpad])
                            yv = yc[:, q, :].rearrange("p (s t) -> p s t", s=4)
                            op(DVE, lambda: V.tensor_scalar(out=yv, in0=vpad[:, q, :, 0:256], scalar1=cw[:, q, 0:1], scalar2=cv[:, 0, q:q + 1],
                                                            op0=ALU.mult, op1=ALU.add), reads=[vpad, cw, cv], writes=[yc])
                            for k in range(1, 31):
                                op(DVE, lambda: V.scalar_tensor_tensor(out=yv, in0=vpad[:, q, :, k:k + 256], scalar=cw[:, q, k:k + 1], in1=yv,
                                                                       op0=ALU.mult, op1=ALU.add), reads=[vpad, cw, yc], writes=[yc])
                        for th in range(2):
                            pa, pb = PS[th], PS[2 + th]
                            for q in range(4):
                                op(ACT, lambda: S.activation(out=ysq[:, 0:512], in_=yc[:, q, th * 512:(th + 1) * 512], func=AF.Square),
                                   reads=[yc], writes=[ysq])
                                op(PE, lambda: T_.matmul(pa[:], lhsT=onesf[:], rhs=yc[:, q, th * 512:(th + 1) * 512], start=(q == 0), stop=(q == 3)),
                                   reads=[onesf, yc], writes=[pa], sig=True)
                                op(PE, lambda: T_.matmul(pb[:], lhsT=onesf[:], rhs=ysq[:, 0:512], start=(q == 0), stop=(q == 3)),
                                   reads=[onesf, ysq], writes=[pb], sig=True)
                            sl = slice(th * 512, (th + 1) * 512)
                            op(DVE, lambda: V.tensor_scalar(out=mean[:, sl], in0=pa[:], scalar1=1.0 / 512.0, scalar2=None, op0=ALU.mult),
                               reads=[pa], writes=[mean])
                            op(DVE, lambda: V.tensor_tensor(out=t1[:, sl], in0=mean[:, sl], in1=mean[:, sl], op=ALU.mult), reads=[mean], writes=[t1])
                            op(DVE, lambda: V.scalar_tensor_tensor(out=rs[:, sl], in0=pb[:], scalar=1.0 / 512.0, in1=t1[:, sl],
                                                                   op0=ALU.mult, op1=ALU.subtract), reads=[pb, t1], writes=[rs])
                            op(DVE, lambda: V.tensor_scalar(out=rs[:, sl], in0=rs[:, sl], scalar1=EPS, scalar2=None, op0=ALU.add),
                               reads=[rs], writes=[rs])
                            op(ACT, lambda: S.sqrt(out=rs[:, sl], in_=rs[:, sl]), reads=[rs], writes=[rs])
                            op(DVE, lambda: V.reciprocal(out=rs[:, sl], in_=rs[:, sl]), reads=[rs], writes=[rs])
                        for q in range(4):
                            op(DVE, lambda: V.tensor_tensor(out=t1[:], in0=yc[:, q, :], in1=mean[:], op=ALU.subtract), reads=[yc, mean], writes=[t1])
                            op(DVE, lambda: V.tensor_tensor(out=t1[:], in0=t1[:], in1=rs[:], op=ALU.mult), reads=[t1, rs], writes=[t1])
                            op(DVE, lambda: V.tensor_scalar(out=t1[:], in0=t1[:], scalar1=cv[:, 1, q:q + 1], scalar2=cv[:, 2, q:q + 1],
                                                            op0=ALU.mult, op1=ALU.add), reads=[t1, cv], writes=[t1])
                            op(ACT, lambda: S.activation(out=t2[:], in_=t1[:], func=AF.Sigmoid), reads=[t1], writes=[t2])
                            op(DVE, lambda: V.tensor_tensor(out=convT[:, q, :], in0=t1[:], in1=t2[:], op=ALU.mult), reads=[t1, t2], writes=[convT])
                        fw.barrier()
                    fw.barrier()

                fw.mute = "stageC" in skip
                with ExitStack() as sub:
                    wbr = [fw.sb(f"wbr{i}", [128, 4, 1024], BF16, es=sub) for i in range(3)]
                    wg = [fw.sb(f"wg{i}", [128, 8, 3, 128], BF16, es=sub) for i in range(2)]
                    wo = fw.sb("wo", [128, 8, 1024], BF16, es=sub)
                    mixedT = fw.sb("mixedT", [128, 8, 1024], BF16, es=sub)
                    sg = [fw.sb(f"sg{i}", [128, 512], es=sub) for i in range(3)]
                    acc = fw.sb("accm", [128, 512], es=sub)
                    tmp = fw.sb("tmpm", [128, 512], es=sub)
                    for i, wsrc in enumerate((w_four, w_conv_out, w_ssm_out)):
                        dma(POOL, wbr[i][:], wsrc[L].rearrange("(k p) n -> p k n", p=128), writes=[wbr[i]])
                    for h2 in range(2):
                        dma(POOL, wo[:, h2 * 4:(h2 + 1) * 4, :], w_out[L][h2 * 512:(h2 + 1) * 512, :].rearrange("(k p) n -> p k n", p=128), writes=[wo])
                    brT = (mixT, convT, ys2T)
                    for dc in range(8):
                        wgb = wg[dc % 2]
                        for b in range(3):
                            c0 = 2048 + b * 1024 + dc * 128
                            dma(POOL, wgb[:, :, b, :], w_in[L][:, c0:c0 + 128].rearrange("(k p) n -> p k n", p=128), writes=[wgb])
                        for th in range(2):
                            ts = slice(th * 512, (th + 1) * 512)
                            for b in range(3):
                                pg = PS[b]
                                for k in range(8):
                                    op(PE, lambda: T_.matmul(pg[:], lhsT=wgb[:, k, b, :], rhs=hT[:, k, ts], start=(k == 0), stop=(k == 7)),
                                       reads=[wgb, hT], writes=[pg], sig=(k == 7))
                                op(ACT, lambda: S.activation(out=sg[b][:], in_=pg[:], func=AF.Sigmoid, bias=binT[:, 16 + b * 8 + dc:17 + b * 8 + dc]),
                                   reads=[pg, binT], writes=[sg[b]])
                                pb = PS[3 + b]
                                for k in range(4):
                                    op(PE, lambda: T_.matmul(pb[:], lhsT=wbr[b][:, k, dc * 128:(dc + 1) * 128], rhs=brT[b][:, k, ts],
                                                             start=(k == 0), stop=(k == 3)), reads=[wbr[b], brT[b]], writes=[pb], sig=(k == 3))
                                if b == 0:
                                    op(DVE, lambda: V.tensor_tensor(out=acc[:], in0=pb[:], in1=sg[b][:], op=ALU.mult), reads=[pb, sg[b]], writes=[acc])
                                else:
                                    op(DVE, lambda: V.tensor_tensor(out=tmp[:], in0=pb[:], in1=sg[b][:], op=ALU.mult), reads=[pb, sg[b]], writes=[tmp])
                                    if b == 1:
                                        op(DVE, lambda: V.tensor_tensor(out=acc[:], in0=acc[:], in1=tmp[:], op=ALU.add), reads=[acc, tmp], writes=[acc])
                                    else:
                                        op(DVE, lambda: V.tensor_tensor(out=mixedT[:, dc, ts], in0=acc[:], in1=tmp[:], op=ALU.add),
                                           reads=[acc, tmp], writes=[mixedT])
                    for tt in range(8):
                        for dh in range(2):
                            pa = PS[6 + dh]
                            ds_ = slice(dh * 512, (dh + 1) * 512)
                            for k in range(8):
                                op(PE, lambda: T_.matmul(pa[:], lhsT=mixedT[:, k, tt * 128:(tt + 1) * 128], rhs=wo[:, k, ds_], start=(k == 0), stop=(k == 7)),
                                   reads=[mixedT, wo], writes=[pa], sig=(k == 7))
                            op(DVE, lambda: V.tensor_tensor(out=tmp[:], in0=pa[:], in1=g1b[:, ds_], op=ALU.mult), reads=[pa, g1b], writes=[tmp])
                            op(DVE, lambda: V.tensor_tensor(out=x[:, tt, ds_], in0=x[:, tt, ds_], in1=tmp[:], op=ALU.add), reads=[x, tmp], writes=[x])
                    fw.barrier()
                fw.barrier()

            fw.mute = False
            if dbg and L == 0:
                DX = fw.view("dbg_x", dbg_x)
                for tt in range(8):
                    dma(SP, dbg_x[tt * 128:(tt + 1) * 128, :], x[:, tt, :], reads=[x], writes=[DX])

            fw.mute = "moe" in skip
            with ExitStack() as moe:
                moe_block(fw, nc, moe, L, PS, x, hT, g2b, identf, norm_transpose,
                          dict(router_w=router_w, router_b=router_b, w_gu=w_gu, b_guT=b_guT, w_dn=w_dn, b_dn=b_dn))
                fw.barrier()

        fw.mute = False
        with ExitStack() as sub:
            fg = fw.sb("fg", [128, 1024], es=sub)
            junk = fw.sb("junkf", [128, 1024], es=sub)
            ss = fw.sb("ssf", [128, 8], es=sub)
            yo = [fw.sb(f"yo{i}", [128, 1024], es=sub) for i in range(2)]
            dma(SP, fg[:], fin_g.partition_broadcast(128), writes=[fg])
            op(DVE, lambda: V.memset(ss[:], 0.0), writes=[ss])
            for tt in range(8):
                op(ACT, lambda: S.activation(out=junk[:], in_=x[:, tt, :], func=AF.Square, accum_out=ss[:, tt:tt + 1]), reads=[x], writes=[junk, ss])
                op(DVE, lambda: V.tensor_scalar(out=ss[:, tt:tt + 1], in0=ss[:, tt:tt + 1], scalar1=1.0 / 1024.0, scalar2=EPS, op0=ALU.mult, op1=ALU.add),
                   reads=[ss], writes=[ss])
                op(ACT, lambda: S.sqrt(out=ss[:, tt:tt + 1], in_=ss[:, tt:tt + 1]), reads=[ss], writes=[ss])
                op(DVE, lambda: V.reciprocal(out=ss[:, tt:tt + 1], in_=ss[:, tt:tt + 1]), reads=[ss], writes=[ss])
                yb = yo[tt % 2]
                op(DVE, lambda: V.scalar_tensor_tensor(out=yb[:], in0=x[:, tt, :], scalar=ss[:, tt:tt + 1], in1=fg[:], op0=ALU.mult, op1=ALU.mult),
                   reads=[x, ss, fg], writes=[yb])
                dma(SP, y_out[tt * 128:(tt + 1) * 128, :], yb[:], reads=[yb], writes=[OUTB])
            fw.barrier()
    return nc


def moe_block(fw, nc, es, L, PS, x, hT, g2b, identf, norm_transpose, W):
    V, S, G_, T_ = nc.vector, nc.scalar, nc.gpsimd, nc.tensor
    op, dma = fw.op, fw.dma
    if "moe_early" in fw.skipset:
        with ExitStack() as sub0:
            norm_transpose(sub0, 8, 24, router=None)
            fw.barrier()
    comb = fw.sb("comb", [128, 8, 32], es=es)
    rw = fw.sb("rw", [128, 8, 32], es=es)
    rb = fw.sb("rb", [128, 32], es=es)
    lg = fw.sb("lg", [128, 32], es=es)
    ex = fw.sb("ex", [128, 32], es=es)
    mk = fw.sb("mk", [128, 32], es=es)
    m8 = fw.sb("m8", [128, 8], es=es)
    sm = fw.sb("sm", [128, 4], es=es)
    fw.mute = ("moe" in fw.skipset) or ("moe_loads" in fw.skipset)
    dma(SP, rw[:], W["router_w"][L].rearrange("(k p) e -> p k e", p=128), writes=[rw])
    dma(SP, rb[:], W["router_b"][L].partition_broadcast(128), writes=[rb])
    fw.barrier()
    fw.mute = "moe" in fw.skipset

    def router(tt, hf):
        pl = PS[6]
        for k in range(8):
            op(PE, lambda: T_.matmul(pl[:, 0:32], lhsT=hf[:, k, :], rhs=rw[:, k, :], start=(k == 0), stop=(k == 7)),
               reads=[hf, rw], writes=[pl], sig=(k == 7))
        op(DVE, lambda: V.tensor_tensor(out=lg[:], in0=pl[:, 0:32], in1=rb[:], op=ALU.add), reads=[pl, rb], writes=[lg])
        if "moe_rt_mm_only" in fw.skipset:
            return
        op(DVE, lambda: V.max(m8[:], lg[:]), reads=[lg], writes=[m8])
        op(DVE, lambda: V.tensor_scalar(out=sm[:, 0:1], in0=m8[:, 0:1], scalar1=-1.0, scalar2=None, op0=ALU.mult), reads=[m8], writes=[sm])
        op(ACT, lambda: S.activation(out=ex[:], in_=lg[:], func=AF.Exp, bias=sm[:, 0:1]), reads=[lg, sm], writes=[ex])
        op(DVE, lambda: V.tensor_scalar(out=mk[:], in0=lg[:], scalar1=m8[:, 3:4], scalar2=None, op0=ALU.is_ge), reads=[lg, m8], writes=[mk])
        op(DVE, lambda: V.tensor_tensor(out=ex[:], in0=ex[:], in1=mk[:], op=ALU.mult), reads=[ex, mk], writes=[ex])
        op(DVE, lambda: V.reduce_sum(out=sm[:, 1:2], in_=ex[:], axis=AX.X), reads=[ex], writes=[sm])
        op(DVE, lambda: V.reciprocal(out=sm[:, 2:3], in_=sm[:, 1:2]), reads=[sm], writes=[sm])
        op(DVE, lambda: V.tensor_scalar(out=comb[:, tt, :], in0=ex[:], scalar1=sm[:, 2:3], scalar2=None, op0=ALU.mult), reads=[ex, sm], writes=[comb])

    with ExitStack() as sub:
        fw.mute = "moe_router" in fw.skipset
        norm_transpose(sub, 8, 24, router=(None if "moe_norouter" in fw.skipset else router))
        fw.barrier()
        fw.mute = "moe" in fw.skipset

    acc = fw.sb("acc", [128, 8, 1024], es=es)
    bgu = fw.sb("bgu", [128, 512], es=es)
    bgu1 = fw.sb("bgu1", [128, 512], es=es)
    bdn = fw.sb("bdn", [32, 1024], es=es)
    combT = fw.sb("combT", [32, 1024], es=es)
    fw.mute = ("moe" in fw.skipset) or ("moe_loads" in fw.skipset)
    dma(SP, bgu[:], W["b_guT"][L], writes=[bgu])
    dma(SP, bdn[:], W["b_dn"][L], writes=[bdn])
    op(DVE, lambda: V.tensor_scalar(out=bgu1[:], in0=bgu[:], scalar1=1.0, scalar2=None, op0=ALU.add), reads=[bgu], writes=[bgu1])
    fw.mute = "moe_init" in fw.skipset
    for half in range(2):
        pa = PS[half]
        for t4 in range(4):
            tt = half * 4 + t4
            op(PE, lambda: T_.transpose(pa[0:32, t4 * 128:(t4 + 1) * 128], comb[:, tt, :], identf[:]), reads=[comb, identf], writes=[pa], sig=(t4 == 3))
        op(DVE, lambda: V.tensor_copy(combT[:, half * 512:(half + 1) * 512], pa[0:32, :]), reads=[pa], writes=[combT])
    for tt in range(8):
        for dh in range(2):
            pa = PS[2 + dh]
            op(PE, lambda: T_.matmul(pa[:], lhsT=combT[:, tt * 128:(tt + 1) * 128], rhs=bdn[:, dh * 512:(dh + 1) * 512], start=True, stop=True),
               reads=[combT, bdn], writes=[pa])
            op(ACT, lambda: S.copy(out=acc[:, tt, dh * 512:(dh + 1) * 512], in_=pa[:]), reads=[pa], writes=[acc])

    fw.mute = "moe" in fw.skipset
    wgu = [fw.sb(f"wgu{i}", [128, 8, 2, 256], BF16, es=es) for i in range(3)]
    wdn = [fw.sb(f"wdn{i}", [128, 8, 1024], BF16, es=es) for i in range(2)]
    actT = [fw.sb(f"actT{i}", [128, 8, 1024], BF16, es=es) for i in range(2)]
    gm = [fw.sb(f"gm{i}", [128, 512], es=es) for i in range(2)]
    sg = [fw.sb(f"sgm{i}", [128, 512], es=es) for i in range(2)]
    um = [fw.sb(f"um{i}", [128, 512], es=es) for i in range(2)]
    cnt = [0]

    def GU(e):
        at = actT[e % 2]
        wd = wdn[e % 2]
        for h2 in range(2):
            dma(POOL, wd[:, h2 * 4:(h2 + 1) * 4, :], W["w_dn"][L, e][h2 * 512:(h2 + 1) * 512, :].rearrange("(k p) n -> p k n", p=128), writes=[wd])
        for pc in range(4):
            wb = wgu[cnt[0] % 3]
            cnt[0] += 1
            for two in range(2):
                c0 = two * 1024 + pc * 256
                dma(POOL, wb[:, :, two, :], W["w_gu"][L, e][:, c0:c0 + 256].rearrange("(k p) n -> p k n", p=128), writes=[wb])
            for fl in range(2):
                fc = pc * 2 + fl
                for th in range(2):
                    u = (fc * 2 + th) % 2
                    ts = slice(th * 512, (th + 1) * 512)
                    pg, pu = PS[u], PS[2 + u]
                    for k in range(8):
                        op(PE, lambda: T_.matmul(pg[:], lhsT=wb[:, k, 0, fl * 128:(fl + 1) * 128], rhs=hT[:, k, ts], start=(k == 0), stop=(k == 7)),
                           reads=[wb, hT], writes=[pg], sig=(k == 7))
                    for k in range(8):
                        op(PE, lambda: T_.matmul(pu[:], lhsT=wb[:, k, 1, fl * 128:(fl + 1) * 128], rhs=hT[:, k, ts], start=(k == 0), stop=(k == 7)),
                           reads=[wb, hT], writes=[pu], sig=(k == 7))
                    cg = e * 16 + fc
                    op(DVE, lambda: V.tensor_scalar(out=gm[u][:], in0=pg[:], scalar1=bgu[:, cg:cg + 1], scalar2=7.0, op0=ALU.add, op1=ALU.min),
                       reads=[pg, bgu], writes=[gm[u]])
                    op(ACT, lambda: S.activation(out=sg[u][:], in_=gm[u][:], func=AF.Sigmoid, scale=1.702), reads=[gm[u]], writes=[sg[u]])
                    op(DVE, lambda: V.tensor_scalar(out=um[u][:], in0=pu[:], scalar1=bgu1[:, cg + 8:cg + 9], scalar2=8.0, op0=ALU.add, op1=ALU.min),
                       reads=[pu, bgu1], writes=[um[u]])
                    op(DVE, lambda: V.tensor_tensor(out=gm[u][:], in0=gm[u][:], in1=sg[u][:], op=ALU.mult), reads=[gm[u], sg[u]], writes=[gm[u]])
                    op(DVE, lambda: V.scalar_tensor_tensor(out=at[:, fc, ts], in0=um[u][:], scalar=-6.0, in1=gm[u][:], op0=ALU.max, op1=ALU.mult),
                       reads=[um[u], gm[u]], writes=[at])

    def DN(e):
        at = actT[e % 2]
        wd = wdn[e % 2]
        for tt in range(8):
            for dh in range(2):
                pa = PS[4 + (tt * 2 + dh) % 2]
                ds_ = slice(dh * 512, (dh + 1) * 512)
                for f in range(8):
                    op(PE, lambda: T_.matmul(pa[:], lhsT=at[:, f, tt * 128:(tt + 1) * 128], rhs=wd[:, f, ds_], start=(f == 0), stop=(f == 7)),
                       reads=[at, wd], writes=[pa], sig=(f == 7))
                op(DVE, lambda: V.scalar_tensor_tensor(out=acc[:, tt, ds_], in0=pa[:], scalar=comb[:, tt, e:e + 1], in1=acc[:, tt, ds_],
                                                       op0=ALU.mult, op1=ALU.add), reads=[pa, comb, acc], writes=[acc])

    fw.mute = "moe_exp" in fw.skipset
    GU(0)
    for e in range(NEXP_RUN[0]):
        if e + 1 < NEXP_RUN[0]:
            GU(e + 1)
        DN(e)
    fw.mute = ("moe" in fw.skipset) or ("moe_fin" in fw.skipset)
    for tt in range(8):
        op(DVE, lambda: V.tensor_tensor(out=acc[:, tt, :], in0=acc[:, tt, :], in1=g2b[:], op=ALU.mult), reads=[acc, g2b], writes=[acc])
        op(DVE, lambda: V.tensor_tensor(out=x[:, tt, :], in0=x[:, tt, :], in1=acc[:, tt, :], op=ALU.add), reads=[x, acc], writes=[x])


def s5_block(fw, nc, es, L, PS, Zc, ys2T, identb, identf, onesb, kcol_sb, W, STB, st_out):
    V, S, G_, T_ = nc.vector, nc.scalar, nc.gpsimd, nc.tensor
    op, dma = fw.op, fw.dma
    PI = math.pi
    U = fw.sb("U", [128, 32, 128], BF16, es=es)
    ST = [fw.sb(f"ST{r}", [128, 32, 128], BF16, es=es) for r in range(2)]
    ChT = [fw.sb(f"ChT{r}", [128, 32, 128], BF16, es=es) for r in range(2)]
    Dh = [fw.sb(f"Dh{r}", [128, 32, 128], BF16, es=es) for r in range(2)]
    fin = fw.sb("fin", [4, 1024], es=es)
    cst = fw.sb("cst", [128, 8], es=es)
    op(POOL, lambda: G_.memset(cst[:, 0:1], 0.5 * PI), writes=[cst])
    op(POOL, lambda: G_.memset(cst[:, 1:2], 256.0), writes=[cst])
    op(DVE, lambda: V.tensor_scalar(out=cst[:, 2:4], in0=kcol_sb[:, 0:2], scalar1=-1.0, scalar2=None, op0=ALU.mult), reads=[kcol_sb], writes=[cst])
    negpi = cst[:, 0:1]

    def bfview(pb):
        return pb[:].bitcast(BF16)

    def sincos(th, t1, s_out, c_out, ti):
        I2P = 1.0 / TWO_PI
        op(DVE, lambda: V.tensor_scalar(out=ti[1], in0=th[1], scalar1=I2P, scalar2=None, op0=ALU.mult), reads=[th[0]], writes=[ti[0]])
        op(DVE, lambda: V.tensor_copy(t1[1], ti[1]), reads=[ti[0]], writes=[t1[0]])
        op(DVE, lambda: V.scalar_tensor_tensor(out=t1[1], in0=t1[1], scalar=-TWO_PI, in1=th[1], op0=ALU.mult, op1=ALU.add),
           reads=[t1[0], th[0]], writes=[t1[0]])
        op(ACT, lambda: S.activation(out=s_out[1], in_=t1[1], func=AF.Sin), reads=[t1[0]], writes=[s_out[0]])
        op(DVE, lambda: V.tensor_scalar(out=ti[1], in0=th[1], scalar1=I2P, scalar2=0.25, op0=ALU.mult, op1=ALU.add), reads=[th[0]], writes=[ti[0]])
        op(DVE, lambda: V.tensor_copy(t1[1], ti[1]), reads=[ti[0]], writes=[t1[0]])
        op(DVE, lambda: V.scalar_tensor_tensor(out=t1[1], in0=t1[1], scalar=-TWO_PI, in1=th[1], op0=ALU.mult, op1=ALU.add),
           reads=[t1[0], th[0]], writes=[t1[0]])
        op(ACT, lambda: S.activation(out=c_out[1], in_=t1[1], func=AF.Sin, bias=negpi), reads=[t1[0], cst], writes=[c_out[0]])

    for rd in range(4):
        pb = PS[rd % 2]
        pv = bfview(pb)
        for gg in range(8):
            g = rd * 8 + gg
            op(PE, lambda: T_.transpose(pv[:, gg * 128:(gg + 1) * 128], Zc[:, g, :], identb[:]), reads=[Zc, identb], writes=[pb], sig=(gg == 7))
        op(DVE, lambda: V.tensor_copy(U[:, rd * 8:(rd + 1) * 8, :], pv.rearrange("p (g c) -> p g c", c=128)), reads=[pb], writes=[U])

    for r in range(2):
        with ExitStack() as sd:
            Bh = fw.sb("Bh", [128, 32, 128], BF16, es=sd)
            with ExitStack() as st:
                lt = fw.sb("lt", [128, 3, 32], es=st)
                B2 = fw.sb("B2", [128, 2, 512], es=st)
                C2 = fw.sb("C2", [128, 2, 512], es=st)
                jv = fw.sb("jv", [128, 4, 8], es=st)
                mask = fw.sb("mask", [128, 128], es=st)
                dcr = fw.sb("dcr", [128, 32], es=st)
                dma(SP, lt[:], W["lamT"][L, r], writes=[lt])
                dma(SP, B2[:], W["BT2"][L, r], writes=[B2])
                dma(SP, C2[:], W["CT2"][L, r], writes=[C2])
                dma(SP, jv[:], W["jvals"][r], writes=[jv])
                dma(SP, mask[:], W["dmask"][r], writes=[mask])
                dma(SP, dcr[:], W["dcolrep"][L], writes=[dcr])
                aT = fw.sb("aT", [128, 2, 32], es=st)
                dtT = fw.sb("dtT", [128, 32], es=st)
                op(ACT, lambda: S.activation(out=dtT[:], in_=lt[:, 2, :], func=AF.Exp), reads=[lt], writes=[dtT])
                for i in range(2):
                    op(DVE, lambda: V.tensor_tensor(out=aT[:, i, :], in0=lt[:, i, :], in1=dtT[:], op=ALU.mult), reads=[lt, dtT], writes=[aT])
                mg = fw.sb("mg", [128, 32, 8], es=st)
                th = fw.sb("th", [128, 32, 8], es=st)
                tq = fw.sb("tq", [128, 32, 8], es=st)
                sn = fw.sb("sn", [128, 32, 8], es=st)
                cs = fw.sb("cs", [128, 32, 8], es=st)
                tiT = fw.sb("tiT", [128, 32, 8], mybir.dt.int32, es=st)
                Pt = [fw.sb(f"Pt{i}", [128, 2, 32, 8], es=st) for i in range(4)]

                def genT(ei):
                    jb = jv[:, ei, :].unsqueeze(1).to_broadcast([128, 32, 8])
                    ar = aT[:, 0, :].unsqueeze(2).to_broadcast([128, 32, 8])
                    ai = aT[:, 1, :].unsqueeze(2).to_broadcast([128, 32, 8])
                    op(DVE, lambda: V.tensor_tensor(out=mg[:], in0=ar, in1=jb, op=ALU.mult), reads=[aT, jv], writes=[mg])
                    op(ACT, lambda: S.activation(out=mg[:], in_=mg[:], func=AF.Exp), reads=[mg], writes=[mg])
                    op(DVE, lambda: V.tensor_tensor(out=th[:], in0=ai, in1=jb, op=ALU.mult), reads=[aT, jv], writes=[th])
                    sincos((th, th[:]), (tq, tq[:]), (sn, sn[:]), (cs, cs[:]), (tiT, tiT[:]))
                    op(DVE, lambda: V.tensor_tensor(out=Pt[ei][:, 0], in0=mg[:], in1=cs[:], op=ALU.mult), reads=[mg, cs], writes=[Pt[ei]])
                    op(DVE, lambda: V.tensor_tensor(out=Pt[ei][:, 1], in0=mg[:], in1=sn[:], op=ALU.mult), reads=[mg, sn], writes=[Pt[ei]])

                for ei in range(4):
                    genT(ei)
                op(DVE, lambda: V.tensor_tensor(out=Pt[1][:], in0=Pt[1][:], in1=Pt[0][:], op=ALU.subtract), reads=[Pt[1], Pt[0]], writes=[Pt[1]])
                inv = fw.sb("inv", [128, 4, 32], es=st)
                op(DVE, lambda: V.tensor_tensor(out=inv[:, 0, :], in0=lt[:, 0, :], in1=lt[:, 0, :], op=ALU.mult), reads=[lt], writes=[inv])
                op(DVE, lambda: V.tensor_tensor(out=inv[:, 1, :], in0=lt[:, 1, :], in1=lt[:, 1, :], op=ALU.mult), reads=[lt], writes=[inv])
                op(DVE, lambda: V.tensor_tensor(out=inv[:, 0, :], in0=inv[:, 0, :], in1=inv[:, 1, :], op=ALU.add), reads=[inv], writes=[inv])
                op(DVE, lambda: V.reciprocal(out=inv[:, 1, :], in_=inv[:, 0, :]), reads=[inv], writes=[inv])
                op(DVE, lambda: V.tensor_tensor(out=inv[:, 2, :], in0=lt[:, 0, :], in1=inv[:, 1, :], op=ALU.mult), reads=[lt, inv], writes=[inv])
                op(DVE, lambda: V.scalar_tensor_tensor(out=inv[:, 3, :], in0=lt[:, 1, :], scalar=-1.0, in1=inv[:, 1, :], op0=ALU.mult, op1=ALU.mult),
                   reads=[lt, inv], writes=[inv])
                ir = inv[:, 2, :].unsqueeze(2).to_broadcast([128, 32, 8])
                ii = inv[:, 3, :].unsqueeze(2).to_broadcast([128, 32, 8])
                op(DVE, lambda: V.tensor_tensor(out=mg[:], in0=Pt[1][:, 0], in1=ir, op=ALU.mult), reads=[Pt[1], inv], writes=[mg])
                op(DVE, lambda: V.tensor_tensor(out=th[:], in0=Pt[1][:, 1], in1=ii, op=ALU.mult), reads=[Pt[1], inv], writes=[th])
                op(DVE, lambda: V.tensor_tensor(out=Pt[0][:, 0], in0=mg[:], in1=th[:], op=ALU.subtract), reads=[mg, th], writes=[Pt[0]])
                op(DVE, lambda: V.tensor_tensor(out=mg[:], in0=Pt[1][:, 0], in1=ii, op=ALU.mult), reads=[Pt[1], inv], writes=[mg])
                op(DVE, lambda: V.tensor_tensor(out=th[:], in0=Pt[1][:, 1], in1=ir, op=ALU.mult), reads=[Pt[1], inv], writes=[th])
                op(DVE, lambda: V.tensor_tensor(out=Pt[0][:, 1], in0=mg[:], in1=th[:], op=ALU.add), reads=[mg, th], writes=[Pt[0]])

                ta = fw.sb("ta", [128, 8, 8, 16], es=st)
                tb = fw.sb("tb", [128, 8, 8, 16], es=st)
                taL, taH = fw.view("taL", ta.t), fw.view("taH", ta.t)
                tbL, tbH = fw.view("tbL", tb.t), fw.view("tbH", tb.t)
                BhT = fw.sb("BhT", [128, 32, 128], BF16, es=st)
                CDT = fw.sb("CDT", [128, 32, 128], BF16, es=st)

                def cplx_table(out, X, Y, neg):
                    oL, oH = fw.view("oL", out.t), fw.view("oH", out.t)
                    for gh in range(4):
                        gs = slice(gh * 8, (gh + 1) * 8)

                        def xb(ps_, c):
                            return X[ps_, c, gs, :].unsqueeze(3).to_broadcast([64, 8, 8, 16])

                        def yb(ps_, c):
                            return Y[ps_, c, gh * 128:(gh + 1) * 128].rearrange("p (g h) -> p g h", h=16).unsqueeze(2).to_broadcast([64, 8, 8, 16])

                        lo, hi = slice(0, 64), slice(64, 128)
                        ov = out[:].rearrange("p g (j h) -> p g j h", h=16)
                        op(DVE, lambda: V.tensor_tensor(out=ta[lo], in0=xb(lo, 0), in1=yb(lo, 0), op=ALU.mult), reads=[X, Y], writes=[taL])
                        op(DVE, lambda: V.tensor_tensor(out=tb[lo], in0=xb(lo, 1), in1=yb(lo, 1), op=ALU.mult), reads=[X, Y], writes=[tbL])
                        op(DVE, lambda: V.tensor_tensor(out=ov[lo, gs], in0=ta[lo], in1=tb[lo], op=ALU.subtract), reads=[taL, tbL], writes=[oL])
                        op(DVE, lambda: V.tensor_tensor(out=ta[hi], in0=xb(hi, 0), in1=yb(hi, 1), op=ALU.mult), reads=[X, Y], writes=[taH])
                        op(DVE, lambda: V.tensor_tensor(out=tb[hi], in0=xb(hi, 1), in1=yb(hi, 0), op=ALU.mult), reads=[X, Y], writes=[tbH])
                        if neg:
                            op(DVE, lambda: V.scalar_tensor_tensor(out=ov[hi, gs], in0=ta[hi], scalar=-1.0, in1=tb[hi], op0=ALU.mult, op1=ALU.subtract),
                               reads=[taH, tbH], writes=[oH])
                        else:
                            op(DVE, lambda: V.tensor_tensor(out=ov[hi, gs], in0=ta[hi], in1=tb[hi], op=ALU.add), reads=[taH, tbH], writes=[oH])
                    return [oL, oH]

                bh_d = cplx_table(BhT, Pt[0], B2, False)
                ch_d = cplx_table(ChT[r], Pt[2], C2, True)
                cd_d = cplx_table(CDT, Pt[3], C2, True)
                op(DVE, lambda: V.tensor_copy(ChT[r][0:1, 0, 0:1], ChT[r][0:1, 0, 0:1]), reads=ch_d, writes=[ChT[r]])
                for rd in range(4):
                    pb = PS[2 + rd % 2]
                    pv = bfview(pb)
                    for gg in range(8):
                        g = rd * 8 + gg
                        op(PE, lambda: T_.transpose(pv[:, gg * 128:(gg + 1) * 128], BhT[:, g, :], identb[:]), reads=bh_d + [identb], writes=[pb], sig=(gg == 7))
                    op(ACT, lambda: S.copy(out=Bh[:, rd * 8:(rd + 1) * 8, :], in_=pv.rearrange("p (g c) -> p g c", c=128)), reads=[pb], writes=[Bh])
                dtmp = fw.sb("dtmp", [128, 4, 128], es=st)
                dtmp2 = fw.sb("dtmp2", [128, 4, 128], es=st)
                for rd in range(8):
                    pd_ = PS[4 + rd % 2]
                    for gg in range(4):
                        g = rd * 4 + gg
                        op(PE, lambda: T_.matmul(pd_[:, gg * 128:(gg + 1) * 128], lhsT=BhT[:, g, :], rhs=CDT[:, g, :], start=True, stop=True),
                           reads=bh_d + cd_d, writes=[pd_], sig=(gg == 3))
                    mb = mask[:].unsqueeze(1).to_broadcast([128, 4, 128])
                    pdv = pd_[:].rearrange("p (g c) -> p g c", c=128)
                    if r == 0:
                        op(DVE, lambda: V.tensor_tensor(out=dtmp[:], in0=pdv, in1=mb, op=ALU.mult), reads=[pd_, mask], writes=[dtmp])
                        op(DVE, lambda: V.tensor_tensor(out=dtmp2[:], in0=identf[:].unsqueeze(1).to_broadcast([128, 4, 128]),
                                                        in1=dcr[:, rd * 4:(rd + 1) * 4].unsqueeze(2).to_broadcast([128, 4, 128]), op=ALU.mult),
                           reads=[identf, dcr], writes=[dtmp2])
                        op(DVE, lambda: V.tensor_tensor(out=Dh[r][:, rd * 4:(rd + 1) * 4, :], in0=dtmp[:], in1=dtmp2[:], op=ALU.add),
                           reads=[dtmp, dtmp2], writes=[Dh[r]])
                    else:
                        op(DVE, lambda: V.tensor_tensor(out=Dh[r][:, rd * 4:(rd + 1) * 4, :], in0=pdv, in1=mb, op=ALU.mult), reads=[pd_, mask], writes=[Dh[r]])
                fw.barrier()

            with ExitStack() as sr:
                Wt = fw.sb("Wt", [128, 32, 128], BF16, es=sr)
                Sx = fw.sb("Sx", [128, 32, 128], BF16, es=sr)
                trib = fw.sb("trib", [128, 128], BF16, es=sr)
                mselb = fw.sb("mselb", [128, 4], BF16, es=sr)
                h0b = fw.sb("h0b", [1, 4096], BF16, es=sr)
                dma(POOL, trib[:], W["tri"][r], writes=[trib])
                dma(POOL, mselb[:], W["msel"], writes=[mselb])
                dma(POOL, h0b[:], W["h0row"][L, r], writes=[h0b])
                lrow = fw.sb("lrow", [128, 3, 512], es=sr)
                Aa = fw.sb("Aa", [128, 2, 512], es=sr)
                dtb = fw.sb("dtb", [128, 512], es=sr)
                mgr = fw.sb("mgr", [128, 512], es=sr)
                thr = fw.sb("thr", [128, 512], es=sr)
                tqr = fw.sb("tqr", [128, 512], es=sr)
                snr = fw.sb("snr", [128, 512], es=sr)
                csr = fw.sb("csr", [128, 512], es=sr)
                tiR = fw.sb("tiR", [128, 512], mybir.dt.int32, es=sr)
                E = [fw.sb(f"E{i}", [128, 2, 512], es=sr) for i in range(3)]
                tar = fw.sb("tar", [128, 256], es=sr)
                tbr = fw.sb("tbr", [128, 256], es=sr)

                def genR(Eo, col, magcol, conj):
                    op(ACT, lambda: S.activation(out=mgr[:], in_=Aa[:, 0, :], func=AF.Exp, scale=magcol), reads=[Aa, cst, kcol_sb], writes=[mgr])
                    op(DVE, lambda: V.tensor_scalar(out=thr[:], in0=Aa[:, 1, :], scalar1=col, scalar2=None, op0=ALU.mult), reads=[Aa, cst, kcol_sb], writes=[thr])
                    sincos((thr, thr[:]), (tqr, tqr[:]), (snr, snr[:]), (csr, csr[:]), (tiR, tiR[:]))
                    op(DVE, lambda: V.tensor_tensor(out=Eo[:, 0, :], in0=mgr[:], in1=csr[:], op=ALU.mult), reads=[mgr, csr], writes=[Eo])
                    if conj:
                        op(DVE, lambda: V.scalar_tensor_tensor(out=Eo[:, 1, :], in0=mgr[:], scalar=-1.0, in1=snr[:], op0=ALU.mult, op1=ALU.mult),
                           reads=[mgr, snr], writes=[Eo])
                    else:
                        op(DVE, lambda: V.tensor_tensor(out=Eo[:, 1, :], in0=mgr[:], in1=snr[:], op=ALU.mult), reads=[mgr, snr], writes=[Eo])

                def cmul(outv, outbuf, srcv, srcbuf, Et, hb, parts=slice(0, 128)):
                    er = Et[parts, 0, hb * 256:(hb + 1) * 256].rearrange("c (g p) -> c g p", p=64)
                    ei = Et[parts, 1, hb * 256:(hb + 1) * 256].rearrange("c (g p) -> c g p", p=64)
                    tav = tar[parts].rearrange("c (g p) -> c g p", p=64)
                    tbv = tbr[parts].rearrange("c (g p) -> c g p", p=64)
                    op(DVE, lambda: V.tensor_tensor(out=tav, in0=srcv[:, :, 0, :], in1=er, op=ALU.mult), reads=[srcbuf, Et], writes=[tar])
                    op(DVE, lambda: V.tensor_tensor(out=tbv, in0=srcv[:, :, 1, :], in1=ei, op=ALU.mult), reads=[srcbuf, Et], writes=[tbr])
                    op(DVE, lambda: V.tensor_tensor(out=outv[:, :, 0, :], in0=tav, in1=tbv, op=ALU.subtract), reads=[tar, tbr], writes=[outbuf])
                    op(DVE, lambda: V.tensor_tensor(out=tav, in0=srcv[:, :, 0, :], in1=ei, op=ALU.mult), reads=[srcbuf, Et], writes=[tar])
                    op(DVE, lambda: V.tensor_tensor(out=tbv, in0=srcv[:, :, 1, :], in1=er, op=ALU.mult), reads=[srcbuf, Et], writes=[tbr])
                    op(DVE, lambda: V.tensor_tensor(out=outv[:, :, 1, :], in0=tav, in1=tbv, op=ALU.add), reads=[tar, tbr], writes=[outbuf])

                for rd in range(4):
                    for i in range(3):
                        dma(SP, lrow[:, i, :], W["lam_row"][L, r, i][:, rd * 512:(rd + 1) * 512].partition_broadcast(128), writes=[lrow])
                    op(ACT, lambda: S.activation(out=dtb[:], in_=lrow[:, 2, :], func=AF.Exp), reads=[lrow], writes=[dtb])
                    for i in range(2):
                        op(DVE, lambda: V.tensor_tensor(out=Aa[:, i, :], in0=lrow[:, i, :], in1=dtb[:], op=ALU.mult), reads=[lrow, dtb], writes=[Aa])
                    genR(E[0], kcol_sb[:, r:r + 1], cst[:, 2 + r:3 + r], True)
                    genR(E[1], kcol_sb[:, 2 + r:3 + r], kcol_sb[:, 2 + r:3 + r], False)
                    genR(E[2], cst[:, 1:2], cst[:, 1:2], False)
                    for hb in range(2):
                        g0 = rd * 8 + hb * 4
                        pz = PS[hb]
                        for gg in range(4):
                            op(PE, lambda: T_.matmul(pz[:, gg * 128:(gg + 1) * 128], lhsT=U[:, g0 + gg, :], rhs=Bh[:, g0 + gg, :], start=True, stop=True),
                               reads=[U, Bh], writes=[pz], sig=(gg == 3))
                        zv = pz[:].rearrange("c (g two p) -> c g two p", two=2, p=64)
                        wv = Wt[:, g0:g0 + 4, :].rearrange("c g (two p) -> c g two p", two=2)
                        cmul(wv, Wt, zv, pz, E[0], hb)
                        px = PS[2 + hb]
                        op(PE, lambda: T_.matmul(px[:], lhsT=trib[:], rhs=Wt[:, g0:g0 + 4, :].rearrange("c g p -> c (g p)"), start=True, stop=False),
                           reads=[trib, Wt], writes=[px], sig=False)
                        op(PE, lambda: T_.matmul(px[:], lhsT=onesb[0:1, :], rhs=h0b[0:1, g0 * 128:(g0 + 4) * 128], start=False, stop=True),
                           reads=[onesb, h0b], writes=[px])
                        xv = px[:].rearrange("c (g two p) -> c g two p", two=2, p=64)
                        sv = Sx[:, g0:g0 + 4, :].rearrange("c g (two p) -> c g two p", two=2)
                        cmul(sv, Sx, xv, px, E[1], hb)
                        pf = PS[4 + hb]
                        op(PE, lambda: T_.matmul(pf[0:4, :], lhsT=mselb[:], rhs=Wt[:, g0:g0 + 4, :].rearrange("c g p -> c (g p)"), start=True, stop=True),
                           reads=[mselb, Wt], writes=[pf])
                        fv = pf[0:4, :].rearrange("c (g two p) -> c g two p", two=2, p=64)
                        ov = fin[0:4, hb * 512:(hb + 1) * 512].rearrange("c (g two p) -> c g two p", two=2, p=64)
                        cmul(ov, fin, fv, pf, E[2], hb, parts=slice(0, 4))
                    dma(SP, st_out[L, r][:, rd * 1024:(rd + 1) * 1024], fin[:], reads=[fin], writes=[STB])
                    pb = PS[6 + rd % 2]
                    pv = bfview(pb)
                    for gg in range(8):
                        g = rd * 8 + gg
                        op(PE, lambda: T_.transpose(pv[:, gg * 128:(gg + 1) * 128], Sx[:, g, :], identb[:]), reads=[Sx, identb], writes=[pb], sig=(gg == 7))
                    op(ACT, lambda: S.copy(out=ST[r][:, rd * 8:(rd + 1) * 8, :], in_=pv.rearrange("p (g c) -> p g c", c=128)), reads=[pb], writes=[ST[r]])
                fw.barrier()
            fw.barrier()


    with ExitStack() as sy:
        Ycm = fw.sb("Ycm", [128, 8, 512], es=sy)
        ysT = fw.sb("ysT", [128, 4, 1024], es=sy)
        gsT = fw.sb("gsT", [128, 4, 1024], BF16, es=sy)
        q1 = fw.sb("q1", [128, 1024], es=sy)
        q2_ = fw.sb("q2", [128, 1024], es=sy)
        wgl = fw.sb("wgl", [128, 4, 512], BF16, es=sy)
        bgl = fw.sb("bgl", [128, 4], es=sy)
        dma(POOL, wgl[:], W["w_glu"][L].rearrange("(k p) n -> p k n", p=128), writes=[wgl])
        dma(SP, bgl[:], W["b_gluT"][L], writes=[bgl])
        for rd in range(8):
            py = PS[rd % 2]
            for gg in range(4):
                g = rd * 4 + gg
                sl = py[:, gg * 128:(gg + 1) * 128]
                op(PE, lambda: T_.matmul(sl, lhsT=ST[0][:, g, :], rhs=ChT[0][:, g, :], start=True, stop=False), reads=[ST[0], ChT[0]], writes=[py], sig=False)
                op(PE, lambda: T_.matmul(sl, lhsT=ST[1][:, g, :], rhs=ChT[1][:, g, :], start=False, stop=False), reads=[ST[1], ChT[1]], writes=[py], sig=False)
                op(PE, lambda: T_.matmul(sl, lhsT=U[:, g, :], rhs=Dh[0][:, g, :], start=False, stop=False), reads=[U, Dh[0]], writes=[py], sig=False)
                op(PE, lambda: T_.matmul(sl, lhsT=U[:, g, :], rhs=Dh[1][:, g, :], start=False, stop=True), reads=[U, Dh[1]], writes=[py], sig=(gg == 3))
            op(DVE, lambda: V.tensor_copy(Ycm[:].rearrange("c j (g h) -> c g j h", h=16)[:, rd * 4:(rd + 1) * 4, :, :],
                                          py[:].rearrange("c (g j h) -> c g j h", g=4, j=8)), reads=[py], writes=[Ycm])
        for j in range(8):
            pt = PS[2 + j % 2]
            for q in range(4):
                op(PE, lambda: T_.transpose(pt[:, q * 128:(q + 1) * 128], Ycm[:, j, q * 128:(q + 1) * 128], identf[:]), reads=[Ycm, identf], writes=[pt], sig=(q == 3))
            op(ACT, lambda: S.copy(out=ysT[:, :, j::8], in_=pt[:].rearrange("p (q c) -> p q c", c=128)), reads=[pt], writes=[ysT])
        for q in range(4):
            op(ACT, lambda: S.activation(out=q1[:], in_=ysT[:, q, :], func=AF.Square), reads=[ysT], writes=[q1])
            op(DVE, lambda: V.tensor_scalar(out=q1[:], in0=q1[:], scalar1=0.044715, scalar2=1.0, op0=ALU.mult, op1=ALU.add), reads=[q1], writes=[q1])
            op(DVE, lambda: V.tensor_tensor(out=q1[:], in0=q1[:], in1=ysT[:, q, :], op=ALU.mult), reads=[q1, ysT], writes=[q1])
            op(ACT, lambda: S.activation(out=q2_[:], in_=q1[:], func=AF.Sigmoid, scale=1.5957691216057308), reads=[q1], writes=[q2_])
            op(DVE, lambda: V.tensor_tensor(out=gsT[:, q, :], in0=ysT[:, q, :], in1=q2_[:], op=ALU.mult), reads=[ysT, q2_], writes=[gsT])
        for qo in range(4):
            for th in range(2):
                ts = slice(th * 512, (th + 1) * 512)
                pa = PS[4 + (qo * 2 + th) % 2]
                for k in range(4):
                    op(PE, lambda: T_.matmul(pa[:], lhsT=wgl[:, k, qo * 128:(qo + 1) * 128], rhs=gsT[:, k, ts], start=(k == 0), stop=(k == 3)),
                       reads=[wgl, gsT], writes=[pa], sig=(k == 3))
                op(ACT, lambda: S.activation(out=q1[:, 0:512], in_=pa[:], func=AF.Sigmoid, bias=bgl[:, qo:qo + 1]), reads=[pa, bgl], writes=[q1])
                op(DVE, lambda: V.tensor_tensor(out=ys2T[:, qo, ts], in0=gsT[:, qo, ts], in1=q1[:, 0:512], op=ALU.mult), reads=[gsT, q1], writes=[ys2T])
        fw.barrier()


PROMPT_SPLIT = [(0, 3), (3, 6), (6, 9), (9, 12), (12, 14), (14, 16)]


def _pos_embed():
    quarter = 256
    freqs = np.exp(np.float32(-math.log(10000.0)) * np.arange(quarter, dtype=np.float32) / np.float32(quarter)).astype(np.float32)
    r = np.repeat(np.arange(16, dtype=np.float32), 64)
    col = np.tile(np.arange(64, dtype=np.float32), 16)
    ar = (r[:, None] * freqs).astype(np.float32)
    ac = (col[:, None] * freqs).astype(np.float32)
    return np.concatenate([np.sin(ar), np.cos(ar), np.sin(ac), np.cos(ac)], axis=-1).astype(np.float32)


def _core_consts(is_sample):
    Lq = 1024 if is_sample else 256
    nseq = 1024 // Lq
    idx = np.arange(1024)
    seq = idx // Lq
    loc = idx % Lq
    ang = 2.0 * np.pi * ((loc[:, None] * loc[None, :]) % Lq) / Lq
    same = (seq[:, None] == seq[None, :])
    sc = 1.0 / math.sqrt(128.0 * Lq)
    CL = (np.cos(ang) * sc * same).astype(np.float32)
    SL = (-np.sin(ang) * sc * same).astype(np.float32)
    nloc = Lq // 8
    c = np.arange(128)
    cs, cl = c // nloc, c % nloc
    kf, kb = cl, nloc - 1 - cl
    tri = np.zeros((2, 128, 128), np.float32)
    samec = cs[:, None] == cs[None, :]
    tri[0] = (samec & (kf[:, None] < kf[None, :])).astype(np.float32)
    tri[1] = (samec & (kb[:, None] < kb[None, :])).astype(np.float32)
    kcol = np.stack([8.0 * (kf + 1), 8.0 * (kb + 1), 8.0 * kf, 8.0 * kb], axis=1).astype(np.float32)
    msel = np.zeros((128, 4), np.float32)
    msel[c, np.minimum(c // 32, 3)] = 1.0
    flag = np.full((128, 1), 1.0 if is_sample else 0.0, np.float32)
    return dict(CLt=CL, SLt=SL, tri=tri, kcol=kcol, msel=msel, flag=flag)


_NC_CACHE = {}
_DBG = {}


def kernel(**inp):
    f32 = lambda a: np.ascontiguousarray(np.asarray(a), dtype=np.float32)
    I = {k: np.asarray(v) for k, v in inp.items()}
    D_ = I["w_mod"].shape[0]
    shared = {}
    shared["w_mod"] = f32(I["w_mod"])
    shared["b_mod"] = f32(I["b_mod"][:, None, :])
    shared["n1gT"] = f32(I["norm1_g"].reshape(D_, 8, 128).transpose(0, 2, 1))
    shared["n2gT"] = f32(I["norm2_g"].reshape(D_, 8, 128).transpose(0, 2, 1))
    shared["w_in"] = f32(I["w_in"])
    shared["b_inT"] = f32(I["b_in"].reshape(D_, 40, 128).transpose(0, 2, 1))
    shared["b_zs"] = f32(I["b_in"][:, None, 1536:2048])
    shared["w_four"] = f32(I["w_four"])
    shared["cdwT"] = f32(I["conv_dw"].reshape(D_, 31, 4, 128).transpose(0, 3, 2, 1))
    shared["cvec"] = f32(np.stack([I["conv_dw_b"], I["conv_ln_g"], I["conv_ln_b"]], axis=1).reshape(D_, 3, 4, 128).transpose(0, 3, 1, 2))
    shared["w_conv_out"] = f32(I["w_conv_out"])
    ldt = np.repeat(I["ssm_log_dt"][..., None], 64, axis=-1)
    shared["lam_row"] = f32(np.stack([I["ssm_lam_re"], I["ssm_lam_im"], ldt], axis=2).reshape(D_, 2, 3, 1, 2048))
    lt = np.stack([I["ssm_lam_re"], I["ssm_lam_im"], ldt], axis=2)
    lt = lt.transpose(0, 1, 4, 2, 3)
    shared["lamT"] = f32(np.concatenate([lt, lt], axis=2))
    b2 = np.stack([I["ssm_b_re"], I["ssm_b_im"]], axis=2)
    b2 = b2.transpose(0, 1, 4, 2, 3, 5).reshape(D_, 2, 64, 2, 512)
    shared["BT2"] = f32(np.concatenate([b2, b2], axis=2))
    c2 = np.stack([I["ssm_c_re"], I["ssm_c_im"]], axis=2)
    c2 = c2.transpose(0, 1, 5, 2, 3, 4).reshape(D_, 2, 64, 2, 512)
    shared["CT2"] = f32(np.concatenate([c2, c2], axis=2))
    dd = I["ssm_d"].reshape(D_, 32, 16)
    shared["dcolrep"] = f32(np.broadcast_to(dd.transpose(0, 2, 1)[:, None, :, :], (D_, 8, 16, 32)).reshape(D_, 128, 32))
    shared["w_glu"] = f32(I["w_ssm_glu"])
    shared["b_gluT"] = f32(I["b_ssm_glu"].reshape(D_, 4, 128).transpose(0, 2, 1))
    shared["w_ssm_out"] = f32(I["w_ssm_out"])
    shared["w_out"] = f32(I["w_out"])
    shared["router_w"] = f32(I["router_w"])
    shared["router_b"] = f32(I["router_b"][:, None, :])
    shared["w_gu"] = f32(I["w_gate_up"])
    shared["b_guT"] = f32(I["b_gate_up"].reshape(D_, NEXP, 16, 128).transpose(0, 3, 1, 2).reshape(D_, 128, NEXP * 16))
    shared["w_dn"] = f32(I["w_down"])
    shared["b_dn"] = f32(I["b_down"])
    shared["fin_g"] = f32(I["final_norm_g"][None, :])
    cc = np.arange(128)
    angc = 2.0 * np.pi * ((cc[:, None] * cc[None, :]) % 128) / 128.0
    shared["ccsc"] = f32(np.concatenate([np.cos(angc), np.sin(angc)], axis=1))
    j = np.arange(8, dtype=np.float32)
    jv = np.zeros((2, 128, 4, 8), np.float32)
    jv[0, :, 0], jv[0, :, 1], jv[0, :, 2], jv[0, :, 3] = 7 - j, 8 - j, j + 1, j - 7
    jv[1, :, 0], jv[1, :, 1], jv[1, :, 2], jv[1, :, 3] = j, j + 1, 8 - j, -j
    shared["jvals"] = jv
    jj = np.arange(128) // 16
    dm = np.zeros((2, 128, 128), np.float32)
    dm[0] = (jj[:, None] <= jj[None, :])
    dm[1] = (jj[:, None] >= jj[None, :])
    shared["dmask"] = dm

    cs_s, cs_p = _core_consts(True), _core_consts(False)
    pos = _pos_embed()
    zeros_pos = np.zeros((1024, 1024), np.float32)
    zeros_h0 = np.zeros((D_, 2, 1, 4096), np.float32)
    in_maps = []
    for core in range(8):
        m = dict(shared)
        if core < 2:
            b = core
            m.update(cs_s)
            m["xin"] = f32(I["x_sample"][b])
            m["pos"] = pos
            cond = I["c"][b]
            h0 = np.stack([I["state_ssm_re"][b], I["state_ssm_im"][b]], axis=3)
            m["h0row"] = f32(h0.reshape(D_, 2, 1, 4096))
        else:
            lo, hi = PROMPT_SPLIT[core - 2]
            m.update(cs_p)
            xi = np.zeros((4, 256, 1024), np.float32)
            xi[:hi - lo] = I["x_prompt"][lo:hi]
            m["xin"] = xi.reshape(1024, 1024)
            m["pos"] = zeros_pos
            cond = I["c_ctx"]
            m["h0row"] = zeros_h0
        m["condT"] = f32(np.asarray(cond).reshape(8, 128).T)
        in_maps.append(m)

    if "nc" not in _NC_CACHE:
        _NC_CACHE["nc"] = build_nc(depth=_DBG.get("depth", DEPTH), dbg=_DBG.get("dbg", False), skip=_DBG.get("skip", ()))
    nc = _NC_CACHE["nc"]
    res = run_bass_kernel_spmd(nc, in_maps, core_ids=list(range(8)))
    R = res.results
    _DBG["R"] = R if _DBG.get("dbg", False) else None
    y_prompt = np.zeros((16, 256, 1024), np.float32)
    y_sample = np.zeros((2, 1024, 1024), np.float32)
    ns_re = np.zeros((16, D_, 2, 32, 64), np.float32)
    ns_im = np.zeros((16, D_, 2, 32, 64), np.float32)
    for core in range(8):
        yo = np.asarray(R[core]["y_out"], dtype=np.float32)
        if core < 2:
            y_sample[core] = yo
        else:
            lo, hi = PROMPT_SPLIT[core - 2]
            so = np.asarray(R[core]["st_out"], dtype=np.float32).reshape(D_, 2, 4, 32, 2, 64)
            for s in range(hi - lo):
                y_prompt[lo + s] = yo[s * 256:(s + 1) * 256]
                ns_re[lo + s] = so[:, :, s, :, 0, :]
                ns_im[lo + s] = so[:, :, s, :, 1, :]
    return (y_prompt, y_sample, ns_re, ns_im)
```

```python
import math
from contextlib import ExitStack
import numpy as np
import concourse.bass as bass
import concourse.mybir as mybir
from concourse.bass_utils import run_bass_kernel_spmd

F32 = mybir.dt.float32
BF16 = mybir.dt.bfloat16
AF = mybir.ActivationFunctionType
ALU = mybir.AluOpType
AX = mybir.AxisListType

PE, DVE, ACT, POOL, SP = 0, 1, 2, 3, 4
NDMA_SEMS = 24
DEPTH = 4
NEXP = 32
NEXP_RUN = [32]
EPS = 1e-6
TWO_PI = 2.0 * math.pi
ANG_OFF = 64.0 * math.pi


class Buf:
    __slots__ = ("name", "t", "w", "r")

    def __init__(self, name, t):
        self.name = name
        self.t = t
        self.w = None
        self.r = []

    def __getitem__(self, k):
        return self.t[k]


class FW:
    def __init__(self, nc, es):
        self.nc = nc
        self.es = es
        self.engs = [nc.tensor, nc.vector, nc.scalar, nc.gpsimd, nc.sync]
        self.esem = [es.enter_context(nc.semaphore(f"es{i}")) for i in range(5)]
        self.ecnt = [0] * 5
        self.dsem = [es.enter_context(nc.semaphore(f"ds{i}")) for i in range(NDMA_SEMS)]
        self.dcnt = [0] * NDMA_SEMS
        self.dnext = 0
        self.dnext_sw = 0
        self.waited = [dict() for _ in range(5)]
        self.uid = 0
        self.mute = False

    def sb(self, name, shape, dt=F32, es=None):
        self.uid += 1
        t = (es or self.es).enter_context(self.nc.sbuf_tensor(f"{name}_{self.uid}", list(shape), dt))
        esz = 2 if dt == BF16 else 4
        nbytes = esz * int(np.prod(shape[1:]))
        pad = (-nbytes) % 64
        if pad:
            self.uid += 1
            (es or self.es).enter_context(self.nc.sbuf_tensor(f"pad_{self.uid}", [128, pad // 2], BF16))
        return Buf(name, t)

    def ps(self, name, shape, dt=F32, es=None):
        self.uid += 1
        t = (es or self.es).enter_context(self.nc.psum_tensor(f"{name}_{self.uid}", list(shape), dt))
        return Buf(name, t)

    def view(self, name, t):
        return Buf(name, t)

    def _wait(self, e, dep):
        if dep is None:
            return
        kind, idx, cnt = dep
        key = (kind, idx)
        if kind == "e" and idx == PE and e == PE:
            return
        if self.waited[e].get(key, 0) >= cnt:
            return
        sem = self.esem[idx] if kind == "e" else self.dsem[idx]
        self.engs[e].wait_ge(sem, cnt)
        self.waited[e][key] = cnt

    def _deps(self, e, reads, writes):
        for b in reads:
            self._wait(e, b.w)
        for b in writes:
            self._wait(e, b.w)
            for d in b.r:
                self._wait(e, d)

    def _mark(self, dep, reads, writes):
        for b in reads:
            b.r.append(dep)
            if len(b.r) > 16:
                m = {}
                for k, i, c in b.r:
                    m[(k, i)] = max(m.get((k, i), 0), c)
                b.r = [(k, i, c) for (k, i), c in m.items()]
        for b in writes:
            b.w = dep
            b.r = []

    def op(self, e, fn, reads=(), writes=(), sig=True):
        if self.mute:
            return None
        self._deps(e, reads, writes)
        ins = fn()
        if sig:
            self.ecnt[e] += 1
            ins.then_inc(self.esem[e], 1)
            dep = ("e", e, self.ecnt[e])
        else:
            dep = ("e", e, self.ecnt[e] + 1)
        self._mark(dep, reads, writes)
        return ins

    def dma(self, e, out, in_, reads=(), writes=(), **kw):
        if self.mute:
            return None
        self._deps(e, reads, writes)
        half = NDMA_SEMS // 2
        if e == POOL:
            k = half + self.dnext_sw
            self.dnext_sw = (self.dnext_sw + 1) % half
        else:
            k = self.dnext
            self.dnext = (self.dnext + 1) % half
        if self.dcnt[k] > 0:
            self._wait(e, ("d", k, self.dcnt[k]))
        ins = self.engs[e].dma_start(out=out, in_=in_, **kw)
        self.dcnt[k] += 16
        ins.then_inc(self.dsem[k], 16)
        dep = ("d", k, self.dcnt[k])
        self._mark(dep, reads, writes)
        return dep

    def barrier(self):
        for e in range(5):
            for i in range(5):
                if self.ecnt[i] > 0:
                    self._wait(e, ("e", i, self.ecnt[i]))
            for k in range(NDMA_SEMS):
                if self.dcnt[k] > 0:
                    self._wait(e, ("d", k, self.dcnt[k]))


def build_nc(depth=DEPTH, dbg=False, skip=()):
    nc = bass.Bass("TRN2", target_bir_lowering=False)
    V, S, G_, T_ = nc.vector, nc.scalar, nc.gpsimd, nc.tensor

    def din(name, shape, dt=F32):
        return nc.dram_tensor(name, list(shape), dt, kind="ExternalInput").ap()

    def dout(name, shape, dt=F32):
        return nc.dram_tensor(name, list(shape), dt, kind="ExternalOutput").ap()

    xin = din("xin", [1024, 1024])
    pos = din("pos", [1024, 1024])
    condT = din("condT", [128, 8])
    h0row = din("h0row", [depth, 2, 1, 4096])
    CLt = din("CLt", [1024, 1024])
    SLt = din("SLt", [1024, 1024])
    ccsc = din("ccsc", [128, 256])
    tri = din("tri", [2, 128, 128])
    kcol = din("kcol", [128, 4])
    msel = din("msel", [128, 4])
    flag = din("flag", [128, 1])
    jvals = din("jvals", [2, 128, 4, 8])
    dmask = din("dmask", [2, 128, 128])
    w_mod = din("w_mod", [depth, 1024, 6144])
    b_mod = din("b_mod", [depth, 1, 6144])
    n1gT = din("n1gT", [depth, 128, 8])
    n2gT = din("n2gT", [depth, 128, 8])
    w_in = din("w_in", [depth, 1024, 5120])
    b_inT = din("b_inT", [depth, 128, 40])
    b_zs = din("b_zs", [depth, 1, 512])
    w_four = din("w_four", [depth, 512, 1024])
    cdwT = din("cdwT", [depth, 128, 4, 31])
    cvec = din("cvec", [depth, 128, 3, 4])
    w_conv_out = din("w_conv_out", [depth, 512, 1024])
    lam_row = din("lam_row", [depth, 2, 3, 1, 2048])
    lamT = din("lamT", [depth, 2, 128, 3, 32])
    BT2 = din("BT2", [depth, 2, 128, 2, 512])
    CT2 = din("CT2", [depth, 2, 128, 2, 512])
    dcolrep = din("dcolrep", [depth, 128, 32])
    w_glu = din("w_glu", [depth, 512, 512])
    b_gluT = din("b_gluT", [depth, 128, 4])
    w_ssm_out = din("w_ssm_out", [depth, 512, 1024])
    w_out = din("w_out", [depth, 1024, 1024])
    router_w = din("router_w", [depth, 1024, 32])
    router_b = din("router_b", [depth, 1, 32])
    w_gu = din("w_gu", [depth, NEXP_RUN[0], 1024, 2048])
    b_guT = din("b_guT", [depth, 128, NEXP * 16])
    w_dn = din("w_dn", [depth, NEXP_RUN[0], 1024, 1024])
    b_dn = din("b_dn", [depth, NEXP, 1024])
    fin_g = din("fin_g", [1, 1024])
    y_out = dout("y_out", [1024, 1024])
    st_out = dout("st_out", [depth, 2, 4, 4096])
    if dbg:
        dbg_x = dout("dbg_x", [1024, 1024])
        dbg_a = dout("dbg_a", [128, 8192])

    with ExitStack() as es:
        fw = FW(nc, es)
        fw.skipset = set(skip)
        op, dma = fw.op, fw.dma

        x = fw.sb("x", [128, 8, 1024])
        hT = fw.sb("hT", [128, 8, 1024], BF16)
        identf = fw.sb("identf", [128, 128])
        identb = fw.sb("identb", [128, 128], BF16)
        onesf = fw.sb("onesf", [128, 128])
        onesb = fw.sb("onesb", [128, 128], BF16)
        g1b = fw.sb("g1b", [128, 1024])
        g2b = fw.sb("g2b", [128, 1024])
        modcol = fw.sb("modcol", [128, 48])
        gscol = fw.sb("gscol", [128, 16])
        scol = fw.sb("scol", [128, 8], BF16)
        small = fw.sb("small", [128, 64])
        flag_sb = fw.sb("flag_sb", [128, 1])
        kcol_sb = fw.sb("kcol_sb", [128, 4])
        PS = [fw.ps(f"ps{i}", [128, 512]) for i in range(8)]
        OUTB = fw.view("y_out", y_out)
        STB = fw.view("st_out", st_out)

        op(POOL, lambda: G_.memset(identf[:], 1.0), writes=[identf])
        op(POOL, lambda: G_.affine_select(identf[:], identf[:], pattern=[[-1, 128]], compare_op=ALU.is_equal,
                                           fill=0.0, base=0, channel_multiplier=1), reads=[identf], writes=[identf])
        op(DVE, lambda: V.tensor_copy(identb[:], identf[:]), reads=[identf], writes=[identb])
        op(POOL, lambda: G_.memset(onesf[:], 1.0), writes=[onesf])
        op(POOL, lambda: G_.memset(onesb[:], 1.0), writes=[onesb])
        dma(SP, flag_sb[:], flag, writes=[flag_sb])
        dma(SP, kcol_sb[:], kcol, writes=[kcol_sb])

        with ExitStack() as sub:
            ptmp = fw.sb("ptmp", [128, 8, 1024], es=sub)
            for tt in range(8):
                dma(SP, x[:, tt, :], xin[tt * 128:(tt + 1) * 128, :], writes=[x])
                dma(ACT, ptmp[:, tt, :], pos[tt * 128:(tt + 1) * 128, :], writes=[ptmp])
            op(DVE, lambda: V.tensor_tensor(out=x[:], in0=x[:], in1=ptmp[:], op=ALU.add), reads=[x, ptmp], writes=[x])
            ctmp = fw.sb("ctmp", [128, 8], es=sub)
            csig = fw.sb("csig", [128, 8], es=sub)
            dma(SP, ctmp[:], condT, writes=[ctmp])
            op(ACT, lambda: S.activation(out=csig[:], in_=ctmp[:], func=AF.Sigmoid), reads=[ctmp], writes=[csig])
            op(DVE, lambda: V.tensor_tensor(out=scol[:], in0=ctmp[:], in1=csig[:], op=ALU.mult), reads=[ctmp, csig], writes=[scol])
            fw.barrier()

        def norm_transpose(sub, gs_off, sh_off, router=None):
            xn = fw.sb("xn", [128, 1024], es=sub)
            junk = fw.sb("junk", [128, 1024], es=sub)
            ss = fw.sb("ss", [128, 8], es=sub)
            rstd = fw.sb("rstd", [128, 8], es=sub)
            hf = fw.sb("hf", [128, 8, 128], es=sub) if router is not None else None
            op(DVE, lambda: V.memset(ss[:], 0.0), writes=[ss])
            for tt in range(8):
                op(ACT, lambda: S.activation(out=junk[:], in_=x[:, tt, :], func=AF.Square, accum_out=ss[:, tt:tt + 1]),
                   reads=[x], writes=[junk, ss])
                op(DVE, lambda: V.tensor_scalar(out=rstd[:, tt:tt + 1], in0=ss[:, tt:tt + 1], scalar1=1.0 / 1024.0, scalar2=EPS,
                                                op0=ALU.mult, op1=ALU.add), reads=[ss], writes=[rstd])
                op(ACT, lambda: S.sqrt(out=rstd[:, tt:tt + 1], in_=rstd[:, tt:tt + 1]), reads=[rstd], writes=[rstd])
                op(DVE, lambda: V.reciprocal(out=rstd[:, tt:tt + 1], in_=rstd[:, tt:tt + 1]), reads=[rstd], writes=[rstd])
                op(ACT, lambda: S.activation(out=xn[:], in_=x[:, tt, :], func=AF.Copy, scale=rstd[:, tt:tt + 1]),
                   reads=[x, rstd], writes=[xn])
                for half in range(2):
                    pa = PS[half]
                    for kk in range(4):
                        k = half * 4 + kk
                        op(PE, lambda: T_.transpose(pa[:, kk * 128:(kk + 1) * 128], xn[:, k * 128:(k + 1) * 128], identf[:]),
                           reads=[xn, identf], writes=[pa], sig=(kk == 3))
                    for kk in range(4):
                        k = half * 4 + kk
                        op(ACT, lambda: S.activation(out=hT[:, k, tt * 128:(tt + 1) * 128], in_=pa[:, kk * 128:(kk + 1) * 128],
                                                     func=AF.Identity, scale=gscol[:, gs_off + k:gs_off + k + 1],
                                                     bias=modcol[:, sh_off + k:sh_off + k + 1]),
                           reads=[pa, gscol, modcol], writes=[hT])
                        if router is not None:
                            op(ACT, lambda: S.activation(out=hf[:, k, :], in_=pa[:, kk * 128:(kk + 1) * 128],
                                                         func=AF.Identity, scale=gscol[:, gs_off + k:gs_off + k + 1],
                                                         bias=modcol[:, sh_off + k:sh_off + k + 1]),
                               reads=[pa, gscol, modcol], writes=[hf])
                if router is not None:
                    router(tt, hf)

        for L in range(depth):
            with ExitStack() as sub:
                modrow = fw.sb("modrow", [1, 6144], es=sub)
                bmrow = fw.sb("bmrow", [1, 6144], es=sub)
                ngT = fw.sb("ngT", [128, 16], es=sub)
                wm = [fw.sb(f"wm{i}", [128, 8, 512], BF16, es=sub) for i in range(2)]
                dma(SP, bmrow[:], b_mod[L], writes=[bmrow])
                dma(SP, ngT[:, 0:8], n1gT[L], writes=[ngT])
                dma(SP, ngT[:, 8:16], n2gT[L], writes=[ngT])
                for n in range(12):
                    wb = wm[n % 2]
                    dma(POOL, wb[:], w_mod[L][:, n * 512:(n + 1) * 512].rearrange("(k p) n -> p k n", p=128), writes=[wb])
                    pa = PS[n % 2]
                    for k in range(8):
                        op(PE, lambda: T_.matmul(pa[0:1, :], lhsT=scol[:, k:k + 1], rhs=wb[:, k, :], start=(k == 0), stop=(k == 7)),
                           reads=[scol, wb], writes=[pa], sig=(k == 7))
                    op(DVE, lambda: V.tensor_tensor(out=modrow[0:1, n * 512:(n + 1) * 512], in0=pa[0:1, :],
                                                    in1=bmrow[0:1, n * 512:(n + 1) * 512], op=ALU.add),
                       reads=[pa, bmrow], writes=[modrow])
                pa = PS[2]
                for j in range(48):
                    op(PE, lambda: T_.matmul(pa[:, j:j + 1], lhsT=modrow[0:1, j * 128:(j + 1) * 128], rhs=onesf[0:1, 0:1],
                                             start=True, stop=True), reads=[modrow, onesf], writes=[pa], sig=(j == 47))
                op(DVE, lambda: V.tensor_copy(modcol[:], pa[:, 0:48]), reads=[pa], writes=[modcol])
                for i, m in enumerate((1, 4)):
                    op(DVE, lambda: V.scalar_tensor_tensor(out=gscol[:, i * 8:(i + 1) * 8], in0=modcol[:, m * 8:(m + 1) * 8], scalar=1.0,
                                                           in1=ngT[:, i * 8:(i + 1) * 8], op0=ALU.add, op1=ALU.mult),
                       reads=[modcol, ngT], writes=[gscol])
                for gb, m in ((g1b, 2), (g2b, 5)):
                    for hh in range(2):
                        pb = PS[3 + hh]
                        op(PE, lambda: T_.matmul(pb[:], lhsT=onesf[0:1, :], rhs=modrow[0:1, m * 1024 + hh * 512: m * 1024 + (hh + 1) * 512],
                                                 start=True, stop=True), reads=[onesf, modrow], writes=[pb])
                        op(DVE, lambda: V.tensor_copy(gb[:, hh * 512:(hh + 1) * 512], pb[:]), reads=[pb], writes=[gb])
                fw.barrier()

            with ExitStack() as sub:
                norm_transpose(sub, 0, 0)
                fw.barrier()

            with ExitStack() as mix:
                ys2T = fw.sb("ys2T", [128, 4, 1024], BF16, es=mix)
                binT = fw.sb("binT", [128, 40], es=mix)
                dma(SP, binT[:], b_inT[L], writes=[binT])

                fw.mute = "s5" in skip
                with ExitStack() as sub:
                    Zc = fw.sb("Zc", [128, 32, 128], BF16, es=sub)
                    with ExitStack() as sz:
                        ws = fw.sb("ws", [128, 8, 512], BF16, es=sz)
                        bzs = fw.sb("bzs", [128, 512], es=sz)
                        dma(SP, bzs[:], b_zs[L].partition_broadcast(128), writes=[bzs])
                        dma(POOL, ws[:], w_in[L][:, 1536:2048].rearrange("(k p) n -> p k n", p=128), writes=[ws])
                        for j in range(8):
                            pa = PS[4 + j % 2]
                            for k in range(8):
                                op(PE, lambda: T_.matmul(pa[:], lhsT=hT[:, k, j::8], rhs=ws[:, k, :], start=(k == 0), stop=(k == 7)),
                                   reads=[ws, hT], writes=[pa], sig=(k == 7))
                            op(DVE, lambda: V.tensor_tensor(out=Zc[:].rearrange("c g (j h) -> c g j h", h=16)[:, :, j, :],
                                                            in0=pa[:].rearrange("c (g h) -> c g h", h=16),
                                                            in1=bzs[:].rearrange("c (g h) -> c g h", h=16), op=ALU.add),
                               reads=[pa, bzs], writes=[Zc])
                        fw.barrier()
                    s5_block(fw, nc, sub, L, PS, Zc, ys2T, identb, identf, onesb, kcol_sb,
                             dict(lam_row=lam_row, lamT=lamT, BT2=BT2, CT2=CT2, dcolrep=dcolrep, jvals=jvals, dmask=dmask,
                                  tri=tri, msel=msel, h0row=h0row, w_glu=w_glu, b_gluT=b_gluT), STB, st_out)
                    fw.barrier()

                fw.mute = "stageA" in skip
                mixT = fw.sb("mixT", [128, 4, 1024], BF16, es=mix)
                convT = fw.sb("convT", [128, 4, 1024], BF16, es=mix)

                with ExitStack() as sub:
                    wi = [fw.sb(f"wi{i}", [128, 8, 512], BF16, es=sub) for i in range(2)]
                    zfT = fw.sb("zfT", [128, 4, 1024], BF16, es=sub)
                    vpad = fw.sb("vpad", [128, 4, 4, 286], es=sub)
                    sig = fw.sb("sig", [128, 512], es=sub)
                    op(POOL, lambda: G_.memset(vpad[:], 0.0), writes=[vpad])

                    def load_wi(piece):
                        wb = wi[piece % 2]
                        dma(POOL, wb[:], w_in[L][:, piece * 512:(piece + 1) * 512].rearrange("(k p) n -> p k n", p=128), writes=[wb])
                        return wb

                    wb = load_wi(0)
                    for cc in range(4):
                        for th in range(2):
                            pa = PS[(cc * 2 + th) % 2]
                            for k in range(8):
                                op(PE, lambda: T_.matmul(pa[:], lhsT=wb[:, k, cc * 128:(cc + 1) * 128], rhs=hT[:, k, th * 512:(th + 1) * 512],
                                                         start=(k == 0), stop=(k == 7)), reads=[wb, hT], writes=[pa], sig=(k == 7))
                            op(ACT, lambda: S.activation(out=zfT[:, cc, th * 512:(th + 1) * 512], in_=pa[:], func=AF.Identity,
                                                         bias=binT[:, cc:cc + 1]), reads=[pa, binT], writes=[zfT])
                    wa = load_wi(1)
                    wbb = load_wi(2)
                    for q in range(4):
                        for th in range(2):
                            pa, pb = PS[2], PS[3]
                            for k in range(8):
                                op(PE, lambda: T_.matmul(pa[:], lhsT=wa[:, k, q * 128:(q + 1) * 128], rhs=hT[:, k, th * 512:(th + 1) * 512],
                                                         start=(k == 0), stop=(k == 7)), reads=[wa, hT], writes=[pa], sig=(k == 7))
                            for k in range(8):
                                op(PE, lambda: T_.matmul(pb[:], lhsT=wbb[:, k, q * 128:(q + 1) * 128], rhs=hT[:, k, th * 512:(th + 1) * 512],
                                                         start=(k == 0), stop=(k == 7)), reads=[wbb, hT], writes=[pb], sig=(k == 7))
                            op(ACT, lambda: S.activation(out=sig[:], in_=pb[:], func=AF.Sigmoid, bias=binT[:, 8 + q:9 + q]),
                               reads=[pb, binT], writes=[sig])
                            op(DVE, lambda: V.scalar_tensor_tensor(
                                out=vpad[:, q, th * 2:(th + 1) * 2, 15:271], in0=pa[:].rearrange("p (s t) -> p s t", s=2),
                                scalar=binT[:, 4 + q:5 + q], in1=sig[:].rearrange("p (s t) -> p s t", s=2), op0=ALU.add, op1=ALU.mult),
                               reads=[pa, sig, binT], writes=[vpad])

                    with ExitStack() as sf:
                        CL = fw.sb("CL", [128, 8, 1024], BF16, es=sf)
                        SL = fw.sb("SL", [128, 8, 1024], BF16, es=sf)
                        ccs = fw.sb("ccs", [128, 256], BF16, es=sf)
                        Ucs = fw.sb("Ucs", [128, 8, 1024], BF16, es=sf)
                        dma(POOL, ccs[:], ccsc, writes=[ccs])
                        for h2 in range(2):
                            dma(POOL, CL[:, h2 * 4:(h2 + 1) * 4, :], CLt[h2 * 512:(h2 + 1) * 512, :].rearrange("(k p) n -> p k n", p=128), writes=[CL])
                            dma(POOL, SL[:, h2 * 4:(h2 + 1) * 4, :], SLt[h2 * 512:(h2 + 1) * 512, :].rearrange("(k p) n -> p k n", p=128), writes=[SL])
                        for tt in range(8):
                            for gh in range(2):
                                pa = PS[gh]
                                for gg in range(2):
                                    g = gh * 2 + gg
                                    op(PE, lambda: T_.matmul(pa[:, gg * 256:(gg + 1) * 256], lhsT=zfT[:, g, tt * 128:(tt + 1) * 128], rhs=ccs[:],
                                                             start=True, stop=True), reads=[zfT, ccs], writes=[pa], sig=(gg == 1))
                                op(ACT if gh else DVE,
                                   (lambda: S.copy(out=Ucs[:, tt, gh * 512:(gh + 1) * 512], in_=pa[:])) if gh else
                                   (lambda: V.tensor_copy(Ucs[:, tt, gh * 512:(gh + 1) * 512], pa[:])),
                                   reads=[pa], writes=[Ucs])
                        for g in range(4):
                            for th in range(2):
                                pa = PS[2 + (g * 2 + th) % 2]
                                for lc in range(8):
                                    op(PE, lambda: T_.matmul(pa[:], lhsT=Ucs[:, lc, g * 256:g * 256 + 128], rhs=CL[:, lc, th * 512:(th + 1) * 512],
                                                             start=(lc == 0), stop=False), reads=[Ucs, CL], writes=[pa], sig=False)
                                    op(PE, lambda: T_.matmul(pa[:], lhsT=Ucs[:, lc, g * 256 + 128:g * 256 + 256], rhs=SL[:, lc, th * 512:(th + 1) * 512],
                                                             start=False, stop=(lc == 7)), reads=[Ucs, SL], writes=[pa], sig=(lc == 7))
                                op(ACT, lambda: S.copy(out=mixT[:, g, th * 512:(th + 1) * 512], in_=pa[:]), reads=[pa], writes=[mixT])
                        fw.barrier()

                    with ExitStack() as sc:
                        cw = fw.sb("cw", [128, 4, 31], es=sc)
                        cv = fw.sb("cv", [128, 3, 4], es=sc)
                        yc = fw.sb("yc", [128, 4, 1024], es=sc)
                        ysq = fw.sb("ysq", [128, 1024], es=sc)
                        mean = fw.sb("mean", [128, 1024], es=sc)
                        rs = fw.sb("rs", [128, 1024], es=sc)
                        t1 = fw.sb("t1", [128, 1024], es=sc)
                        t2 = fw.sb("t2", [128, 1024], es=sc)
                        dma(SP, cw[:], cdwT[L], writes=[cw])
                        dma(SP, cv[:], cvec[L], writes=[cv])
                        for q in range(4):
                            op(DVE, lambda: V.tensor_scalar(out=vpad[:, q, 1:4, 0:15], in0=vpad[:, q, 0:3, 256:271], scalar1=flag_sb[:, 0:1],
                                                            scalar2=None, op0=ALU.mult), reads=[vpad, flag_sb], writes=[vpad])
                            op(DVE, lambda: V.tensor_scalar(out=vpad[:, q, 0:3, 271:286], in0=vpad[:, q, 1:4, 15:30], scalar1=flag_sb[:, 0:1],
                                                            scalar2=None, op0=ALU.mult), reads=[vpad, flag_sb], writes=[vpad])
                            yv = yc[:, q, :].rearrange("p (s t) -> p s t", s=4)
                            op(DVE, lambda: V.tensor_scalar(out=yv, in0=vpad[:, q, :, 0:256], scalar1=cw[:, q, 0:1], scalar2=cv[:, 0, q:q + 1],
                                                            op0=ALU.mult, op1=ALU.add), reads=[vpad, cw, cv], writes=[yc])
                            for k in range(1, 31):
                                op(DVE, lambda: V.scalar_tensor_tensor(out=yv, in0=vpad[:, q, :, k:k + 256], scalar=cw[:, q, k:k + 1], in1=yv,
                                                                       op0=ALU.mult, op1=ALU.add), reads=[vpad, cw, yc], writes=[yc])
                        for th in range(2):
                            pa, pb = PS[th], PS[2 + th]
                            for q in range(4):
                                op(ACT, lambda: S.activation(out=ysq[:, 0:512], in_=yc[:, q, th * 512:(th + 1) * 512], func=AF.Square),
                                   reads=[yc], writes=[ysq])
                                op(PE, lambda: T_.matmul(pa[:], lhsT=onesf[:], rhs=yc[:, q, th * 512:(th + 1) * 512], start=(q == 0), stop=(q == 3)),
                                   reads=[onesf, yc], writes=[pa], sig=True)
                                op(PE, lambda: T_.matmul(pb[:], lhsT=onesf[:], rhs=ysq[:, 0:512], start=(q == 0), stop=(q == 3)),
                                   reads=[onesf, ysq], writes=[pb], sig=True)
                            sl = slice(th * 512, (th + 1) * 512)
                            op(DVE, lambda: V.tensor_scalar(out=mean[:, sl], in0=pa[:], scalar1=1.0 / 512.0, scalar2=None, op0=ALU.mult),
                               reads=[pa], writes=[mean])
                            op(DVE, lambda: V.tensor_tensor(out=t1[:, sl], in0=mean[:, sl], in1=mean[:, sl], op=ALU.mult), reads=[mean], writes=[t1])
                            op(DVE, lambda: V.scalar_tensor_tensor(out=rs[:, sl], in0=pb[:], scalar=1.0 / 512.0, in1=t1[:, sl],
                                                                   op0=ALU.mult, op1=ALU.subtract), reads=[pb, t1], writes=[rs])
                            op(DVE, lambda: V.tensor_scalar(out=rs[:, sl], in0=rs[:, sl], scalar1=EPS, scalar2=None, op0=ALU.add),
                               reads=[rs], writes=[rs])
                            op(ACT, lambda: S.sqrt(out=rs[:, sl], in_=rs[:, sl]), reads=[rs], writes=[rs])
                            op(DVE, lambda: V.reciprocal(out=rs[:, sl], in_=rs[:, sl]), reads=[rs], writes=[rs])
                        for q in range(4):
                            op(DVE, lambda: V.tensor_tensor(out=t1[:], in0=yc[:, q, :], in1=mean[:], op=ALU.subtract), reads=[yc, mean], writes=[t1])
                            op(DVE, lambda: V.tensor_tensor(out=t1[:], in0=t1[:], in1=rs[:], op=ALU.mult), reads=[t1, rs], writes=[t1])
                            op(DVE, lambda: V.tensor_scalar(out=t1[:], in0=t1[:], scalar1=cv[:, 1, q:q + 1], scalar2=cv[:, 2, q:q + 1],
                                                            op0=ALU.mult, op1=ALU.add), reads=[t1, cv], writes=[t1])
                            op(ACT, lambda: S.activation(out=t2[:], in_=t1[:], func=AF.Sigmoid), reads=[t1], writes=[t2])
                            op(DVE, lambda: V.tensor_tensor(out=convT[:, q, :], in0=t1[:], in1=t2[:], op=ALU.mult), reads=[t1, t2], writes=[convT])
                        fw.barrier()
                    fw.barrier()

                fw.mute = "stageC" in skip
                with ExitStack() as sub:
                    wbr = [fw.sb(f"wbr{i}", [128, 4, 1024], BF16, es=sub) for i in range(3)]
                    wg = [fw.sb(f"wg{i}", [128, 8, 3, 128], BF16, es=sub) for i in range(2)]
                    wo = fw.sb("wo", [128, 8, 1024], BF16, es=sub)
                    mixedT = fw.sb("mixedT", [128, 8, 1024], BF16, es=sub)
                    sg = [fw.sb(f"sg{i}", [128, 512], es=sub) for i in range(3)]
                    acc = fw.sb("accm", [128, 512], es=sub)
                    tmp = fw.sb("tmpm", [128, 512], es=sub)
                    for i, wsrc in enumerate((w_four, w_conv_out, w_ssm_out)):
                        dma(POOL, wbr[i][:], wsrc[L].rearrange("(k p) n -> p k n", p=128), writes=[wbr[i]])
                    for h2 in range(2):
                        dma(POOL, wo[:, h2 * 4:(h2 + 1) * 4, :], w_out[L][h2 * 512:(h2 + 1) * 512, :].rearrange("(k p) n -> p k n", p=128), writes=[wo])
                    brT = (mixT, convT, ys2T)
                    for dc in range(8):
                        wgb = wg[dc % 2]
                        for b in range(3):
                            c0 = 2048 + b * 1024 + dc * 128
                            dma(POOL, wgb[:, :, b, :], w_in[L][:, c0:c0 + 128].rearrange("(k p) n -> p k n", p=128), writes=[wgb])
                        for th in range(2):
                            ts = slice(th * 512, (th + 1) * 512)
                            for b in range(3):
                                pg = PS[b]
                                for k in range(8):
                                    op(PE, lambda: T_.matmul(pg[:], lhsT=wgb[:, k, b, :], rhs=hT[:, k, ts], start=(k == 0), stop=(k == 7)),
                                       reads=[wgb, hT], writes=[pg], sig=(k == 7))
                                op(ACT, lambda: S.activation(out=sg[b][:], in_=pg[:], func=AF.Sigmoid, bias=binT[:, 16 + b * 8 + dc:17 + b * 8 + dc]),
                                   reads=[pg, binT], writes=[sg[b]])
                                pb = PS[3 + b]
                                for k in range(4):
                                    op(PE, lambda: T_.matmul(pb[:], lhsT=wbr[b][:, k, dc * 128:(dc + 1) * 128], rhs=brT[b][:, k, ts],
                                                             start=(k == 0), stop=(k == 3)), reads=[wbr[b], brT[b]], writes=[pb], sig=(k == 3))
                                if b == 0:
                                    op(DVE, lambda: V.tensor_tensor(out=acc[:], in0=pb[:], in1=sg[b][:], op=ALU.mult), reads=[pb, sg[b]], writes=[acc])
                                else:
                                    op(DVE, lambda: V.tensor_tensor(out=tmp[:], in0=pb[:], in1=sg[b][:], op=ALU.mult), reads=[pb, sg[b]], writes=[tmp])
                                    if b == 1:
                                        op(DVE, lambda: V.tensor_tensor(out=acc[:], in0=acc[:], in1=tmp[:], op=ALU.add), reads=[acc, tmp], writes=[acc])
                                    else:
                                        op(DVE, lambda: V.tensor_tensor(out=mixedT[:, dc, ts], in0=acc[:], in1=tmp[:], op=ALU.add),
                                           reads=[acc, tmp], writes=[mixedT])
                    for tt in range(8):
                        for dh in range(2):
                            pa = PS[6 + dh]
                            ds_ = slice(dh * 512, (dh + 1) * 512)
                            for k in range(8):
                                op(PE, lambda: T_.matmul(pa[:], lhsT=mixedT[:, k, tt * 128:(tt + 1) * 128], rhs=wo[:, k, ds_], start=(k == 0), stop=(k == 7)),
                                   reads=[mixedT, wo], writes=[pa], sig=(k == 7))
                            op(DVE, lambda: V.tensor_tensor(out=tmp[:], in0=pa[:], in1=g1b[:, ds_], op=ALU.mult), reads=[pa, g1b], writes=[tmp])
                            op(DVE, lambda: V.tensor_tensor(out=x[:, tt, ds_], in0=x[:, tt, ds_], in1=tmp[:], op=ALU.add), reads=[x, tmp], writes=[x])
                    fw.barrier()
                fw.barrier()

            fw.mute = False
            if dbg and L == 0:
                DX = fw.view("dbg_x", dbg_x)
                for tt in range(8):
                    dma(SP, dbg_x[tt * 128:(tt + 1) * 128, :], x[:, tt, :], reads=[x], writes=[DX])

            fw.mute = "moe" in skip
            with ExitStack() as moe:
                moe_block(fw, nc, moe, L, PS, x, hT, g2b, identf, norm_transpose,
                          dict(router_w=router_w, router_b=router_b, w_gu=w_gu, b_guT=b_guT, w_dn=w_dn, b_dn=b_dn))
                fw.barrier()

        fw.mute = False
        with ExitStack() as sub:
            fg = fw.sb("fg", [128, 1024], es=sub)
            junk = fw.sb("junkf", [128, 1024], es=sub)
            ss = fw.sb("ssf", [128, 8], es=sub)
            yo = [fw.sb(f"yo{i}", [128, 1024], es=sub) for i in range(2)]
            dma(SP, fg[:], fin_g.partition_broadcast(128), writes=[fg])
            op(DVE, lambda: V.memset(ss[:], 0.0), writes=[ss])
            for tt in range(8):
                op(ACT, lambda: S.activation(out=junk[:], in_=x[:, tt, :], func=AF.Square, accum_out=ss[:, tt:tt + 1]), reads=[x], writes=[junk, ss])
                op(DVE, lambda: V.tensor_scalar(out=ss[:, tt:tt + 1], in0=ss[:, tt:tt + 1], scalar1=1.0 / 1024.0, scalar2=EPS, op0=ALU.mult, op1=ALU.add),
                   reads=[ss], writes=[ss])
                op(ACT, lambda: S.sqrt(out=ss[:, tt:tt + 1], in_=ss[:, tt:tt + 1]), reads=[ss], writes=[ss])
                op(DVE, lambda: V.reciprocal(out=ss[:, tt:tt + 1], in_=ss[:, tt:tt + 1]), reads=[ss], writes=[ss])
                yb = yo[tt % 2]
                op(DVE, lambda: V.scalar_tensor_tensor(out=yb[:], in0=x[:, tt, :], scalar=ss[:, tt:tt + 1], in1=fg[:], op0=ALU.mult, op1=ALU.mult),
                   reads=[x, ss, fg], writes=[yb])
                dma(SP, y_out[tt * 128:(tt + 1) * 128, :], yb[:], reads=[yb], writes=[OUTB])
            fw.barrier()
    return nc


def moe_block(fw, nc, es, L, PS, x, hT, g2b, identf, norm_transpose, W):
    V, S, G_, T_ = nc.vector, nc.scalar, nc.gpsimd, nc.tensor
    op, dma = fw.op, fw.dma
    if "moe_early" in fw.skipset:
        with ExitStack() as sub0:
            norm_transpose(sub0, 8, 24, router=None)
            fw.barrier()
    comb = fw.sb("comb", [128, 8, 32], es=es)
    rw = fw.sb("rw", [128, 8, 32], es=es)
    rb = fw.sb("rb", [128, 32], es=es)
    lg = fw.sb("lg", [128, 32], es=es)
    ex = fw.sb("ex", [128, 32], es=es)
    mk = fw.sb("mk", [128, 32], es=es)
    m8 = fw.sb("m8", [128, 8], es=es)
    sm = fw.sb("sm", [128, 4], es=es)
    fw.mute = ("moe" in fw.skipset) or ("moe_loads" in fw.skipset)
    dma(SP, rw[:], W["router_w"][L].rearrange("(k p) e -> p k e", p=128), writes=[rw])
    dma(SP, rb[:], W["router_b"][L].partition_broadcast(128), writes=[rb])
    fw.barrier()
    fw.mute = "moe" in fw.skipset

    def router(tt, hf):
        pl = PS[6]
        for k in range(8):
            op(PE, lambda: T_.matmul(pl[:, 0:32], lhsT=hf[:, k, :], rhs=rw[:, k, :], start=(k == 0), stop=(k == 7)),
               reads=[hf, rw], writes=[pl], sig=(k == 7))
        op(DVE, lambda: V.tensor_tensor(out=lg[:], in0=pl[:, 0:32], in1=rb[:], op=ALU.add), reads=[pl, rb], writes=[lg])
        if "moe_rt_mm_only" in fw.skipset:
            return
        op(DVE, lambda: V.max(m8[:], lg[:]), reads=[lg], writes=[m8])
        op(DVE, lambda: V.tensor_scalar(out=sm[:, 0:1], in0=m8[:, 0:1], scalar1=-1.0, scalar2=None, op0=ALU.mult), reads=[m8], writes=[sm])
        op(ACT, lambda: S.activation(out=ex[:], in_=lg[:], func=AF.Exp, bias=sm[:, 0:1]), reads=[lg, sm], writes=[ex])
        op(DVE, lambda: V.tensor_scalar(out=mk[:], in0=lg[:], scalar1=m8[:, 3:4], scalar2=None, op0=ALU.is_ge), reads=[lg, m8], writes=[mk])
        op(DVE, lambda: V.tensor_tensor(out=ex[:], in0=ex[:], in1=mk[:], op=ALU.mult), reads=[ex, mk], writes=[ex])
        op(DVE, lambda: V.reduce_sum(out=sm[:, 1:2], in_=ex[:], axis=AX.X), reads=[ex], writes=[sm])
        op(DVE, lambda: V.reciprocal(out=sm[:, 2:3], in_=sm[:, 1:2]), reads=[sm], writes=[sm])
        op(DVE, lambda: V.tensor_scalar(out=comb[:, tt, :], in0=ex[:], scalar1=sm[:, 2:3], scalar2=None, op0=ALU.mult), reads=[ex, sm], writes=[comb])

    with ExitStack() as sub:
        fw.mute = "moe_router" in fw.skipset
        norm_transpose(sub, 8, 24, router=(None if "moe_norouter" in fw.skipset else router))
        fw.barrier()
        fw.mute = "moe" in fw.skipset

    acc = fw.sb("acc", [128, 8, 1024], es=es)
    bgu = fw.sb("bgu", [128, 512], es=es)
    bgu1 = fw.sb("bgu1", [128, 512], es=es)
    bdn = fw.sb("bdn", [32, 1024], es=es)
    combT = fw.sb("combT", [32, 1024], es=es)
    fw.mute = ("moe" in fw.skipset) or ("moe_loads" in fw.skipset)
    dma(SP, bgu[:], W["b_guT"][L], writes=[bgu])
    dma(SP, bdn[:], W["b_dn"][L], writes=[bdn])
    op(DVE, lambda: V.tensor_scalar(out=bgu1[:], in0=bgu[:], scalar1=1.0, scalar2=None, op0=ALU.add), reads=[bgu], writes=[bgu1])
    fw.mute = "moe_init" in fw.skipset
    for half in range(2):
        pa = PS[half]
        for t4 in range(4):
            tt = half * 4 + t4
            op(PE, lambda: T_.transpose(pa[0:32, t4 * 128:(t4 + 1) * 128], comb[:, tt, :], identf[:]), reads=[comb, identf], writes=[pa], sig=(t4 == 3))
        op(DVE, lambda: V.tensor_copy(combT[:, half * 512:(half + 1) * 512], pa[0:32, :]), reads=[pa], writes=[combT])
    for tt in range(8):
        for dh in range(2):
            pa = PS[2 + dh]
            op(PE, lambda: T_.matmul(pa[:], lhsT=combT[:, tt * 128:(tt + 1) * 128], rhs=bdn[:, dh * 512:(dh + 1) * 512], start=True, stop=True),
               reads=[combT, bdn], writes=[pa])
            op(ACT, lambda: S.copy(out=acc[:, tt, dh * 512:(dh + 1) * 512], in_=pa[:]), reads=[pa], writes=[acc])

    fw.mute = "moe" in fw.skipset
    wgu = [fw.sb(f"wgu{i}", [128, 8, 2, 256], BF16, es=es) for i in range(3)]
    wdn = [fw.sb(f"wdn{i}", [128, 8, 1024], BF16, es=es) for i in range(2)]
    actT = [fw.sb(f"actT{i}", [128, 8, 1024], BF16, es=es) for i in range(2)]
    gm = [fw.sb(f"gm{i}", [128, 512], es=es) for i in range(2)]
    sg = [fw.sb(f"sgm{i}", [128, 512], es=es) for i in range(2)]
    um = [fw.sb(f"um{i}", [128, 512], es=es) for i in range(2)]
    cnt = [0]

    def GU(e):
        at = actT[e % 2]
        wd = wdn[e % 2]
        for pc in range(4):
            wb = wgu[cnt[0] % 3]
            cnt[0] += 1
            for two in range(2):
                c0 = two * 1024 + pc * 256
                dma(POOL, wb[:, :, two, :], W["w_gu"][L, e][:, c0:c0 + 256].rearrange("(k p) n -> p k n", p=128), writes=[wb])
            if pc == 1:
                for h2 in range(2):
                    dma(POOL, wd[:, h2 * 4:(h2 + 1) * 4, :], W["w_dn"][L, e][h2 * 512:(h2 + 1) * 512, :].rearrange("(k p) n -> p k n", p=128), writes=[wd])
            for fl in range(2):
                fc = pc * 2 + fl
                for th in range(2):
                    u = (fc * 2 + th) % 2
                    ts = slice(th * 512, (th + 1) * 512)
                    pg, pu = PS[u], PS[2 + u]
                    for k in range(8):
                        op(PE, lambda: T_.matmul(pg[:], lhsT=wb[:, k, 0, fl * 128:(fl + 1) * 128], rhs=hT[:, k, ts], start=(k == 0), stop=(k == 7)),
                           reads=[wb, hT], writes=[pg], sig=(k == 7))
                    for k in range(8):
                        op(PE, lambda: T_.matmul(pu[:], lhsT=wb[:, k, 1, fl * 128:(fl + 1) * 128], rhs=hT[:, k, ts], start=(k == 0), stop=(k == 7)),
                           reads=[wb, hT], writes=[pu], sig=(k == 7))
                    cg = e * 16 + fc
                    op(DVE, lambda: V.tensor_scalar(out=gm[u][:], in0=pg[:], scalar1=bgu[:, cg:cg + 1], scalar2=7.0, op0=ALU.add, op1=ALU.min),
                       reads=[pg, bgu], writes=[gm[u]])
                    op(ACT, lambda: S.activation(out=sg[u][:], in_=gm[u][:], func=AF.Sigmoid, scale=1.702), reads=[gm[u]], writes=[sg[u]])
                    op(DVE, lambda: V.tensor_scalar(out=um[u][:], in0=pu[:], scalar1=bgu1[:, cg + 8:cg + 9], scalar2=8.0, op0=ALU.add, op1=ALU.min),
                       reads=[pu, bgu1], writes=[um[u]])
                    op(DVE, lambda: V.tensor_tensor(out=gm[u][:], in0=gm[u][:], in1=sg[u][:], op=ALU.mult), reads=[gm[u], sg[u]], writes=[gm[u]])
                    op(DVE, lambda: V.scalar_tensor_tensor(out=at[:, fc, ts], in0=um[u][:], scalar=-6.0, in1=gm[u][:], op0=ALU.max, op1=ALU.mult),
                       reads=[um[u], gm[u]], writes=[at])

    def DN(e):
        at = actT[e % 2]
        wd = wdn[e % 2]
        for tt in range(8):
            for dh in range(2):
                pa = PS[4 + (tt * 2 + dh) % 2]
                ds_ = slice(dh * 512, (dh + 1) * 512)
                for f in range(8):
                    op(PE, lambda: T_.matmul(pa[:], lhsT=at[:, f, tt * 128:(tt + 1) * 128], rhs=wd[:, f, ds_], start=(f == 0), stop=(f == 7)),
                       reads=[at, wd], writes=[pa], sig=(f == 7))
                op(DVE, lambda: V.scalar_tensor_tensor(out=acc[:, tt, ds_], in0=pa[:], scalar=comb[:, tt, e:e + 1], in1=acc[:, tt, ds_],
                                                       op0=ALU.mult, op1=ALU.add), reads=[pa, comb, acc], writes=[acc])

    fw.mute = "moe_exp" in fw.skipset
    GU(0)
    for e in range(NEXP_RUN[0]):
        if e + 1 < NEXP_RUN[0]:
            GU(e + 1)
        DN(e)
    fw.mute = ("moe" in fw.skipset) or ("moe_fin" in fw.skipset)
    for tt in range(8):
        op(DVE, lambda: V.tensor_tensor(out=acc[:, tt, :], in0=acc[:, tt, :], in1=g2b[:], op=ALU.mult), reads=[acc, g2b], writes=[acc])
        op(DVE, lambda: V.tensor_tensor(out=x[:, tt, :], in0=x[:, tt, :], in1=acc[:, tt, :], op=ALU.add), reads=[x, acc], writes=[x])


def s5_block(fw, nc, es, L, PS, Zc, ys2T, identb, identf, onesb, kcol_sb, W, STB, st_out):
    V, S, G_, T_ = nc.vector, nc.scalar, nc.gpsimd, nc.tensor
    op, dma = fw.op, fw.dma
    PI = math.pi
    U = fw.sb("U", [128, 32, 128], BF16, es=es)
    ST = [fw.sb(f"ST{r}", [128, 32, 128], BF16, es=es) for r in range(2)]
    ChT = [fw.sb(f"ChT{r}", [128, 32, 128], BF16, es=es) for r in range(2)]
    Dh = [fw.sb(f"Dh{r}", [128, 32, 128], BF16, es=es) for r in range(2)]
    fin = fw.sb("fin", [4, 1024], es=es)
    cst = fw.sb("cst", [128, 8], es=es)
    op(POOL, lambda: G_.memset(cst[:, 0:1], 0.5 * PI), writes=[cst])
    op(POOL, lambda: G_.memset(cst[:, 1:2], 256.0), writes=[cst])
    op(DVE, lambda: V.tensor_scalar(out=cst[:, 2:4], in0=kcol_sb[:, 0:2], scalar1=-1.0, scalar2=None, op0=ALU.mult), reads=[kcol_sb], writes=[cst])
    negpi = cst[:, 0:1]

    def bfview(pb):
        return pb[:].bitcast(BF16)

    def sincos(th, t1, s_out, c_out, ti):
        I2P = 1.0 / TWO_PI
        op(DVE, lambda: V.tensor_scalar(out=ti[1], in0=th[1], scalar1=I2P, scalar2=None, op0=ALU.mult), reads=[th[0]], writes=[ti[0]])
        op(DVE, lambda: V.tensor_copy(t1[1], ti[1]), reads=[ti[0]], writes=[t1[0]])
        op(DVE, lambda: V.scalar_tensor_tensor(out=t1[1], in0=t1[1], scalar=-TWO_PI, in1=th[1], op0=ALU.mult, op1=ALU.add),
           reads=[t1[0], th[0]], writes=[t1[0]])
        op(ACT, lambda: S.activation(out=s_out[1], in_=t1[1], func=AF.Sin), reads=[t1[0]], writes=[s_out[0]])
        op(DVE, lambda: V.tensor_scalar(out=ti[1], in0=th[1], scalar1=I2P, scalar2=0.25, op0=ALU.mult, op1=ALU.add), reads=[th[0]], writes=[ti[0]])
        op(DVE, lambda: V.tensor_copy(t1[1], ti[1]), reads=[ti[0]], writes=[t1[0]])
        op(DVE, lambda: V.scalar_tensor_tensor(out=t1[1], in0=t1[1], scalar=-TWO_PI, in1=th[1], op0=ALU.mult, op1=ALU.add),
           reads=[t1[0], th[0]], writes=[t1[0]])
        op(ACT, lambda: S.activation(out=c_out[1], in_=t1[1], func=AF.Sin, bias=negpi), reads=[t1[0], cst], writes=[c_out[0]])

    for rd in range(4):
        pb = PS[rd % 2]
        pv = bfview(pb)
        for gg in range(8):
            g = rd * 8 + gg
            op(PE, lambda: T_.transpose(pv[:, gg * 128:(gg + 1) * 128], Zc[:, g, :], identb[:]), reads=[Zc, identb], writes=[pb], sig=(gg == 7))
        op(DVE, lambda: V.tensor_copy(U[:, rd * 8:(rd + 1) * 8, :], pv.rearrange("p (g c) -> p g c", c=128)), reads=[pb], writes=[U])

    for r in range(2):
        with ExitStack() as sd:
            Bh = fw.sb("Bh", [128, 32, 128], BF16, es=sd)
            with ExitStack() as st:
                lt = fw.sb("lt", [128, 3, 32], es=st)
                B2 = fw.sb("B2", [128, 2, 512], es=st)
                C2 = fw.sb("C2", [128, 2, 512], es=st)
                jv = fw.sb("jv", [128, 4, 8], es=st)
                mask = fw.sb("mask", [128, 128], es=st)
                dcr = fw.sb("dcr", [128, 32], es=st)
                dma(SP, lt[:], W["lamT"][L, r], writes=[lt])
                dma(SP, B2[:], W["BT2"][L, r], writes=[B2])
                dma(SP, C2[:], W["CT2"][L, r], writes=[C2])
                dma(SP, jv[:], W["jvals"][r], writes=[jv])
                dma(SP, mask[:], W["dmask"][r], writes=[mask])
                dma(SP, dcr[:], W["dcolrep"][L], writes=[dcr])
                aT = fw.sb("aT", [128, 2, 32], es=st)
                dtT = fw.sb("dtT", [128, 32], es=st)
                op(ACT, lambda: S.activation(out=dtT[:], in_=lt[:, 2, :], func=AF.Exp), reads=[lt], writes=[dtT])
                for i in range(2):
                    op(DVE, lambda: V.tensor_tensor(out=aT[:, i, :], in0=lt[:, i, :], in1=dtT[:], op=ALU.mult), reads=[lt, dtT], writes=[aT])
                mg = fw.sb("mg", [128, 32, 8], es=st)
                th = fw.sb("th", [128, 32, 8], es=st)
                tq = fw.sb("tq", [128, 32, 8], es=st)
                sn = fw.sb("sn", [128, 32, 8], es=st)
                cs = fw.sb("cs", [128, 32, 8], es=st)
                tiT = fw.sb("tiT", [128, 32, 8], mybir.dt.int32, es=st)
                Pt = [fw.sb(f"Pt{i}", [128, 2, 32, 8], es=st) for i in range(4)]

                def genT(ei):
                    jb = jv[:, ei, :].unsqueeze(1).to_broadcast([128, 32, 8])
                    ar = aT[:, 0, :].unsqueeze(2).to_broadcast([128, 32, 8])
                    ai = aT[:, 1, :].unsqueeze(2).to_broadcast([128, 32, 8])
                    op(DVE, lambda: V.tensor_tensor(out=mg[:], in0=ar, in1=jb, op=ALU.mult), reads=[aT, jv], writes=[mg])
                    op(ACT, lambda: S.activation(out=mg[:], in_=mg[:], func=AF.Exp), reads=[mg], writes=[mg])
                    op(DVE, lambda: V.tensor_tensor(out=th[:], in0=ai, in1=jb, op=ALU.mult), reads=[aT, jv], writes=[th])
                    sincos((th, th[:]), (tq, tq[:]), (sn, sn[:]), (cs, cs[:]), (tiT, tiT[:]))
                    op(DVE, lambda: V.tensor_tensor(out=Pt[ei][:, 0], in0=mg[:], in1=cs[:], op=ALU.mult), reads=[mg, cs], writes=[Pt[ei]])
                    op(DVE, lambda: V.tensor_tensor(out=Pt[ei][:, 1], in0=mg[:], in1=sn[:], op=ALU.mult), reads=[mg, sn], writes=[Pt[ei]])

                for ei in range(4):
                    genT(ei)
                op(DVE, lambda: V.tensor_tensor(out=Pt[1][:], in0=Pt[1][:], in1=Pt[0][:], op=ALU.subtract), reads=[Pt[1], Pt[0]], writes=[Pt[1]])
                inv = fw.sb("inv", [128, 4, 32], es=st)
                op(DVE, lambda: V.tensor_tensor(out=inv[:, 0, :], in0=lt[:, 0, :], in1=lt[:, 0, :], op=ALU.mult), reads=[lt], writes=[inv])
                op(DVE, lambda: V.tensor_tensor(out=inv[:, 1, :], in0=lt[:, 1, :], in1=lt[:, 1, :], op=ALU.mult), reads=[lt], writes=[inv])
                op(DVE, lambda: V.tensor_tensor(out=inv[:, 0, :], in0=inv[:, 0, :], in1=inv[:, 1, :], op=ALU.add), reads=[inv], writes=[inv])
                op(DVE, lambda: V.reciprocal(out=inv[:, 1, :], in_=inv[:, 0, :]), reads=[inv], writes=[inv])
                op(DVE, lambda: V.tensor_tensor(out=inv[:, 2, :], in0=lt[:, 0, :], in1=inv[:, 1, :], op=ALU.mult), reads=[lt, inv], writes=[inv])
                op(DVE, lambda: V.scalar_tensor_tensor(out=inv[:, 3, :], in0=lt[:, 1, :], scalar=-1.0, in1=inv[:, 1, :], op0=ALU.mult, op1=ALU.mult),
                   reads=[lt, inv], writes=[inv])
                ir = inv[:, 2, :].unsqueeze(2).to_broadcast([128, 32, 8])
                ii = inv[:, 3, :].unsqueeze(2).to_broadcast([128, 32, 8])
                op(DVE, lambda: V.tensor_tensor(out=mg[:], in0=Pt[1][:, 0], in1=ir, op=ALU.mult), reads=[Pt[1], inv], writes=[mg])
                op(DVE, lambda: V.tensor_tensor(out=th[:], in0=Pt[1][:, 1], in1=ii, op=ALU.mult), reads=[Pt[1], inv], writes=[th])
                op(DVE, lambda: V.tensor_tensor(out=Pt[0][:, 0], in0=mg[:], in1=th[:], op=ALU.subtract), reads=[mg, th], writes=[Pt[0]])
                op(DVE, lambda: V.tensor_tensor(out=mg[:], in0=Pt[1][:, 0], in1=ii, op=ALU.mult), reads=[Pt[1], inv], writes=[mg])
                op(DVE, lambda: V.tensor_tensor(out=th[:], in0=Pt[1][:, 1], in1=ir, op=ALU.mult), reads=[Pt[1], inv], writes=[th])
                op(DVE, lambda: V.tensor_tensor(out=Pt[0][:, 1], in0=mg[:], in1=th[:], op=ALU.add), reads=[mg, th], writes=[Pt[0]])

                ta = fw.sb("ta", [128, 8, 8, 16], es=st)
                tb = fw.sb("tb", [128, 8, 8, 16], es=st)
                taL, taH = fw.view("taL", ta.t), fw.view("taH", ta.t)
                tbL, tbH = fw.view("tbL", tb.t), fw.view("tbH", tb.t)
                BhT = fw.sb("BhT", [128, 32, 128], BF16, es=st)
                CDT = fw.sb("CDT", [128, 32, 128], BF16, es=st)

                def cplx_table(out, X, Y, neg):
                    oL, oH = fw.view("oL", out.t), fw.view("oH", out.t)
                    for gh in range(4):
                        gs = slice(gh * 8, (gh + 1) * 8)

                        def xb(ps_, c):
                            return X[ps_, c, gs, :].unsqueeze(3).to_broadcast([64, 8, 8, 16])

                        def yb(ps_, c):
                            return Y[ps_, c, gh * 128:(gh + 1) * 128].rearrange("p (g h) -> p g h", h=16).unsqueeze(2).to_broadcast([64, 8, 8, 16])

                        lo, hi = slice(0, 64), slice(64, 128)
                        ov = out[:].rearrange("p g (j h) -> p g j h", h=16)
                        op(DVE, lambda: V.tensor_tensor(out=ta[lo], in0=xb(lo, 0), in1=yb(lo, 0), op=ALU.mult), reads=[X, Y], writes=[taL])
                        op(DVE, lambda: V.tensor_tensor(out=tb[lo], in0=xb(lo, 1), in1=yb(lo, 1), op=ALU.mult), reads=[X, Y], writes=[tbL])
                        op(DVE, lambda: V.tensor_tensor(out=ov[lo, gs], in0=ta[lo], in1=tb[lo], op=ALU.subtract), reads=[taL, tbL], writes=[oL])
                        op(DVE, lambda: V.tensor_tensor(out=ta[hi], in0=xb(hi, 0), in1=yb(hi, 1), op=ALU.mult), reads=[X, Y], writes=[taH])
                        op(DVE, lambda: V.tensor_tensor(out=tb[hi], in0=xb(hi, 1), in1=yb(hi, 0), op=ALU.mult), reads=[X, Y], writes=[tbH])
                        if neg:
                            op(DVE, lambda: V.scalar_tensor_tensor(out=ov[hi, gs], in0=ta[hi], scalar=-1.0, in1=tb[hi], op0=ALU.mult, op1=ALU.subtract),
                               reads=[taH, tbH], writes=[oH])
                        else:
                            op(DVE, lambda: V.tensor_tensor(out=ov[hi, gs], in0=ta[hi], in1=tb[hi], op=ALU.add), reads=[taH, tbH], writes=[oH])
                    return [oL, oH]

                bh_d = cplx_table(BhT, Pt[0], B2, False)
                ch_d = cplx_table(ChT[r], Pt[2], C2, True)
                cd_d = cplx_table(CDT, Pt[3], C2, True)
                op(DVE, lambda: V.tensor_copy(ChT[r][0:1, 0, 0:1], ChT[r][0:1, 0, 0:1]), reads=ch_d, writes=[ChT[r]])
                for rd in range(4):
                    pb = PS[2 + rd % 2]
                    pv = bfview(pb)
                    for gg in range(8):
                        g = rd * 8 + gg
                        op(PE, lambda: T_.transpose(pv[:, gg * 128:(gg + 1) * 128], BhT[:, g, :], identb[:]), reads=bh_d + [identb], writes=[pb], sig=(gg == 7))
                    op(ACT, lambda: S.copy(out=Bh[:, rd * 8:(rd + 1) * 8, :], in_=pv.rearrange("p (g c) -> p g c", c=128)), reads=[pb], writes=[Bh])
                dtmp = fw.sb("dtmp", [128, 4, 128], es=st)
                dtmp2 = fw.sb("dtmp2", [128, 4, 128], es=st)
                for rd in range(8):
                    pd_ = PS[4 + rd % 2]
                    for gg in range(4):
                        g = rd * 4 + gg
                        op(PE, lambda: T_.matmul(pd_[:, gg * 128:(gg + 1) * 128], lhsT=BhT[:, g, :], rhs=CDT[:, g, :], start=True, stop=True),
                           reads=bh_d + cd_d, writes=[pd_], sig=(gg == 3))
                    mb = mask[:].unsqueeze(1).to_broadcast([128, 4, 128])
                    pdv = pd_[:].rearrange("p (g c) -> p g c", c=128)
                    if r == 0:
                        op(DVE, lambda: V.tensor_tensor(out=dtmp[:], in0=pdv, in1=mb, op=ALU.mult), reads=[pd_, mask], writes=[dtmp])
                        op(DVE, lambda: V.tensor_tensor(out=dtmp2[:], in0=identf[:].unsqueeze(1).to_broadcast([128, 4, 128]),
                                                        in1=dcr[:, rd * 4:(rd + 1) * 4].unsqueeze(2).to_broadcast([128, 4, 128]), op=ALU.mult),
                           reads=[identf, dcr], writes=[dtmp2])
                        op(DVE, lambda: V.tensor_tensor(out=Dh[r][:, rd * 4:(rd + 1) * 4, :], in0=dtmp[:], in1=dtmp2[:], op=ALU.add),
                           reads=[dtmp, dtmp2], writes=[Dh[r]])
                    else:
                        op(DVE, lambda: V.tensor_tensor(out=Dh[r][:, rd * 4:(rd + 1) * 4, :], in0=pdv, in1=mb, op=ALU.mult), reads=[pd_, mask], writes=[Dh[r]])
                fw.barrier()

            with ExitStack() as sr:
                Wt = fw.sb("Wt", [128, 32, 128], BF16, es=sr)
                Sx = fw.sb("Sx", [128, 32, 128], BF16, es=sr)
                trib = fw.sb("trib", [128, 128], BF16, es=sr)
                mselb = fw.sb("mselb", [128, 4], BF16, es=sr)
                h0b = fw.sb("h0b", [1, 4096], BF16, es=sr)
                dma(POOL, trib[:], W["tri"][r], writes=[trib])
                dma(POOL, mselb[:], W["msel"], writes=[mselb])
                dma(POOL, h0b[:], W["h0row"][L, r], writes=[h0b])
                lrow = fw.sb("lrow", [128, 3, 512], es=sr)
                Aa = fw.sb("Aa", [128, 2, 512], es=sr)
                dtb = fw.sb("dtb", [128, 512], es=sr)
                mgr = fw.sb("mgr", [128, 512], es=sr)
                thr = fw.sb("thr", [128, 512], es=sr)
                tqr = fw.sb("tqr", [128, 512], es=sr)
                snr = fw.sb("snr", [128, 512], es=sr)
                csr = fw.sb("csr", [128, 512], es=sr)
                tiR = fw.sb("tiR", [128, 512], mybir.dt.int32, es=sr)
                E = [fw.sb(f"E{i}", [128, 2, 512], es=sr) for i in range(3)]
                tar = fw.sb("tar", [128, 256], es=sr)
                tbr = fw.sb("tbr", [128, 256], es=sr)

                def genR(Eo, col, magcol, conj):
                    op(ACT, lambda: S.activation(out=mgr[:], in_=Aa[:, 0, :], func=AF.Exp, scale=magcol), reads=[Aa, cst, kcol_sb], writes=[mgr])
                    op(DVE, lambda: V.tensor_scalar(out=thr[:], in0=Aa[:, 1, :], scalar1=col, scalar2=None, op0=ALU.mult), reads=[Aa, cst, kcol_sb], writes=[thr])
                    sincos((thr, thr[:]), (tqr, tqr[:]), (snr, snr[:]), (csr, csr[:]), (tiR, tiR[:]))
                    op(DVE, lambda: V.tensor_tensor(out=Eo[:, 0, :], in0=mgr[:], in1=csr[:], op=ALU.mult), reads=[mgr, csr], writes=[Eo])
                    if conj:
                        op(DVE, lambda: V.scalar_tensor_tensor(out=Eo[:, 1, :], in0=mgr[:], scalar=-1.0, in1=snr[:], op0=ALU.mult, op1=ALU.mult),
                           reads=[mgr, snr], writes=[Eo])
                    else:
                        op(DVE, lambda: V.tensor_tensor(out=Eo[:, 1, :], in0=mgr[:], in1=snr[:], op=ALU.mult), reads=[mgr, snr], writes=[Eo])

                def cmul(outv, outbuf, srcv, srcbuf, Et, hb, parts=slice(0, 128)):
                    er = Et[parts, 0, hb * 256:(hb + 1) * 256].rearrange("c (g p) -> c g p", p=64)
                    ei = Et[parts, 1, hb * 256:(hb + 1) * 256].rearrange("c (g p) -> c g p", p=64)
                    tav = tar[parts].rearrange("c (g p) -> c g p", p=64)
                    tbv = tbr[parts].rearrange("c (g p) -> c g p", p=64)
                    op(DVE, lambda: V.tensor_tensor(out=tav, in0=srcv[:, :, 0, :], in1=er, op=ALU.mult), reads=[srcbuf, Et], writes=[tar])
                    op(DVE, lambda: V.tensor_tensor(out=tbv, in0=srcv[:, :, 1, :], in1=ei, op=ALU.mult), reads=[srcbuf, Et], writes=[tbr])
                    op(DVE, lambda: V.tensor_tensor(out=outv[:, :, 0, :], in0=tav, in1=tbv, op=ALU.subtract), reads=[tar, tbr], writes=[outbuf])
                    op(DVE, lambda: V.tensor_tensor(out=tav, in0=srcv[:, :, 0, :], in1=ei, op=ALU.mult), reads=[srcbuf, Et], writes=[tar])
                    op(DVE, lambda: V.tensor_tensor(out=tbv, in0=srcv[:, :, 1, :], in1=er, op=ALU.mult), reads=[srcbuf, Et], writes=[tbr])
                    op(DVE, lambda: V.tensor_tensor(out=outv[:, :, 1, :], in0=tav, in1=tbv, op=ALU.add), reads=[tar, tbr], writes=[outbuf])

                for rd in range(4):
                    for i in range(3):
                        dma(SP, lrow[:, i, :], W["lam_row"][L, r, i][:, rd * 512:(rd + 1) * 512].partition_broadcast(128), writes=[lrow])
                    op(ACT, lambda: S.activation(out=dtb[:], in_=lrow[:, 2, :], func=AF.Exp), reads=[lrow], writes=[dtb])
                    for i in range(2):
                        op(DVE, lambda: V.tensor_tensor(out=Aa[:, i, :], in0=lrow[:, i, :], in1=dtb[:], op=ALU.mult), reads=[lrow, dtb], writes=[Aa])
                    genR(E[0], kcol_sb[:, r:r + 1], cst[:, 2 + r:3 + r], True)
                    genR(E[1], kcol_sb[:, 2 + r:3 + r], kcol_sb[:, 2 + r:3 + r], False)
                    genR(E[2], cst[:, 1:2], cst[:, 1:2], False)
                    for hb in range(2):
                        g0 = rd * 8 + hb * 4
                        pz = PS[hb]
                        for gg in range(4):
                            op(PE, lambda: T_.matmul(pz[:, gg * 128:(gg + 1) * 128], lhsT=U[:, g0 + gg, :], rhs=Bh[:, g0 + gg, :], start=True, stop=True),
                               reads=[U, Bh], writes=[pz], sig=(gg == 3))
                        zv = pz[:].rearrange("c (g two p) -> c g two p", two=2, p=64)
                        wv = Wt[:, g0:g0 + 4, :].rearrange("c g (two p) -> c g two p", two=2)
                        cmul(wv, Wt, zv, pz, E[0], hb)
                        px = PS[2 + hb]
                        op(PE, lambda: T_.matmul(px[:], lhsT=trib[:], rhs=Wt[:, g0:g0 + 4, :].rearrange("c g p -> c (g p)"), start=True, stop=False),
                           reads=[trib, Wt], writes=[px], sig=False)
                        op(PE, lambda: T_.matmul(px[:], lhsT=onesb[0:1, :], rhs=h0b[0:1, g0 * 128:(g0 + 4) * 128], start=False, stop=True),
                           reads=[onesb, h0b], writes=[px])
                        xv = px[:].rearrange("c (g two p) -> c g two p", two=2, p=64)
                        sv = Sx[:, g0:g0 + 4, :].rearrange("c g (two p) -> c g two p", two=2)
                        cmul(sv, Sx, xv, px, E[1], hb)
                        pf = PS[4 + hb]
                        op(PE, lambda: T_.matmul(pf[0:4, :], lhsT=mselb[:], rhs=Wt[:, g0:g0 + 4, :].rearrange("c g p -> c (g p)"), start=True, stop=True),
                           reads=[mselb, Wt], writes=[pf])
                        fv = pf[0:4, :].rearrange("c (g two p) -> c g two p", two=2, p=64)
                        ov = fin[0:4, hb * 512:(hb + 1) * 512].rearrange("c (g two p) -> c g two p", two=2, p=64)
                        cmul(ov, fin, fv, pf, E[2], hb, parts=slice(0, 4))
                    dma(SP, st_out[L, r][:, rd * 1024:(rd + 1) * 1024], fin[:], reads=[fin], writes=[STB])
                    pb = PS[6 + rd % 2]
                    pv = bfview(pb)
                    for gg in range(8):
                        g = rd * 8 + gg
                        op(PE, lambda: T_.transpose(pv[:, gg * 128:(gg + 1) * 128], Sx[:, g, :], identb[:]), reads=[Sx, identb], writes=[pb], sig=(gg == 7))
                    op(ACT, lambda: S.copy(out=ST[r][:, rd * 8:(rd + 1) * 8, :], in_=pv.rearrange("p (g c) -> p g c", c=128)), reads=[pb], writes=[ST[r]])
                fw.barrier()
            fw.barrier()


    with ExitStack() as sy:
        Ycm = fw.sb("Ycm", [128, 8, 512], es=sy)
        ysT = fw.sb("ysT", [128, 4, 1024], es=sy)
        gsT = fw.sb("gsT", [128, 4, 1024], BF16, es=sy)
        q1 = fw.sb("q1", [128, 1024], es=sy)
        q2_ = fw.sb("q2", [128, 1024], es=sy)
        wgl = fw.sb("wgl", [128, 4, 512], BF16, es=sy)
        bgl = fw.sb("bgl", [128, 4], es=sy)
        dma(POOL, wgl[:], W["w_glu"][L].rearrange("(k p) n -> p k n", p=128), writes=[wgl])
        dma(SP, bgl[:], W["b_gluT"][L], writes=[bgl])
        for rd in range(8):
            py = PS[rd % 2]
            for gg in range(4):
                g = rd * 4 + gg
                sl = py[:, gg * 128:(gg + 1) * 128]
                op(PE, lambda: T_.matmul(sl, lhsT=ST[0][:, g, :], rhs=ChT[0][:, g, :], start=True, stop=False), reads=[ST[0], ChT[0]], writes=[py], sig=False)
                op(PE, lambda: T_.matmul(sl, lhsT=ST[1][:, g, :], rhs=ChT[1][:, g, :], start=False, stop=False), reads=[ST[1], ChT[1]], writes=[py], sig=False)
                op(PE, lambda: T_.matmul(sl, lhsT=U[:, g, :], rhs=Dh[0][:, g, :], start=False, stop=False), reads=[U, Dh[0]], writes=[py], sig=False)
                op(PE, lambda: T_.matmul(sl, lhsT=U[:, g, :], rhs=Dh[1][:, g, :], start=False, stop=True), reads=[U, Dh[1]], writes=[py], sig=(gg == 3))
            op(DVE, lambda: V.tensor_copy(Ycm[:].rearrange("c j (g h) -> c g j h", h=16)[:, rd * 4:(rd + 1) * 4, :, :],
                                          py[:].rearrange("c (g j h) -> c g j h", g=4, j=8)), reads=[py], writes=[Ycm])
        for j in range(8):
            pt = PS[2 + j % 2]
            for q in range(4):
                op(PE, lambda: T_.transpose(pt[:, q * 128:(q + 1) * 128], Ycm[:, j, q * 128:(q + 1) * 128], identf[:]), reads=[Ycm, identf], writes=[pt], sig=(q == 3))
            op(ACT, lambda: S.copy(out=ysT[:, :, j::8], in_=pt[:].rearrange("p (q c) -> p q c", c=128)), reads=[pt], writes=[ysT])
        for q in range(4):
            op(ACT, lambda: S.activation(out=q1[:], in_=ysT[:, q, :], func=AF.Square), reads=[ysT], writes=[q1])
            op(DVE, lambda: V.tensor_scalar(out=q1[:], in0=q1[:], scalar1=0.044715, scalar2=1.0, op0=ALU.mult, op1=ALU.add), reads=[q1], writes=[q1])
            op(DVE, lambda: V.tensor_tensor(out=q1[:], in0=q1[:], in1=ysT[:, q, :], op=ALU.mult), reads=[q1, ysT], writes=[q1])
            op(ACT, lambda: S.activation(out=q2_[:], in_=q1[:], func=AF.Sigmoid, scale=1.5957691216057308), reads=[q1], writes=[q2_])
            op(DVE, lambda: V.tensor_tensor(out=gsT[:, q, :], in0=ysT[:, q, :], in1=q2_[:], op=ALU.mult), reads=[ysT, q2_], writes=[gsT])
        for qo in range(4):
            for th in range(2):
                ts = slice(th * 512, (th + 1) * 512)
                pa = PS[4 + (qo * 2 + th) % 2]
                for k in range(4):
                    op(PE, lambda: T_.matmul(pa[:], lhsT=wgl[:, k, qo * 128:(qo + 1) * 128], rhs=gsT[:, k, ts], start=(k == 0), stop=(k == 3)),
                       reads=[wgl, gsT], writes=[pa], sig=(k == 3))
                op(ACT, lambda: S.activation(out=q1[:, 0:512], in_=pa[:], func=AF.Sigmoid, bias=bgl[:, qo:qo + 1]), reads=[pa, bgl], writes=[q1])
                op(DVE, lambda: V.tensor_tensor(out=ys2T[:, qo, ts], in0=gsT[:, qo, ts], in1=q1[:, 0:512], op=ALU.mult), reads=[gsT, q1], writes=[ys2T])
        fw.barrier()


PROMPT_SPLIT = [(0, 3), (3, 6), (6, 9), (9, 12), (12, 14), (14, 16)]


def _pos_embed():
    quarter = 256
    freqs = np.exp(np.float32(-math.log(10000.0)) * np.arange(quarter, dtype=np.float32) / np.float32(quarter)).astype(np.float32)
    r = np.repeat(np.arange(16, dtype=np.float32), 64)
    col = np.tile(np.arange(64, dtype=np.float32), 16)
    ar = (r[:, None] * freqs).astype(np.float32)
    ac = (col[:, None] * freqs).astype(np.float32)
    return np.concatenate([np.sin(ar), np.cos(ar), np.sin(ac), np.cos(ac)], axis=-1).astype(np.float32)


def _core_consts(is_sample):
    Lq = 1024 if is_sample else 256
    nseq = 1024 // Lq
    idx = np.arange(1024)
    seq = idx // Lq
    loc = idx % Lq
    ang = 2.0 * np.pi * ((loc[:, None] * loc[None, :]) % Lq) / Lq
    same = (seq[:, None] == seq[None, :])
    sc = 1.0 / math.sqrt(128.0 * Lq)
    CL = (np.cos(ang) * sc * same).astype(np.float32)
    SL = (-np.sin(ang) * sc * same).astype(np.float32)
    nloc = Lq // 8
    c = np.arange(128)
    cs, cl = c // nloc, c % nloc
    kf, kb = cl, nloc - 1 - cl
    tri = np.zeros((2, 128, 128), np.float32)
    samec = cs[:, None] == cs[None, :]
    tri[0] = (samec & (kf[:, None] < kf[None, :])).astype(np.float32)
    tri[1] = (samec & (kb[:, None] < kb[None, :])).astype(np.float32)
    kcol = np.stack([8.0 * (kf + 1), 8.0 * (kb + 1), 8.0 * kf, 8.0 * kb], axis=1).astype(np.float32)
    msel = np.zeros((128, 4), np.float32)
    msel[c, np.minimum(c // 32, 3)] = 1.0
    flag = np.full((128, 1), 1.0 if is_sample else 0.0, np.float32)
    return dict(CLt=CL, SLt=SL, tri=tri, kcol=kcol, msel=msel, flag=flag)


_NC_CACHE = {}
_DBG = {}


def kernel(**inp):
    f32 = lambda a: np.ascontiguousarray(np.asarray(a), dtype=np.float32)
    I = {k: np.asarray(v) for k, v in inp.items()}
    D_ = I["w_mod"].shape[0]
    shared = {}
    shared["w_mod"] = f32(I["w_mod"])
    shared["b_mod"] = f32(I["b_mod"][:, None, :])
    shared["n1gT"] = f32(I["norm1_g"].reshape(D_, 8, 128).transpose(0, 2, 1))
    shared["n2gT"] = f32(I["norm2_g"].reshape(D_, 8, 128).transpose(0, 2, 1))
    shared["w_in"] = f32(I["w_in"])
    shared["b_inT"] = f32(I["b_in"].reshape(D_, 40, 128).transpose(0, 2, 1))
    shared["b_zs"] = f32(I["b_in"][:, None, 1536:2048])
    shared["w_four"] = f32(I["w_four"])
    shared["cdwT"] = f32(I["conv_dw"].reshape(D_, 31, 4, 128).transpose(0, 3, 2, 1))
    shared["cvec"] = f32(np.stack([I["conv_dw_b"], I["conv_ln_g"], I["conv_ln_b"]], axis=1).reshape(D_, 3, 4, 128).transpose(0, 3, 1, 2))
    shared["w_conv_out"] = f32(I["w_conv_out"])
    ldt = np.repeat(I["ssm_log_dt"][..., None], 64, axis=-1)
    shared["lam_row"] = f32(np.stack([I["ssm_lam_re"], I["ssm_lam_im"], ldt], axis=2).reshape(D_, 2, 3, 1, 2048))
    lt = np.stack([I["ssm_lam_re"], I["ssm_lam_im"], ldt], axis=2)
    lt = lt.transpose(0, 1, 4, 2, 3)
    shared["lamT"] = f32(np.concatenate([lt, lt], axis=2))
    b2 = np.stack([I["ssm_b_re"], I["ssm_b_im"]], axis=2)
    b2 = b2.transpose(0, 1, 4, 2, 3, 5).reshape(D_, 2, 64, 2, 512)
    shared["BT2"] = f32(np.concatenate([b2, b2], axis=2))
    c2 = np.stack([I["ssm_c_re"], I["ssm_c_im"]], axis=2)
    c2 = c2.transpose(0, 1, 5, 2, 3, 4).reshape(D_, 2, 64, 2, 512)
    shared["CT2"] = f32(np.concatenate([c2, c2], axis=2))
    dd = I["ssm_d"].reshape(D_, 32, 16)
    shared["dcolrep"] = f32(np.broadcast_to(dd.transpose(0, 2, 1)[:, None, :, :], (D_, 8, 16, 32)).reshape(D_, 128, 32))
    shared["w_glu"] = f32(I["w_ssm_glu"])
    shared["b_gluT"] = f32(I["b_ssm_glu"].reshape(D_, 4, 128).transpose(0, 2, 1))
    shared["w_ssm_out"] = f32(I["w_ssm_out"])
    shared["w_out"] = f32(I["w_out"])
    shared["router_w"] = f32(I["router_w"])
    shared["router_b"] = f32(I["router_b"][:, None, :])
    shared["w_gu"] = f32(I["w_gate_up"])
    shared["b_guT"] = f32(I["b_gate_up"].reshape(D_, NEXP, 16, 128).transpose(0, 3, 1, 2).reshape(D_, 128, NEXP * 16))
    shared["w_dn"] = f32(I["w_down"])
    shared["b_dn"] = f32(I["b_down"])
    shared["fin_g"] = f32(I["final_norm_g"][None, :])
    cc = np.arange(128)
    angc = 2.0 * np.pi * ((cc[:, None] * cc[None, :]) % 128) / 128.0
    shared["ccsc"] = f32(np.concatenate([np.cos(angc), np.sin(angc)], axis=1))
    j = np.arange(8, dtype=np.float32)
    jv = np.zeros((2, 128, 4, 8), np.float32)
    jv[0, :, 0], jv[0, :, 1], jv[0, :, 2], jv[0, :, 3] = 7 - j, 8 - j, j + 1, j - 7
    jv[1, :, 0], jv[1, :, 1], jv[1, :, 2], jv[1, :, 3] = j, j + 1, 8 - j, -j
    shared["jvals"] = jv
    jj = np.arange(128) // 16
    dm = np.zeros((2, 128, 128), np.float32)
    dm[0] = (jj[:, None] <= jj[None, :])
    dm[1] = (jj[:, None] >= jj[None, :])
    shared["dmask"] = dm

    cs_s, cs_p = _core_consts(True), _core_consts(False)
    pos = _pos_embed()
    zeros_pos = np.zeros((1024, 1024), np.float32)
    zeros_h0 = np.zeros((D_, 2, 1, 4096), np.float32)
    in_maps = []
    for core in range(8):
        m = dict(shared)
        if core < 2:
            b = core
            m.update(cs_s)
            m["xin"] = f32(I["x_sample"][b])
            m["pos"] = pos
            cond = I["c"][b]
            h0 = np.stack([I["state_ssm_re"][b], I["state_ssm_im"][b]], axis=3)
            m["h0row"] = f32(h0.reshape(D_, 2, 1, 4096))
        else:
            lo, hi = PROMPT_SPLIT[core - 2]
            m.update(cs_p)
            xi = np.zeros((4, 256, 1024), np.float32)
            xi[:hi - lo] = I["x_prompt"][lo:hi]
            m["xin"] = xi.reshape(1024, 1024)
            m["pos"] = zeros_pos
            cond = I["c_ctx"]
            m["h0row"] = zeros_h0
        m["condT"] = f32(np.asarray(cond).reshape(8, 128).T)
        in_maps.append(m)

    if "nc" not in _NC_CACHE:
        _NC_CACHE["nc"] = build_nc(depth=_DBG.get("depth", DEPTH), dbg=_DBG.get("dbg", False), skip=_DBG.get("skip", ()))
    nc = _NC_CACHE["nc"]
    res = run_bass_kernel_spmd(nc, in_maps, core_ids=list(range(8)))
    R = res.results
    _DBG["R"] = R if _DBG.get("dbg", False) else None
    y_prompt = np.zeros((16, 256, 1024), np.float32)
    y_sample = np.zeros((2, 1024, 1024), np.float32)
    ns_re = np.zeros((16, D_, 2, 32, 64), np.float32)
    ns_im = np.zeros((16, D_, 2, 32, 64), np.float32)
    for core in range(8):
        yo = np.asarray(R[core]["y_out"], dtype=np.float32)
        if core < 2:
            y_sample[core] = yo
        else:
            lo, hi = PROMPT_SPLIT[core - 2]
            so = np.asarray(R[core]["st_out"], dtype=np.float32).reshape(D_, 2, 4, 32, 2, 64)
            for s in range(hi - lo):
                y_prompt[lo + s] = yo[s * 256:(s + 1) * 256]
                ns_re[lo + s] = so[:, :, s, :, 0, :]
                ns_im[lo + s] = so[:, :, s, :, 1, :]
    return (y_prompt, y_sample, ns_re, ns_im)
```
